# Optimizing a Trainium2 kernel written in Bass

```python
import jax, jax.numpy as jnp
from jax import lax
import numpy as np

D_MODEL = 2048
BATCH = 1
SEQ = 8192
DEPTH = 1

MIX_WIDTH = D_MODEL
MLSTM_HEADS = 4
MLSTM_WIDTH = MIX_WIDTH // 2
MLSTM_DV = MLSTM_WIDTH // MLSTM_HEADS
MLSTM_DQK = MLSTM_DV // 2
MLSTM_CHUNK = 64
GATE_SOFTCAP = 15.0
FOX_HEAD_DIM = 128
FOX_WIDTH = MIX_WIDTH - MLSTM_WIDTH
FOX_HEADS = FOX_WIDTH // FOX_HEAD_DIM
FOX_BLOCK = 128
D_IN_PROJ = 2 * MLSTM_HEADS * MLSTM_DQK + 2 * MLSTM_WIDTH + 2 * MLSTM_HEADS + 3 * FOX_WIDTH + FOX_HEADS
N_EXPERTS = 32
TOP_K = 4
D_FF = D_MODEL
SWIGLU_LIMIT = 7.0
SWIGLU_ALPHA = 1.702
MOE_BLOCK = 128
ADA_SCALE = 0.5
EPS = 1e-6

kernel_name = 'hymba_mlstm_fox_moe_adaln_block'


def rms_last(a, g):
    a = a.astype(jnp.float32)
    return a * lax.rsqrt(jnp.mean(a * a, axis=-1, keepdims=True) + EPS) * g.astype(jnp.float32)


def modulate(x, g, shift, scale):
    return rms_last(x, g) * (1.0 + scale[:, None, :]) + shift[:, None, :]


def soft_cap(a):
    return GATE_SOFTCAP * jnp.tanh(a / GATE_SOFTCAP)


def split_heads(a, n_heads):
    b, s, _ = a.shape
    return a.reshape(b, s, n_heads, -1).transpose(0, 2, 1, 3)


def merge_heads(a):
    b, h, s, d = a.shape
    return a.transpose(0, 2, 1, 3).reshape(b, s, h * d)


def mlstm_chunkwise(q, k, v, i_pre, f_pre):
    b, h, s, _ = q.shape
    L = MLSTM_CHUNK
    nc = s // L
    q = q * (MLSTM_DQK ** -0.5)
    log_f = jax.nn.log_sigmoid(f_pre)

    def to_chunks(a):
        return jnp.moveaxis(a.reshape(b, h, nc, L, *a.shape[3:]), 2, 0)

    causal = jnp.tril(jnp.ones((L, L), dtype=bool))

    def step(carry, inp):
        C, n, m = carry
        qc, kc, vc, ic, fc = inp
        g = jnp.cumsum(fc, axis=-1)
        dmat = jnp.where(causal, g[..., :, None] - g[..., None, :] + ic[..., None, :], -jnp.inf)
        m_inter = g + m[..., None]
        m_t = jnp.maximum(m_inter, jnp.max(dmat, axis=-1))
        scores = jnp.einsum('bhtd,bhsd->bhts', qc, kc) * jnp.exp(dmat - m_t[..., None])
        inter = jnp.exp(m_inter - m_t)
        num = jnp.einsum('bhts,bhsv->bhtv', scores, vc) + inter[..., None] * jnp.einsum('bhtd,bhdv->bhtv', qc, C)
        den = jnp.sum(scores, axis=-1) + inter * jnp.einsum('bhtd,bhd->bht', qc, n)
        h_out = num / jnp.maximum(jnp.abs(den), jnp.exp(-m_t))[..., None]
        g_last = g[..., -1]
        log_w = g_last[..., None] - g + ic
        m_new = jnp.maximum(g_last + m, jnp.max(log_w, axis=-1))
        w = jnp.exp(log_w - m_new[..., None])
        decay = jnp.exp(g_last + m - m_new)
        C = decay[..., None, None] * C + jnp.einsum('bhs,bhsd,bhsv->bhdv', w, kc, vc)
        n = decay[..., None] * n + jnp.einsum('bhs,bhsd->bhd', w, kc)
        return (C, n, m_new), h_out

    init = (jnp.zeros((b, h, MLSTM_DQK, MLSTM_DV), jnp.float32),
            jnp.zeros((b, h, MLSTM_DQK), jnp.float32),
            jnp.zeros((b, h), jnp.float32))
    _, hs = lax.scan(step, init, (to_chunks(q), to_chunks(k), to_chunks(v), to_chunks(i_pre), to_chunks(log_f)))
    return jnp.moveaxis(hs, 0, 2).reshape(b, h, s, MLSTM_DV)


def forgetting_attention(q, k, v, f_pre):
    b, h, s, d = q.shape
    nb = s // FOX_BLOCK
    F = jnp.cumsum(jax.nn.log_sigmoid(f_pre), axis=-1)
    qb = jnp.moveaxis(q.reshape(b, h, nb, FOX_BLOCK, d), 2, 0)
    Fb = jnp.moveaxis(F.reshape(b, h, nb, FOX_BLOCK), 2, 0)
    kpos = jnp.arange(s)
    scale = FOX_HEAD_DIM ** -0.5

    def block(args):
        qi, Fi, bi = args
        qpos = bi * FOX_BLOCK + jnp.arange(FOX_BLOCK)
        logits = jnp.einsum('bhtd,bhsd->bhts', qi, k) * scale + Fi[..., :, None] - F[..., None, :]
        logits = jnp.where(kpos[None, :] <= qpos[:, None], logits, -jnp.inf)
        p = jax.nn.softmax(logits, axis=-1)
        return jnp.einsum('bhts,bhsd->bhtd', p, v)

    out = lax.map(block, (qb, Fb, jnp.arange(nb)))
    return jnp.moveaxis(out, 0, 2).reshape(b, h, s, d)


def moe_ffn(xt, w_router, b_router, w_up_gate, b_up_gate, w_down, b_down):
    t, d = xt.shape
    logits = (xt @ w_router + b_router).astype(jnp.float32)
    top_logits, top_idx = lax.top_k(logits, TOP_K)
    top_w = jax.nn.softmax(top_logits, axis=-1)
    a = t * TOP_K
    flat_e = top_idx.reshape(a)
    flat_tok = jnp.repeat(jnp.arange(t, dtype=jnp.int32), TOP_K)
    flat_w = top_w.reshape(a)
    order = jnp.argsort(flat_e)
    sorted_e = flat_e[order]
    counts = jnp.zeros((N_EXPERTS,), jnp.int32).at[flat_e].add(1)
    n_blocks_e = (counts + MOE_BLOCK - 1) // MOE_BLOCK
    blk_end = jnp.cumsum(n_blocks_e)
    blk_start = blk_end - n_blocks_e
    tok_start = jnp.cumsum(counts) - counts
    rank = jnp.arange(a, dtype=jnp.int32) - tok_start[sorted_e]
    dest = blk_start[sorted_e] * MOE_BLOCK + rank
    nb = -(-a // MOE_BLOCK) + N_EXPERTS
    rows = nb * MOE_BLOCK
    row_tok = jnp.full((rows,), t, jnp.int32).at[dest].set(flat_tok[order])
    row_w = jnp.zeros((rows,), jnp.float32).at[dest].set(flat_w[order])
    block_e = jnp.minimum(jnp.searchsorted(blk_end, jnp.arange(nb), side='right'), N_EXPERTS - 1)
    x_pad = jnp.concatenate([xt, jnp.zeros((1, d), xt.dtype)], axis=0)
    xb = x_pad[row_tok].reshape(nb, MOE_BLOCK, d)

    def expert_block(args):
        xi, e = args
        gu = xi @ w_up_gate[e] + b_up_gate[e]
        gate = jnp.minimum(gu[:, :D_FF], SWIGLU_LIMIT)
        up = jnp.clip(gu[:, D_FF:], -SWIGLU_LIMIT, SWIGLU_LIMIT)
        act = (up + 1.0) * gate * jax.nn.sigmoid(SWIGLU_ALPHA * gate)
        return act @ w_down[e] + b_down[e]

    yb = lax.map(expert_block, (xb, block_e)).reshape(rows, d).astype(jnp.float32)
    out = jnp.zeros((t + 1, d), jnp.float32).at[row_tok].add(yb * row_w[:, None])
    return out[:t]


def setup_inputs(seed: int = 0) -> dict:
    key = jax.random.key(seed)
    ks = jax.random.split(key, 24)
    f32 = jnp.float32
    L, D, E, F = DEPTH, D_MODEL, N_EXPERTS, D_FF

    def nrm(k, shape, scale):
        return jax.random.normal(k, shape, f32) * scale

    return {
        'x': nrm(ks[0], (BATCH, SEQ, D), 1.0),
        'c': nrm(ks[1], (BATCH, D), 1.0),
        'w_ada': nrm(ks[2], (L, D, 6 * D), ADA_SCALE * D ** -0.5),
        'b_ada': nrm(ks[3], (L, 6 * D), 0.02),
        'norm_mix': 1.0 + nrm(ks[4], (L, D), 0.02),
        'w_in': nrm(ks[5], (L, D, D_IN_PROJ), D ** -0.5),
        'b_i': nrm(ks[6], (L, MLSTM_HEADS), 0.1),
        'b_f': jnp.linspace(3.0, 6.0, MLSTM_HEADS, dtype=f32)[None, :] + nrm(ks[7], (L, MLSTM_HEADS), 0.1),
        'fox_b_f': jnp.linspace(1.0, 5.0, FOX_HEADS, dtype=f32)[None, :] + nrm(ks[8], (L, FOX_HEADS), 0.1),
        'fox_q_norm': 1.0 + nrm(ks[9], (L, FOX_HEAD_DIM), 0.02),
        'fox_k_norm': 1.0 + nrm(ks[10], (L, FOX_HEAD_DIM), 0.02),
        'mlstm_out_norm': 1.0 + nrm(ks[11], (L, MLSTM_HEADS, MLSTM_DV), 0.02),
        'fox_out_norm': 1.0 + nrm(ks[12], (L, FOX_HEADS, FOX_HEAD_DIM), 0.02),
        'w_out': nrm(ks[13], (L, MIX_WIDTH, D), MIX_WIDTH ** -0.5),
        'norm_ffn': 1.0 + nrm(ks[14], (L, D), 0.02),
        'w_router': nrm(ks[15], (L, D, E), D ** -0.5),
        'b_router': nrm(ks[16], (L, E), 0.01),
        'w_up_gate': nrm(ks[17], (L, E, D, 2 * F), D ** -0.5),
        'b_up_gate': nrm(ks[18], (L, E, 2 * F), 0.02),
        'w_down': nrm(ks[19], (L, E, F, D), F ** -0.5),
        'b_down': nrm(ks[20], (L, E, D), 0.02),
        'w_ada_final': nrm(ks[21], (D, 2 * D), ADA_SCALE * D ** -0.5),
        'b_ada_final': nrm(ks[22], (2 * D,), 0.02),
        'norm_final': 1.0 + nrm(ks[23], (D,), 0.02),
    }


def reference(x, c, w_ada, b_ada, norm_mix, w_in, b_i, b_f, fox_b_f, fox_q_norm, fox_k_norm,
              mlstm_out_norm, fox_out_norm, w_out, norm_ffn, w_router, b_router, w_up_gate,
              b_up_gate, w_down, b_down, w_ada_final, b_ada_final, norm_final):
    b, s, d = x.shape
    c_act = jax.nn.silu(c.astype(jnp.float32))
    sizes = [MLSTM_HEADS * MLSTM_DQK, MLSTM_HEADS * MLSTM_DQK, MLSTM_WIDTH, MLSTM_WIDTH,
             MLSTM_HEADS, MLSTM_HEADS, FOX_WIDTH, FOX_WIDTH, FOX_WIDTH, FOX_HEADS]
    offsets = []
    acc = 0
    for sz in sizes[:-1]:
        acc += sz
        offsets.append(acc)
    h_res = x.astype(jnp.float32)
    for l in range(DEPTH):
        mod = c_act @ w_ada[l] + b_ada[l]
        sh1, sc1, g1, sh2, sc2, g2 = jnp.split(mod, 6, axis=-1)
        hn = modulate(h_res, norm_mix[l], sh1, sc1)
        proj = (hn @ w_in[l]).astype(jnp.float32)
        mq, mk, mv, mo, mi, mf, fq, fk, fv, ff = jnp.split(proj, offsets, axis=-1)
        i_pre = soft_cap(mi + b_i[l]).transpose(0, 2, 1)
        f_pre = soft_cap(mf + b_f[l]).transpose(0, 2, 1)
        hm = mlstm_chunkwise(split_heads(mq, MLSTM_HEADS), split_heads(mk, MLSTM_HEADS),
                             split_heads(mv, MLSTM_HEADS), i_pre, f_pre)
        hm = merge_heads(rms_last(hm, mlstm_out_norm[l][:, None, :])) * jax.nn.sigmoid(mo)
        qf = rms_last(split_heads(fq, FOX_HEADS), fox_q_norm[l])
        kf = rms_last(split_heads(fk, FOX_HEADS), fox_k_norm[l])
        fox_f = (ff + fox_b_f[l]).transpose(0, 2, 1)
        hf = forgetting_attention(qf, kf, split_heads(fv, FOX_HEADS), fox_f)
        hf = merge_heads(rms_last(hf, fox_out_norm[l][:, None, :]))
        y = jnp.concatenate([hm, hf], axis=-1) @ w_out[l]
        h_res = h_res + g1[:, None, :] * y
        h2 = modulate(h_res, norm_ffn[l], sh2, sc2)
        ffn = moe_ffn(h2.reshape(b * s, d), w_router[l], b_router[l], w_up_gate[l],
                      b_up_gate[l], w_down[l], b_down[l]).reshape(b, s, d)
        h_res = h_res + g2[:, None, :] * ffn
    modf = c_act @ w_ada_final + b_ada_final
    shf, scf = jnp.split(modf, 2, axis=-1)
    out = modulate(h_res, norm_final, shf, scf)
    return out.astype(x.dtype)
```

```python
import contextlib
import numpy as np
import ml_dtypes
import concourse.bass as bass
import concourse.mybir as mybir
from concourse.bass_utils import run_bass_kernel_spmd

F32 = mybir.dt.float32
BF16 = mybir.dt.bfloat16
ALU = mybir.AluOpType
AF = mybir.ActivationFunctionType
AX = mybir.AxisListType
NCORES = 8

D = 2048
KC = D // 128
SEQ = 8192
EPS = 1e-6


class Res:
    __slots__ = ("name", "w", "r", "sem", "cnt")

    def __init__(self, name):
        self.name = name
        self.w = None
        self.r = []
        self.sem = None
        self.cnt = 0


class Prog:
    ENG = ("pe", "act", "dve", "pool", "sp")

    def __init__(self, nc, stack):
        self.nc = nc
        self.stack = stack
        self.ops = []
        self.e = {"pe": nc.tensor, "act": nc.scalar, "dve": nc.vector, "pool": nc.gpsimd, "sp": nc.sync}
        self.nres = 0

    def res(self, name=None):
        self.nres += 1
        return Res(name or f"r{self.nres}")

    def sb(self, name, shape, dt):
        t = self.stack.enter_context(self.nc.sbuf_tensor(name, list(shape), dt))
        return t

    def ps(self, name, shape, dt=F32):
        return self.stack.enter_context(self.nc.psum_tensor(name, list(shape), dt))

    def op(self, eng, fn, reads=(), writes=(), dma=None):
        i = len(self.ops)
        deps = set()
        for r in reads:
            if r.w is not None:
                deps.add(r.w)
        for w in writes:
            if w.w is not None:
                deps.add(w.w)
            deps.update(w.r)
        for r in reads:
            r.r.append(i)
        for w in writes:
            w.w = i
            w.r = []
        self.ops.append(dict(eng=eng, fn=fn, deps=deps, dma=dma, wset=set(id(w) for w in writes),
                             rset=set(id(r) for r in reads)))
        return i

    def emit(self, final_ops=()):
        nc = self.nc
        ops = self.ops
        def stream(o):
            return ("dma", id(o["dma"])) if o["dma"] is not None else o["eng"]
        seen = {e: {} for e in self.ENG}
        pos = {}
        kept = []
        signal = set()
        for i, o in enumerate(ops):
            k = {}
            for d in o["deps"]:
                od = ops[d]
                sd = stream(od)
                if sd == o["eng"] and od["dma"] is None:
                    if o["eng"] == "pe":
                        continue
                    if not (od["wset"] & o["rset"]):
                        continue
                k[sd] = max(k.get(sd, -1), d)
            kk = []
            for sd, d in k.items():
                if seen[o["eng"]].get(sd, -1) >= d:
                    continue
                seen[o["eng"]][sd] = d
                kk.append((sd, d))
                signal.add(d)
            kept.append(kk)
        for d in final_ops:
            signal.add(d)
        for i, o in enumerate(ops):
            if o["dma"] is not None:
                signal.add(i)
        sems = {}
        def sem_of(sd):
            if sd not in sems:
                sems[sd] = self.stack.enter_context(nc.semaphore(f"s{len(sems)}"))
            return sems[sd]
        val = {}
        cur = {}
        for i, o in enumerate(ops):
            if i in signal:
                sd = stream(o)
                inc = 16 if o["dma"] is not None else 1
                cur[sd] = cur.get(sd, 0) + inc
                val[i] = cur[sd]
        for i, o in enumerate(ops):
            eng = self.e[o["eng"]]
            for sd, d in kept[i]:
                eng.wait_ge(sem_of(sd), val[d])
            inst = o["fn"]()
            if i in signal:
                sd = stream(o)
                inst.then_inc(sem_of(sd), 16 if o["dma"] is not None else 1)
        for d in final_ops:
            self.e["sp"].wait_ge(sem_of(stream(ops[d])), val[d])


def new_nc():
    return bass.Bass("TRN2", target_bir_lowering=False)


def build_l0():
    nc = new_nc()
    NW = 2048
    c_in = nc.dram_tensor("c", [128, KC], F32, kind="ExternalInput").ap()
    w_in = nc.dram_tensor("w", [D, NW], F32, kind="ExternalInput").ap()
    b_in = nc.dram_tensor("b", [1, NW], F32, kind="ExternalInput").ap()
    out = nc.dram_tensor("mod", [1, NW], F32, kind="ExternalOutput").ap()
    with contextlib.ExitStack() as st:
        P = Prog(nc, st)
        ct = P.sb("ct", [128, KC], F32); r_ct = P.res()
        ca = P.sb("ca", [128, KC], F32); r_ca = P.res()
        bt = P.sb("bt", [1, NW], F32); r_bt = P.res()
        ot = P.sb("ot", [1, NW], F32); r_ot = P.res()
        wt = [P.sb(f"wt{i}", [128, KC, 512], F32) for i in range(2)]
        r_wt = [P.res() for _ in range(2)]
        pst = [P.ps(f"ps{i}", [1, 512]) for i in range(2)]
        r_ps = [P.res() for _ in range(2)]
        P.op("sp", lambda: nc.sync.dma_start(out=ct[:], in_=c_in), writes=[r_ct], dma=r_ct)
        P.op("sp", lambda: nc.sync.dma_start(out=bt[:], in_=b_in), writes=[r_bt], dma=r_bt)
        P.op("act", lambda: nc.scalar.activation(out=ca[:], in_=ct[:], func=AF.Silu), reads=[r_ct], writes=[r_ca])
        wv = w_in.rearrange("(kc p) n -> p kc n", p=128)
        for g in range(4):
            b = g % 2
            P.op("sp", lambda g=g, b=b: nc.sync.dma_start(out=wt[b][:], in_=wv[:, :, g * 512:(g + 1) * 512]),
                 writes=[r_wt[b]], dma=r_wt[b])
            for kc in range(KC):
                P.op("pe", lambda b=b, kc=kc: nc.tensor.matmul(pst[b][:], lhsT=ca[:, kc:kc + 1], rhs=wt[b][:, kc, :],
                                                               start=(kc == 0), stop=(kc == KC - 1)),
                     reads=[r_ca, r_wt[b]], writes=[r_ps[b]])
            P.op("dve", lambda g=g, b=b: nc.vector.tensor_tensor(out=ot[:, g * 512:(g + 1) * 512], in0=pst[b][:],
                                                                 in1=bt[:, g * 512:(g + 1) * 512], op=ALU.add),
                 reads=[r_ps[b], r_bt], writes=[r_ot])
        f = P.op("sp", lambda: nc.sync.dma_start(out=out, in_=ot[:]), reads=[r_ot], dma=r_ot)
        P.emit(final_ops=[f])
    return nc


def run_l0(c, w_ada, b_ada, w_ada_final, b_ada_final):
    wcat = np.concatenate([w_ada[0], w_ada_final], axis=1)
    bcat = np.concatenate([b_ada[0], b_ada_final], axis=0)
    cl = np.ascontiguousarray(c[0].reshape(KC, 128).T)
    in_maps = []
    for j in range(NCORES):
        in_maps.append({"c": cl, "w": np.ascontiguousarray(wcat[:, j * 2048:(j + 1) * 2048]),
                        "b": np.ascontiguousarray(bcat[None, j * 2048:(j + 1) * 2048])})
    res = run_bass_kernel_spmd(build_l0(), in_maps, core_ids=list(range(NCORES)))
    return np.concatenate([r["mod"][0] for r in res.results])


NC1 = 1027


def build_l1(seq):
    NT = seq // 128
    nc = new_nc()
    xT = nc.dram_tensor("xT", [D, seq], F32, kind="ExternalInput").ap()
    w1 = nc.dram_tensor("w1", [D, NC1], F32, kind="ExternalInput").ap()
    v16d = nc.dram_tensor("v16", [128, 48], F32, kind="ExternalInput").ap()
    scald = nc.dram_tensor("scal", [1, 3], F32, kind="ExternalInput").ap()
    gvecd = nc.dram_tensor("gvec", [128, 512], F32, kind="ExternalInput").ap()
    cf32d = nc.dram_tensor("cf32", [128, 256], F32, kind="ExternalInput").ap()
    cbfd = nc.dram_tensor("cbf", [128, 384], BF16, kind="ExternalInput").ap()
    hout = nc.dram_tensor("hout", [seq, 256], BF16, kind="ExternalOutput").ap()
    V, S, G, T = nc.vector, nc.scalar, nc.gpsimd, nc.tensor
    with contextlib.ExitStack() as st:
        P = Prog(nc, st)
        R = P.res
        bank = [P.ps(f"bank{i}", [128, 512]) for i in range(8)]
        rb = [R(f"bank{i}") for i in range(8)]
        v16 = P.sb("v16t", [128, 48], F32); r_v16 = R()
        gs = P.sb("gs", [128, 16], F32); r_gs = R()
        scal = P.sb("scalt", [1, 3], F32); r_scal = R()
        gv = P.sb("gv", [128, 512], F32); r_gv = R()
        c32 = P.sb("c32", [128, 256], F32); r_c32 = R()
        cb = P.sb("cb", [128, 384], BF16); r_cb = R()
        UT32, ONE32 = c32[:, 0:128], c32[:, 128:256]
        IDB, UTB, ONEB = cb[:, 0:128], cb[:, 128:256], cb[:, 256:384]
        wst = [P.sb(f"wst{i}", [128, NC1], F32) for i in range(2)]; r_wst = [R(), R()]
        Wb = P.sb("Wb", [128, KC, NC1 + 1], BF16); r_Wb = R()
        shWrow = P.sb("shWrow", [1, NC1], F32); r_shWrow = R()
        shWb = P.sb("shWb", [128, NC1], F32); r_shWb = R()
        XG = 256
        xst = [P.sb(f"xst{i}", [128, KC, XG], F32) for i in range(2)]; r_xst = [R(), R()]
        xb = [P.sb(f"xb{i}", [128, KC, XG], BF16) for i in range(2)]; r_xb = [R(), R()]
        sq = [P.sb(f"sq{i}", [128, KC, XG], BF16) for i in range(2)]; r_sq = [R(), R()]
        pj = [P.sb(f"pj{i}", [128, NC1], F32) for i in range(2)]; r_pj = [R(), R()]
        kT = P.sb("kT", [128, seq], BF16); r_kT = [R() for _ in range(NT)]
        vaug = P.sb("vaug", [128, NT, 130], BF16); r_v = [R() for _ in range(NT)]
        cmat = P.sb("cmat", [128, NT], F32); r_cmat = R()
        nU = P.sb("nU", [128, NT], F32); r_nU = R()
        biasm = [P.sb(f"biasm{i}", [128, NT], F32) for i in range(2)]; r_bias = [R(), R()]
        two = lambda nm, shp, dt: ([P.sb(f"{nm}{i}", shp, dt) for i in range(2)], [R(), R()])
        qn, r_qn = two("qn", [128, 128], BF16)
        kn, r_kn = two("kn", [128, 128], BF16)
        qT, r_qT = two("qT", [128, 128], BF16)
        mqb, r_mqb = two("mqb", [128, 128], BF16)
        kab, r_kab = two("kab", [128, 128], BF16)
        mqT, r_mqT = two("mqT", [128, 128], BF16)
        kaT, r_kaT = two("kaT", [128, 128], BF16)
        vm, r_vm = two("vm", [128, 258], BF16)
        sg, r_sg = two("sg", [128, 128], F32)
        SmT, r_SmT = two("SmT", [128, 128], BF16)
        outt, r_outt = two("outt", [128, 256], BF16)
        NPT = 3
        PT = [P.sb(f"PT{i}", [128, 128], BF16) for i in range(NPT)]; r_PT = [R() for _ in range(NPT)]
        junk = P.sb("junk", [128, 256], F32); r_junk = R()
        junk2 = P.sb("junk2", [128, 256], F32); r_junk2 = R()
        sm, r_sm = two("sm", [128, 32], F32)
        Cst = P.sb("Cst", [128, 257], F32); r_C = R()
        Ctmp = P.sb("Ctmp", [128, 257], F32); r_Ctmp = R()
        Cb = P.sb("Cb", [128, 258], BF16); r_Cb = R()
        h_m = P.sb("h_m", [128, 256], F32); r_hm = R()
        hfu = P.sb("hfu", [128, 128], F32); r_hfu = R()
        otmp = P.sb("otmp", [128, 128], F32); r_otmp = R()

        P.op("sp", lambda: nc.sync.dma_start(out=v16[:], in_=v16d), writes=[r_v16], dma=r_v16)
        P.op("sp", lambda: nc.sync.dma_start(out=scal[:], in_=scald), writes=[r_scal], dma=r_scal)
        P.op("sp", lambda: nc.sync.dma_start(out=gv[:], in_=gvecd), writes=[r_gv], dma=r_gv)
        P.op("sp", lambda: nc.sync.dma_start(out=c32[:], in_=cf32d), writes=[r_c32], dma=r_c32)
        P.op("sp", lambda: nc.sync.dma_start(out=cb[:], in_=cbfd), writes=[r_cb], dma=r_cb)
        P.op("dve", lambda: V.tensor_scalar(out=gs[:], in0=v16[:, 16:32], scalar1=1.0, scalar2=None, op0=ALU.add),
             reads=[r_v16], writes=[r_gs])
        P.op("dve", lambda: V.tensor_tensor(out=gs[:], in0=gs[:], in1=v16[:, 0:16], op=ALU.mult),
             reads=[r_v16, r_gs], writes=[r_gs])
        P.op("dve", lambda: V.tensor_scalar(out=gv[:, 0:128], in0=gv[:, 0:128], scalar1=128.0 ** -0.5, scalar2=None,
                                            op0=ALU.mult), reads=[r_gv], writes=[r_gv])
        P.op("pool", lambda: G.memset(vaug[:], 1.0), writes=r_v)
        for i in range(2):
            P.op("pool", lambda i=i: G.memset(vm[i][:], 1.0), writes=[r_vm[i]])
        P.op("pool", lambda: G.memset(Cst[:], 0.0), writes=[r_C])
        P.op("pool", lambda: G.memset(Cb[:], 0.0), writes=[r_Cb])
        P.op("pool", lambda: G.memset(nU[:], 0.0), writes=[r_nU])
        groups = [(0, 512, 0), (512, 1024, 1), (1024, NC1, 2)]
        for kc in range(KC):
            b = kc % 2
            P.op("sp", lambda kc=kc, b=b: nc.sync.dma_start(out=wst[b][:], in_=w1[kc * 128:(kc + 1) * 128, :]),
                 writes=[r_wst[b]], dma=r_wst[b])
            for (c0, c1, bk) in groups:
                P.op("pe", lambda kc=kc, b=b, c0=c0, c1=c1, bk=bk: T.matmul(
                    bank[bk][0:1, 0:c1 - c0], lhsT=v16[:, 32 + kc:33 + kc], rhs=wst[b][:, c0:c1],
                    start=(kc == 0), stop=(kc == KC - 1)), reads=[r_v16, r_wst[b]], writes=[rb[bk]])
            P.op("dve", lambda kc=kc, b=b: V.tensor_scalar(out=Wb[:, kc, 0:NC1], in0=wst[b][:], scalar1=gs[:, kc:kc + 1],
                                                          scalar2=None, op0=ALU.mult),
                 reads=[r_wst[b], r_gs], writes=[r_Wb])
        for (c0, c1, bk) in groups:
            P.op("dve", lambda c0=c0, c1=c1, bk=bk: V.tensor_copy(out=shWrow[:, c0:c1], in_=bank[bk][0:1, 0:c1 - c0]),
                 reads=[], writes=[rb[bk], r_shWrow])
        P.op("dve", lambda: V.tensor_tensor(out=shWrow[:, 1024:1027], in0=shWrow[:, 1024:1027], in1=scal[:, 0:3], op=ALU.add),
             reads=[r_scal, r_shWrow], writes=[r_shWrow])
        for (c0, c1, bk) in groups:
            P.op("pe", lambda c0=c0, c1=c1, bk=bk: T.matmul(bank[bk][:, 0:c1 - c0], lhsT=c32[0:1, 128:256],
                                                          rhs=shWrow[0:1, c0:c1], start=True, stop=True),
                 reads=[r_c32, r_shWrow], writes=[rb[bk]])
            P.op("dve", lambda c0=c0, c1=c1, bk=bk: V.tensor_copy(out=shWb[:, c0:c1], in_=bank[bk][:, 0:c1 - c0]),
                 writes=[rb[bk], r_shWb])

        xv = xT.rearrange("(kc p) t -> p kc t", p=128)
        trp = bank[2][:, 256:512].bitcast(BF16)
        psS = bank[2]
        final = []
        ptk = 0
        for i in range(NT):
            p = i % 2
            gb = (i // 2) % 2
            if i % 2 == 0:
                gi = i // 2
                P.op("sp", lambda gi=gi, gb=gb: nc.sync.dma_start(out=xst[gb][:], in_=xv[:, :, gi * XG:(gi + 1) * XG]),
                     writes=[r_xst[gb]], dma=r_xst[gb])
                P.op("act", lambda gb=gb: S.copy(out=xb[gb][:], in_=xst[gb][:]), reads=[r_xst[gb]], writes=[r_xb[gb]])
                P.op("pool", lambda gb=gb: G.tensor_tensor(out=sq[gb][:], in0=xst[gb][:], in1=xst[gb][:], op=ALU.mult),
                     reads=[r_xst[gb]], writes=[r_sq[gb]])
            ts = slice((i % 2) * 128, (i % 2) * 128 + 128)
            smt, r_smt = sm[p], r_sm[p]
            for kc in range(KC):
                P.op("pe", lambda gb=gb, kc=kc, ts=ts: T.matmul(psS[:, 8:9], lhsT=sq[gb][:, kc, ts], rhs=cb[:, 256:257],
                                                              start=(kc == 0), stop=(kc == KC - 1)),
                     reads=[r_sq[gb], r_cb], writes=[rb[2]])
            P.op("act", lambda smt=smt: S.activation(out=smt[:, 0:1], in_=psS[:, 8:9], func=AF.Ln, bias=EPS, scale=1.0 / D),
                 writes=[rb[2], r_smt])
            P.op("act", lambda smt=smt: S.activation(out=smt[:, 1:2], in_=smt[:, 0:1], func=AF.Exp, scale=-0.5),
                 reads=[r_smt], writes=[r_smt])
            for (c0, c1, bk) in groups:
                for kc in range(KC):
                    P.op("pe", lambda gb=gb, kc=kc, ts=ts, c0=c0, c1=c1, bk=bk: T.matmul(
                        bank[bk][:, 0:c1 - c0], lhsT=xb[gb][:, kc, ts], rhs=Wb[:, kc, c0:c1],
                        start=(kc == 0), stop=(kc == KC - 1)), reads=[r_xb[gb], r_Wb], writes=[rb[bk]])
            for (c0, c1, bk) in groups:
                P.op("dve", lambda p=p, smt=smt, c0=c0, c1=c1, bk=bk: V.scalar_tensor_tensor(
                    out=pj[p][:, c0:c1], in0=bank[bk][:, 0:c1 - c0], scalar=smt[:, 1:2], in1=shWb[:, c0:c1],
                    op0=ALU.mult, op1=ALU.add), reads=[r_smt, r_shWb], writes=[rb[bk], r_pj[p]])
            pjt, r_pjt = pj[p], r_pj[p]
            P.op("act", lambda pjt=pjt, smt=smt: S.activation(out=smt[:, 2:4], in_=pjt[:, 1024:1026], func=AF.Exp, scale=2.0 / 15.0),
                 reads=[r_pjt], writes=[r_smt])
            P.op("dve", lambda smt=smt: V.tensor_scalar(out=smt[:, 2:4], in0=smt[:, 2:4], scalar1=1.0, scalar2=None, op0=ALU.add),
                 reads=[r_smt], writes=[r_smt])
            P.op("dve", lambda smt=smt: V.reciprocal(out=smt[:, 2:4], in_=smt[:, 2:4]), reads=[r_smt], writes=[r_smt])
            P.op("dve", lambda smt=smt: V.tensor_scalar(out=smt[:, 4:6], in0=smt[:, 2:4], scalar1=-30.0, scalar2=15.0,
                                                       op0=ALU.mult, op1=ALU.add), reads=[r_smt], writes=[r_smt])
            P.op("dve", lambda smt=smt, pjt=pjt: V.tensor_copy(out=smt[:, 6:7], in_=pjt[:, 1026:1027]), reads=[r_pjt, r_smt], writes=[r_smt])
            P.op("act", lambda smt=smt: S.activation(out=smt[:, 8:10], in_=smt[:, 5:7], func=AF.Exp, scale=-1.0),
                 reads=[r_smt], writes=[r_smt])
            P.op("act", lambda smt=smt: S.activation(out=smt[:, 8:10], in_=smt[:, 8:10], func=AF.Ln, bias=1.0, scale=1.0),
                 reads=[r_smt], writes=[r_smt])
            P.op("pe", lambda smt=smt: T.matmul(psS[:, 10:12], lhsT=UT32, rhs=smt[:, 8:10], start=True, stop=True),
                 reads=[r_c32, r_smt], writes=[rb[2]])
            P.op("pe", lambda smt=smt: T.matmul(psS[:, 12:14], lhsT=ONE32, rhs=smt[:, 8:10], start=True, stop=True),
                 reads=[r_c32, r_smt], writes=[rb[2]])
            P.op("dve", lambda smt=smt: V.tensor_copy(out=smt[:, 10:14], in_=psS[:, 10:14]), reads=[r_smt], writes=[rb[2], r_smt])
            P.op("dve", lambda smt=smt: V.tensor_tensor(out=smt[:, 14:15], in0=smt[:, 4:5], in1=smt[:, 10:11], op=ALU.add),
                 reads=[r_smt], writes=[r_smt])
            P.op("act", lambda smt=smt: S.activation(out=smt[:, 15:16], in_=smt[:, 14:15], func=AF.Exp), reads=[r_smt], writes=[r_smt])
            P.op("act", lambda smt=smt: S.activation(out=smt[:, 16:17], in_=smt[:, 10:11], func=AF.Exp, scale=-1.0), reads=[r_smt], writes=[r_smt])
            P.op("act", lambda smt=smt: S.activation(out=smt[:, 17:18], in_=smt[:, 12:13], func=AF.Exp, scale=-1.0), reads=[r_smt], writes=[r_smt])
            A_, B_, DEC_ = smt[:, 15:16], smt[:, 16:17], smt[:, 17:18]
            P.op("dve", lambda smt=smt, i=i: V.tensor_copy(out=cmat[:, i:i + 1], in_=smt[:, 11:12]), reads=[r_smt], writes=[r_cmat])
            P.op("dve", lambda smt=smt, i=i: V.tensor_scalar(out=nU[:, 0:i + 1], in0=nU[:, 0:i + 1], scalar1=smt[:, 13:14], scalar2=None,
                                                            op0=ALU.add), reads=[r_smt, r_nU], writes=[r_nU])
            P.op("dve", lambda p=p, i=i: V.tensor_tensor(out=biasm[p][:, 0:i + 1], in0=cmat[:, 0:i + 1], in1=nU[:, 0:i + 1], op=ALU.subtract),
                 reads=[r_cmat, r_nU], writes=[r_bias[p]])
            P.op("pool", lambda pjt=pjt: G.tensor_tensor(out=junk[:], in0=pjt[:, 0:256], in1=pjt[:, 0:256], op=ALU.mult),
                 reads=[r_pjt], writes=[r_junk])
            P.op("dve", lambda smt=smt: V.tensor_reduce(out=smt[:, 18:20], in_=junk[:].rearrange("p (a b) -> p a b", a=2), axis=AX.X, op=ALU.add),
                 reads=[r_junk, r_smt], writes=[r_smt])
            P.op("act", lambda smt=smt: S.activation(out=smt[:, 18:20], in_=smt[:, 18:20], func=AF.Ln, bias=EPS, scale=1.0 / 128), reads=[r_smt], writes=[r_smt])
            P.op("act", lambda smt=smt: S.activation(out=smt[:, 20:22], in_=smt[:, 18:20], func=AF.Exp, scale=-0.5), reads=[r_smt], writes=[r_smt])
            P.op("dve", lambda p=p, pjt=pjt, smt=smt: V.scalar_tensor_tensor(out=qn[p][:], in0=pjt[:, 0:128], scalar=smt[:, 20:21], in1=gv[:, 0:128],
                                                                          op0=ALU.mult, op1=ALU.mult), reads=[r_pjt, r_smt, r_gv], writes=[r_qn[p]])
            P.op("dve", lambda p=p, pjt=pjt, smt=smt: V.scalar_tensor_tensor(out=kn[p][:], in0=pjt[:, 128:256], scalar=smt[:, 21:22], in1=gv[:, 128:256],
                                                                          op0=ALU.mult, op1=ALU.mult), reads=[r_pjt, r_smt, r_gv], writes=[r_kn[p]])
            P.op("pool", lambda p=p, pjt=pjt: G.tensor_scalar(out=mqb[p][:], in0=pjt[:, 384:512], scalar1=128.0 ** -0.5, scalar2=None, op0=ALU.mult),
                 reads=[r_pjt], writes=[r_mqb[p]])
            P.op("dve", lambda p=p, pjt=pjt, A_=A_: V.tensor_scalar(out=kab[p][:], in0=pjt[:, 512:640], scalar1=A_, scalar2=None, op0=ALU.mult),
                 reads=[r_pjt, r_smt], writes=[r_kab[p]])
            P.op("pool", lambda p=p, pjt=pjt: G.tensor_copy(out=vm[p][:, 0:256], in_=pjt[:, 640:896]), reads=[r_pjt], writes=[r_vm[p]])
            P.op("pool", lambda i=i, pjt=pjt: G.tensor_copy(out=vaug[:, i, 0:128], in_=pjt[:, 256:384]), reads=[r_pjt], writes=[r_v[i]])
            P.op("act", lambda p=p, pjt=pjt: S.activation(out=sg[p][:], in_=pjt[:, 896:1024], func=AF.Exp, scale=-1.0), reads=[r_pjt], writes=[r_sg[p]])
            P.op("pool", lambda p=p: G.tensor_scalar(out=sg[p][:], in0=sg[p][:], scalar1=1.0, scalar2=None, op0=ALU.add), reads=[r_sg[p]], writes=[r_sg[p]])
            P.op("dve", lambda p=p: V.reciprocal(out=sg[p][:], in_=sg[p][:]), reads=[r_sg[p]], writes=[r_sg[p]])
            for k, (src, r_src) in enumerate([(qn[p], r_qn[p]), (kn[p], r_kn[p]), (mqb[p], r_mqb[p]), (kab[p], r_kab[p])]):
                P.op("pe", lambda k=k, src=src: T.transpose(out=trp[:, k * 128:(k + 1) * 128], in_=src[:], identity=IDB),
                     reads=[r_src, r_cb], writes=[rb[2]])
            P.op("act", lambda p=p: S.copy(out=qT[p][:], in_=trp[:, 0:128]), writes=[rb[2], r_qT[p]])
            P.op("act", lambda i=i: S.copy(out=kT[:, i * 128:(i + 1) * 128], in_=trp[:, 128:256]), writes=[rb[2], r_kT[i]])
            P.op("dve", lambda p=p: V.tensor_copy(out=mqT[p][:], in_=trp[:, 256:384]), writes=[rb[2], r_mqT[p]])
            P.op("dve", lambda p=p: V.tensor_copy(out=kaT[p][:], in_=trp[:, 384:512]), writes=[rb[2], r_kaT[p]])
            P.op("pe", lambda p=p: T.matmul(bank[3][:, 0:257], lhsT=kab[p][:], rhs=vm[p][:, 0:257], start=True, stop=True),
                 reads=[r_kab[p], r_vm[p]], writes=[rb[3]])
            P.op("pe", lambda p=p: T.matmul(bank[4][:, 0:128], lhsT=kaT[p][:], rhs=mqT[p][:], start=True, stop=True),
                 reads=[r_kaT[p], r_mqT[p]], writes=[rb[4]])
            P.op("dve", lambda p=p: V.tensor_tensor(out=SmT[p][:], in0=bank[4][:, 0:128], in1=UT32, op=ALU.mult),
                 reads=[r_c32], writes=[rb[4], r_SmT[p]])
            P.op("pe", lambda p=p: T.matmul(bank[4][:, 128:385], lhsT=mqT[p][:], rhs=Cb[:, 0:257], start=True, stop=False),
                 reads=[r_mqT[p], r_Cb], writes=[rb[4]])
            P.op("pe", lambda p=p: T.matmul(bank[4][:, 128:385], lhsT=SmT[p][:], rhs=vm[p][:, 0:257], start=False, stop=True),
                 reads=[r_SmT[p], r_vm[p]], writes=[rb[4]])
            P.op("dve", lambda: V.tensor_tensor(out=Ctmp[:], in0=bank[3][:, 0:257], in1=Cst[:], op=ALU.add),
                 reads=[r_C], writes=[rb[3], r_Ctmp])
            P.op("dve", lambda DEC_=DEC_: V.tensor_scalar(out=Cst[:], in0=Ctmp[:], scalar1=DEC_, scalar2=None, op0=ALU.mult),
                 reads=[r_Ctmp, r_smt], writes=[r_C])
            P.op("pool", lambda: G.tensor_copy(out=Cb[:, 0:257], in_=Cst[:]), reads=[r_C], writes=[r_Cb])
            P.op("dve", lambda smt=smt, B_=B_: V.tensor_scalar(out=smt[:, 22:23], in0=bank[4][:, 384:385], scalar1=B_, scalar2=None,
                                                             op0=ALU.mult), reads=[r_smt], writes=[rb[4], r_smt])
            P.op("dve", lambda smt=smt: V.tensor_scalar(out=smt[:, 30:31], in0=smt[:, 22:23], scalar1=-1.0, scalar2=1.0,
                                                       op0=ALU.mult, op1=ALU.max), reads=[r_smt], writes=[r_smt])
            P.op("dve", lambda smt=smt: V.tensor_tensor(out=smt[:, 22:23], in0=smt[:, 22:23], in1=smt[:, 30:31], op=ALU.max),
                 reads=[r_smt], writes=[r_smt])
            P.op("dve", lambda smt=smt: V.reciprocal(out=smt[:, 23:24], in_=smt[:, 22:23]), reads=[r_smt], writes=[r_smt])
            P.op("dve", lambda smt=smt, B_=B_: V.tensor_tensor(out=smt[:, 24:25], in0=smt[:, 23:24], in1=B_, op=ALU.mult), reads=[r_smt], writes=[r_smt])
            P.op("dve", lambda smt=smt: V.tensor_scalar(out=h_m[:], in0=bank[4][:, 128:384], scalar1=smt[:, 24:25], scalar2=None, op0=ALU.mult),
                 reads=[r_smt], writes=[rb[4], r_hm])
            P.op("pool", lambda: G.tensor_tensor(out=junk2[:], in0=h_m[:], in1=h_m[:], op=ALU.mult), reads=[r_hm], writes=[r_junk2])
            P.op("dve", lambda smt=smt: V.tensor_reduce(out=smt[:, 25:26], in_=junk2[:], axis=AX.X, op=ALU.add), reads=[r_junk2, r_smt], writes=[r_smt])
            P.op("act", lambda smt=smt: S.activation(out=smt[:, 25:26], in_=smt[:, 25:26], func=AF.Ln, bias=EPS, scale=1.0 / 256), reads=[r_smt], writes=[r_smt])
            P.op("act", lambda smt=smt: S.activation(out=smt[:, 26:27], in_=smt[:, 25:26], func=AF.Exp, scale=-0.5), reads=[r_smt], writes=[r_smt])
            P.op("dve", lambda smt=smt: V.scalar_tensor_tensor(out=otmp[:], in0=h_m[:, 0:128], scalar=smt[:, 26:27], in1=gv[:, 384:512],
                                                             op0=ALU.mult, op1=ALU.mult), reads=[r_hm, r_smt, r_gv], writes=[r_otmp])
            P.op("dve", lambda p=p: V.tensor_tensor(out=outt[p][:, 0:128], in0=otmp[:], in1=sg[p][:], op=ALU.mult),
                 reads=[r_otmp, r_sg[p]], writes=[r_outt[p]])
            def qk(sb, p=p):
                pb = 5 + (sb % 2)
                P.op("pe", lambda: T.matmul(bank[pb][:, 0:128], lhsT=kT[:, sb * 128:(sb + 1) * 128], rhs=qT[p][:], start=True, stop=True),
                     reads=[r_kT[sb], r_qT[p]], writes=[rb[pb]])
            def pv(sb, k, p=p, i=i):
                pb = 5 + (sb % 2)
                P.op("act", lambda: S.activation(out=PT[k][:], in_=bank[pb][:, 0:128], func=AF.Exp, bias=biasm[p][:, sb:sb + 1], scale=1.0),
                     reads=[r_bias[p]], writes=[rb[pb], r_PT[k]])
                if sb == i:
                    P.op("pool", lambda: G.tensor_tensor(out=PT[k][:], in0=PT[k][:], in1=UTB, op=ALU.mult), reads=[r_cb, r_PT[k]], writes=[r_PT[k]])
                P.op("pe", lambda: T.matmul(bank[7][:, 0:129], lhsT=PT[k][:], rhs=vaug[:, sb, 0:129], start=(sb == 0), stop=(sb == i)),
                     reads=[r_PT[k], r_v[sb]], writes=[rb[7]])
            qk(0)
            for sb in range(i + 1):
                if sb + 1 <= i:
                    qk(sb + 1)
                pv(sb, ptk % NPT)
                ptk += 1
            P.op("dve", lambda smt=smt: V.reciprocal(out=smt[:, 27:28], in_=bank[7][:, 128:129]), reads=[r_smt], writes=[rb[7], r_smt])
            P.op("dve", lambda smt=smt: V.tensor_scalar(out=hfu[:], in0=bank[7][:, 0:128], scalar1=smt[:, 27:28], scalar2=None, op0=ALU.mult),
                 reads=[r_smt], writes=[rb[7], r_hfu])
            P.op("pool", lambda: G.tensor_tensor(out=junk2[:, 0:128], in0=hfu[:], in1=hfu[:], op=ALU.mult), reads=[r_hfu], writes=[r_junk2])
            P.op("dve", lambda smt=smt: V.tensor_reduce(out=smt[:, 28:29], in_=junk2[:, 0:128], axis=AX.X, op=ALU.add), reads=[r_junk2, r_smt], writes=[r_smt])
            P.op("act", lambda smt=smt: S.activation(out=smt[:, 28:29], in_=smt[:, 28:29], func=AF.Ln, bias=EPS, scale=1.0 / 128), reads=[r_smt], writes=[r_smt])
            P.op("act", lambda smt=smt: S.activation(out=smt[:, 29:30], in_=smt[:, 28:29], func=AF.Exp, scale=-0.5), reads=[r_smt], writes=[r_smt])
            P.op("dve", lambda p=p, smt=smt: V.scalar_tensor_tensor(out=outt[p][:, 128:256], in0=hfu[:], scalar=smt[:, 29:30], in1=gv[:, 256:384],
                                                                  op0=ALU.mult, op1=ALU.mult), reads=[r_hfu, r_smt, r_gv], writes=[r_outt[p]])
            f = P.op("sp", lambda p=p, i=i: nc.sync.dma_start(out=hout[i * 128:(i + 1) * 128, :], in_=outt[p][:]), reads=[r_outt[p]], dma=r_outt[p])
            final.append(f)
        P.emit(final_ops=final[-2:])
    return nc


W_OFF = dict(mq=0, mk=512, mv=1024, mo=2048, mi=3072, mf=3076, fq=3080, fk=4104, fv=5128, ff=6152)


def l1_inputs(j, xT, w_in, norm_mix, sc1, sh1, b_i, b_f, fox_b_f, gq, gk, gfo, gmo):
    h, half = j // 2, j % 2
    o = W_OFF
    cols = np.concatenate([
        np.arange(o["fq"] + 128 * j, o["fq"] + 128 * j + 128), np.arange(o["fk"] + 128 * j, o["fk"] + 128 * j + 128),
        np.arange(o["fv"] + 128 * j, o["fv"] + 128 * j + 128), np.arange(o["mq"] + 128 * h, o["mq"] + 128 * h + 128),
        np.arange(o["mk"] + 128 * h, o["mk"] + 128 * h + 128),
        np.arange(o["mv"] + 256 * h + 128 * half, o["mv"] + 256 * h + 128 * half + 128),
        np.arange(o["mv"] + 256 * h + 128 * (1 - half), o["mv"] + 256 * h + 128 * (1 - half) + 128),
        np.arange(o["mo"] + 256 * h + 128 * half, o["mo"] + 256 * h + 128 * half + 128),
        [o["mi"] + h, o["mf"] + h, o["ff"] + j]]).astype(np.int64)
    lay = lambda v: v.reshape(KC, 128).T
    v16 = np.ascontiguousarray(np.concatenate([lay(norm_mix), lay(sc1), lay(sh1)], axis=1), dtype=np.float32)
    scal = np.array([[b_i[h], b_f[h], fox_b_f[j]]], np.float32)
    bc = lambda v: np.broadcast_to(v[None, :], (128, v.shape[0]))
    gvec = np.ascontiguousarray(np.concatenate([bc(gq), bc(gk), bc(gfo[j]), bc(gmo[h, 128 * half:128 * half + 128])], axis=1), dtype=np.float32)
    ut = np.triu(np.ones((128, 128), np.float32))
    cf32 = np.ascontiguousarray(np.concatenate([ut, np.ones((128, 128), np.float32)], axis=1))
    cbf = np.ascontiguousarray(np.concatenate([np.eye(128, dtype=np.float32), ut, np.ones((128, 128), np.float32)], axis=1)).astype(ml_dtypes.bfloat16)
    return {"xT": xT, "w1": np.ascontiguousarray(w_in[:, cols]), "v16": v16, "scal": scal, "gvec": gvec, "cf32": cf32, "cbf": cbf}


NE = 32


def rank_ops(P, nc, bankr, rbr, maskb, r_maskb, i, cb, r_cb, mask32, r_mask, rk_out, r_rk, lo=32):
    V, T = nc.vector, nc.tensor
    for ii in range(i + 1):
        lhs = cb[:, 256:384] if ii < i else cb[:, 128:256]
        P.op("pe", lambda ii=ii, lhs=lhs: T.matmul(bankr[:, lo:lo + NE], lhsT=lhs, rhs=maskb[:, ii, :], start=(ii == 0), stop=(ii == i)),
             reads=[r_cb, r_maskb], writes=[rbr])
    P.op("dve", lambda: V.scalar_tensor_tensor(out=rk_out, in0=bankr[:, lo:lo + NE], scalar=1.0, in1=mask32, op0=ALU.add, op1=ALU.mult),
         reads=[r_mask], writes=[rbr, r_rk])
    P.op("dve", lambda: V.tensor_scalar(out=rk_out, in0=rk_out, scalar1=-1.0, scalar2=None, op0=ALU.add), reads=[r_rk], writes=[r_rk])


def build_l2(TL, CAP):
    NTL = TL // 128
    nc = new_nc()
    mixT = nc.dram_tensor("mixT", [D, TL], BF16, kind="ExternalInput").ap()
    xd = nc.dram_tensor("x", [TL, D], F32, kind="ExternalInput").ap()
    wod = nc.dram_tensor("w_out", [D, D], F32, kind="ExternalInput").ap()
    bcd = nc.dram_tensor("bc", [128, 3 * D + NE], F32, kind="ExternalInput").ap()
    sh2d = nc.dram_tensor("sh2", [128, D], F32, kind="ExternalInput").ap()
    wrd = nc.dram_tensor("w_r", [D, NE], F32, kind="ExternalInput").ap()
    cf32d = nc.dram_tensor("cf32", [128, 128 + 640], F32, kind="ExternalInput").ap()
    cbfd = nc.dram_tensor("cbf", [128, 384], BF16, kind="ExternalInput").ap()
    hres_o = nc.dram_tensor("hres", [TL, D], F32, kind="ExternalOutput").ap()
    G_o = nc.dram_tensor("G", [TL, NE], F32, kind="ExternalOutput").ap()
    XT_o = nc.dram_tensor("XT", [NE, D, CAP], BF16, kind="ExternalOutput").ap()
    V, S, G_, T = nc.vector, nc.scalar, nc.gpsimd, nc.tensor
    with contextlib.ExitStack() as st:
        P = Prog(nc, st)
        R = P.res
        bank = [P.ps(f"bank{i}", [128, 512]) for i in range(8)]
        rb = [R(f"bank{i}") for i in range(8)]
        wobf = P.sb("wob", [128, KC * D], BF16); r_wob = R()
        wob = wobf[:, :].rearrange("p (kc n) -> p kc n", kc=KC)
        bc = P.sb("bct", [128, 3 * D + NE], F32); r_bc = R()
        g1b, gs2b, brb = bc[:, 0:D], bc[:, D:2 * D], bc[:, 3 * D:3 * D + NE]
        sh2b = P.sb("sh2b", [128, D], F32); r_sh2 = R()
        wr = P.sb("wr", [128, KC, NE], F32); r_wr = R()
        c32 = P.sb("c32", [128, 768], F32); r_c32 = R()
        cb = P.sb("cb", [128, 384], BF16); r_cb = R()
        ID32, IOTA = c32[:, 0:128], c32[:, 128:128 + CAP]
        mx = [P.sb(f"mx{i}", [128, KC, 128], BF16) for i in range(2)]; r_mx = [R(), R()]
        _xt = P.sb("xt0", [128, D], F32); _rxt = R(); xt = [_xt, _xt]; r_xt = [_rxt, _rxt]
        hres = [P.sb(f"hres{i}", [128, D], F32) for i in range(2)]; r_hres = [R(), R()]
        h2f = P.sb("h2f", [128, D], F32); r_h2f = R()
        h2b = P.sb("h2b", [128, NTL, D], BF16); r_h2b = [R() for _ in range(NTL)]
        h2T = P.sb("h2T", [128, KC, 128], F32); r_h2T = R()
        maskb = P.sb("maskb", [128, NTL, NE], BF16); r_maskb = R()
        rk = P.sb("rk", [128, NTL, NE], F32); r_rk = R()
        sm = [P.sb(f"sm{i}", [128, 16], F32) for i in range(2)]; r_sm = [R(), R()]
        lg = [P.sb(f"lg{i}", [128, 4 * NE], F32) for i in range(2)]; r_lg = [R(), R()]
        assert 2 * NTL * CAP + 2 * KC * CAP <= KC * D
        sel = [wobf[:, q * NTL * CAP:(q + 1) * NTL * CAP].rearrange("p (i s) -> p i s", i=NTL) for q in range(2)]; r_sel = [R(), R()]
        xb0 = 2 * NTL * CAP
        xte = [wobf[:, xb0 + q * KC * CAP:xb0 + (q + 1) * KC * CAP].rearrange("p (fc s) -> p fc s", fc=KC) for q in range(2)]; r_xte = [R(), R()]

        r_wobk = [R() for _ in range(KC)]
        for kc in range(KC):
            P.op("pool", lambda kc=kc: G_.dma_start(out=wob[:, kc, :], in_=wod[kc * 128:(kc + 1) * 128, :]), writes=[r_wobk[kc]], dma=r_wobk[kc])
        P.op("sp", lambda: nc.sync.dma_start(out=bc[:], in_=bcd), writes=[r_bc], dma=r_bc)
        P.op("sp", lambda: nc.sync.dma_start(out=sh2b[:], in_=sh2d), writes=[r_sh2], dma=r_sh2)
        P.op("sp", lambda: nc.sync.dma_start(out=wr[:], in_=wrd.rearrange("(kc p) n -> p kc n", p=128)), writes=[r_wr], dma=r_wr)
        P.op("sp", lambda: nc.sync.dma_start(out=c32[:], in_=cf32d), writes=[r_c32], dma=r_c32)
        P.op("sp", lambda: nc.sync.dma_start(out=cb[:], in_=cbfd), writes=[r_cb], dma=r_cb)
        P.op("dve", lambda: V.scalar_tensor_tensor(out=bc[:, D:2 * D], in0=bc[:, 2 * D:3 * D], scalar=1.0, in1=bc[:, D:2 * D], op0=ALU.add, op1=ALU.mult),
             reads=[r_bc], writes=[r_bc])
        mv = mixT.rearrange("(kc p) t -> p kc t", p=128)
        finals = []
        for i in range(NTL):
            p = i % 2
            tsl = slice(i * 128, (i + 1) * 128)
            smt, r_smt = sm[p], r_sm[p]
            lgt, r_lgt = lg[p], r_lg[p]
            P.op("sp", lambda p=p, tsl=tsl: nc.sync.dma_start(out=mx[p][:], in_=mv[:, :, tsl]), writes=[r_mx[p]], dma=r_mx[p])
            P.op("sp", lambda p=p, tsl=tsl: nc.sync.dma_start(out=xt[p][:], in_=xd[tsl, :]), writes=[r_xt[p]], dma=r_xt[p])
            for dg in range(4):
                for kc in range(KC):
                    P.op("pe", lambda p=p, dg=dg, kc=kc: T.matmul(bank[dg][:, :], lhsT=mx[p][:, kc, :], rhs=wob[:, kc, dg * 512:(dg + 1) * 512],
                                                                 start=(kc == 0), stop=(kc == KC - 1)), reads=[r_mx[p], r_wobk[kc]], writes=[rb[dg]])
            for dg in range(4):
                ds = slice(dg * 512, (dg + 1) * 512)
                P.op("dve", lambda p=p, dg=dg, ds=ds: V.tensor_tensor(out=hres[p][:, ds], in0=bank[dg][:, :], in1=g1b[:, ds], op=ALU.mult),
                     reads=[r_bc], writes=[rb[dg], r_hres[p]])
                P.op("pool", lambda p=p, ds=ds: G_.tensor_tensor(out=hres[p][:, ds], in0=hres[p][:, ds], in1=xt[p][:, ds], op=ALU.add),
                     reads=[r_xt[p], r_hres[p]], writes=[r_hres[p]])
            f = P.op("sp", lambda p=p, tsl=tsl: nc.sync.dma_start(out=hres_o[tsl, :], in_=hres[p][:]), reads=[r_hres[p]], dma=r_hres[p])
            finals.append(f)
            P.op("pool", lambda p=p: G_.tensor_tensor(out=h2f[:], in0=hres[p][:], in1=hres[p][:], op=ALU.mult), reads=[r_hres[p]], writes=[r_h2f])
            P.op("dve", lambda smt=smt: V.tensor_reduce(out=smt[:, 0:1], in_=h2f[:], axis=AX.X, op=ALU.add), reads=[r_h2f], writes=[r_smt])
            P.op("act", lambda smt=smt: S.activation(out=smt[:, 1:2], in_=smt[:, 0:1], func=AF.Ln, bias=EPS, scale=1.0 / D), reads=[r_smt], writes=[r_smt])
            P.op("act", lambda smt=smt: S.activation(out=smt[:, 2:3], in_=smt[:, 1:2], func=AF.Exp, scale=-0.5), reads=[r_smt], writes=[r_smt])
            P.op("dve", lambda p=p, smt=smt: V.scalar_tensor_tensor(out=h2f[:], in0=hres[p][:], scalar=smt[:, 2:3], in1=gs2b, op0=ALU.mult, op1=ALU.mult),
                 reads=[r_hres[p], r_smt, r_bc, r_h2f], writes=[r_h2f])
            P.op("pool", lambda: G_.tensor_tensor(out=h2f[:], in0=h2f[:], in1=sh2b[:], op=ALU.add), reads=[r_sh2, r_h2f], writes=[r_h2f])
            P.op("act", lambda i=i: S.copy(out=h2b[:, i, :], in_=h2f[:]), reads=[r_h2f], writes=[r_h2b[i]])
            for q4 in range(4):
                tb = 4 + (q4 % 2)
                for k4 in range(4):
                    kc = q4 * 4 + k4
                    P.op("pe", lambda tb=tb, k4=k4, kc=kc: T.transpose(out=bank[tb][:, k4 * 128:(k4 + 1) * 128], in_=h2f[:, kc * 128:(kc + 1) * 128], identity=ID32),
                         reads=[r_h2f, r_c32], writes=[rb[tb]])
                eng = "act" if q4 % 2 == 0 else "dve"
                if eng == "act":
                    P.op("act", lambda tb=tb, q4=q4: S.copy(out=h2T[:, q4 * 4:(q4 + 1) * 4, :], in_=bank[tb][:, :].rearrange("p (a b) -> p a b", a=4)),
                         writes=[rb[tb], r_h2T])
                else:
                    P.op("dve", lambda tb=tb, q4=q4: V.tensor_copy(out=h2T[:, q4 * 4:(q4 + 1) * 4, :], in_=bank[tb][:, :].rearrange("p (a b) -> p a b", a=4)),
                         writes=[rb[tb], r_h2T])
            for kc in range(KC):
                P.op("pe", lambda kc=kc: T.matmul(bank[6][:, 0:NE], lhsT=h2T[:, kc, :], rhs=wr[:, kc, :], start=(kc == 0), stop=(kc == KC - 1)),
                     reads=[r_h2T, r_wr], writes=[rb[6]])
            P.op("dve", lambda lgt=lgt: V.tensor_tensor(out=lgt[:, 0:NE], in0=bank[6][:, 0:NE], in1=brb, op=ALU.add), reads=[r_bc], writes=[rb[6], r_lgt])
            P.op("dve", lambda lgt=lgt, smt=smt: V.max(out=smt[:, 8:16], in_=lgt[:, 0:NE]), reads=[r_lgt], writes=[r_smt])
            P.op("dve", lambda lgt=lgt, smt=smt: V.tensor_scalar(out=lgt[:, NE:2 * NE], in0=lgt[:, 0:NE], scalar1=smt[:, 11:12], scalar2=None, op0=ALU.is_ge),
                 reads=[r_lgt, r_smt], writes=[r_lgt])
            P.op("dve", lambda smt=smt: V.tensor_scalar(out=smt[:, 3:4], in0=smt[:, 8:9], scalar1=-1.0, scalar2=None, op0=ALU.mult), reads=[r_smt], writes=[r_smt])
            P.op("act", lambda lgt=lgt, smt=smt: S.activation(out=lgt[:, 2 * NE:3 * NE], in_=lgt[:, 0:NE], func=AF.Exp, bias=smt[:, 3:4], scale=1.0),
                 reads=[r_lgt, r_smt], writes=[r_lgt])
            P.op("dve", lambda lgt=lgt: V.tensor_tensor(out=lgt[:, 2 * NE:3 * NE], in0=lgt[:, 2 * NE:3 * NE], in1=lgt[:, NE:2 * NE], op=ALU.mult), reads=[r_lgt], writes=[r_lgt])
            P.op("dve", lambda lgt=lgt, smt=smt: V.tensor_reduce(out=smt[:, 4:5], in_=lgt[:, 2 * NE:3 * NE], axis=AX.X, op=ALU.add), reads=[r_lgt], writes=[r_smt])
            P.op("dve", lambda smt=smt: V.reciprocal(out=smt[:, 5:6], in_=smt[:, 4:5]), reads=[r_smt], writes=[r_smt])
            P.op("dve", lambda lgt=lgt, smt=smt: V.tensor_scalar(out=lgt[:, 3 * NE:4 * NE], in0=lgt[:, 2 * NE:3 * NE], scalar1=smt[:, 5:6], scalar2=None, op0=ALU.mult),
                 reads=[r_lgt, r_smt], writes=[r_lgt])
            f = P.op("sp", lambda lgt=lgt, tsl=tsl: nc.sync.dma_start(out=G_o[tsl, :], in_=lgt[:, 3 * NE:4 * NE]), reads=[r_lgt], dma=r_lgt)
            finals.append(f)
            P.op("pool", lambda lgt=lgt, i=i: G_.tensor_copy(out=maskb[:, i, :], in_=lgt[:, NE:2 * NE]), reads=[r_lgt], writes=[r_maskb])
            rank_ops(P, nc, bank[6], rb[6], maskb, r_maskb, i, cb, r_cb, lgt[:, NE:2 * NE], r_lgt, rk[:, i, :], r_rk)
        gbanks = [0, 1, 2, 3, 7, 4, 5]
        gi = 0
        ncc = (CAP + 511) // 512
        ccw = CAP // ncc
        P.op("dve", lambda: V.memset(sel[0][:, 0, 0:2], 0.0), writes=r_wobk + r_sel + r_xte)
        for e in range(NE):
            q = e % 2
            for i in range(NTL):
                P.op("dve", lambda q=q, i=i, e=e: V.tensor_scalar(out=sel[q][:, i, :], in0=IOTA, scalar1=rk[:, i, e:e + 1], scalar2=None, op0=ALU.is_equal),
                     reads=[r_c32, r_rk], writes=[r_sel[q]])
            for fc in range(KC):
                for cc in range(ncc):
                    cs = slice(cc * ccw, (cc + 1) * ccw)
                    gbk = gbanks[gi % len(gbanks)]; gi += 1
                    for i in range(NTL):
                        P.op("pe", lambda gbk=gbk, q=q, i=i, fc=fc, cs=cs: T.matmul(bank[gbk][:, 0:ccw], lhsT=h2b[:, i, fc * 128:(fc + 1) * 128], rhs=sel[q][:, i, cs],
                                                                                start=(i == 0), stop=(i == NTL - 1)), reads=[r_h2b[i], r_sel[q]], writes=[rb[gbk]])
                    if gi % 2 == 0:
                        P.op("act", lambda gbk=gbk, q=q, fc=fc, cs=cs: S.copy(out=xte[q][:, fc, cs], in_=bank[gbk][:, 0:ccw]), writes=[rb[gbk], r_xte[q]])
                    else:
                        P.op("dve", lambda gbk=gbk, q=q, fc=fc, cs=cs: V.tensor_copy(out=xte[q][:, fc, cs], in_=bank[gbk][:, 0:ccw]), writes=[rb[gbk], r_xte[q]])
            f = P.op("sp", lambda q=q, e=e: nc.sync.dma_start(out=XT_o[e].rearrange("(fc p) s -> p fc s", p=128), in_=xte[q][:]), reads=[r_xte[q]], dma=r_xte[q])
            finals.append(f)
        P.emit(final_ops=finals)
    return nc


def l2_inputs(mixT_g, x_g, w_out, g1, norm_ffn, sc2, sh2, w_router, b_router):
    bcst = lambda v: np.broadcast_to(v[None, :], (128, v.shape[0]))
    bc = np.ascontiguousarray(np.concatenate([bcst(g1), bcst(norm_ffn), bcst(sc2), bcst(b_router)], axis=1), dtype=np.float32)
    sl = np.tril(np.ones((128, 128), np.float32), -1).T
    cf32 = np.ascontiguousarray(np.concatenate([np.eye(128, dtype=np.float32), bcst(np.arange(640, dtype=np.float32))], axis=1))
    cbf = np.ascontiguousarray(np.concatenate([np.eye(128, dtype=np.float32), sl, np.ones((128, 128), np.float32)], axis=1)).astype(ml_dtypes.bfloat16)
    return {"mixT": mixT_g, "x": x_g, "w_out": w_out, "bc": bc, "sh2": np.ascontiguousarray(bcst(sh2), dtype=np.float32),
            "w_r": w_router, "cf32": cf32, "cbf": cbf}


EPC = 4


def build_l3(NS, NCH=1):
    nc = new_nc()
    NSC = NS // NCH
    NSG = (NSC + 511) // 512
    SGW = NSC // NSG
    NSB = NSC // 128
    xtd = nc.dram_tensor("XT", [EPC, D, NS], BF16, kind="ExternalInput").ap()
    wugd = nc.dram_tensor("w_ug", [EPC, D, 2 * D], F32, kind="ExternalInput").ap()
    bugd = nc.dram_tensor("b_ug", [EPC, 128, 32], F32, kind="ExternalInput").ap()
    wdd = nc.dram_tensor("w_d", [EPC, D, D], F32, kind="ExternalInput").ap()
    bdd = nc.dram_tensor("b_d", [EPC, 128, D], F32, kind="ExternalInput").ap()
    yd = nc.dram_tensor("y", [EPC, NS, D], BF16, kind="ExternalOutput").ap()
    cug = nc.dram_tensor("cache_ug", [32, 128, KC * 128], BF16).ap() if NCH > 1 else None
    cdn = nc.dram_tensor("cache_dn", [KC, 128, D], BF16).ap() if NCH > 1 else None
    V, S, G_, T = nc.vector, nc.scalar, nc.gpsimd, nc.tensor
    with contextlib.ExitStack() as st:
        P = Prog(nc, st)
        R = P.res
        bank = [P.ps(f"bank{i}", [128, 512]) for i in range(8)]
        rb = [R(f"bank{i}") for i in range(8)]
        xs = P.sb("xs", [128, KC, NSC], BF16); r_xs = R()
        actT = P.sb("actT", [128, KC, NSC], BF16); r_actT = [R() for _ in range(KC)]
        wd = P.sb("wd", [128, KC, D], BF16); r_wd = [R() for _ in range(KC)]
        NW = 3
        wg = [P.sb(f"wg{i}", [128, KC, 128], BF16) for i in range(NW)]; r_wg = [R() for _ in range(NW)]
        wu = [P.sb(f"wu{i}", [128, KC, 128], BF16) for i in range(NW)]; r_wu = [R() for _ in range(NW)]
        bug = P.sb("bug", [128, 32], F32); r_bug = R()
        bdb = P.sb("bdb", [128, D], F32); r_bdb = R()
        gcl = [P.sb(f"gcl{i}", [128, SGW], F32) for i in range(2)]; r_gcl = [R(), R()]
        sig = [P.sb(f"sig{i}", [128, SGW], F32) for i in range(2)]; r_sig = [R(), R()]
        ucl = [P.sb(f"ucl{i}", [128, SGW], F32) for i in range(2)]; r_ucl = [R(), R()]
        yst = [P.sb(f"yst{i}", [128, D], BF16) for i in range(2)]; r_yst = [R(), R()]
        r_cug = [R() for _ in range(32)]
        r_cdn = [R() for _ in range(KC)]
        r_wgst = [R() for _ in range(NW)]; r_wust = [R() for _ in range(NW)]; r_wdst = [R() for _ in range(KC)]
        finals = []
        wi = 0
        ei = 0
        for v in range(EPC * NCH):
            e, ch = v // NCH, v % NCH
            so = ch * NSC
            P.op("sp", lambda e=e, so=so: nc.sync.dma_start(out=xs[:], in_=xtd[e][:, so:so + NSC].rearrange("(kc p) s -> p kc s", p=128)), writes=[r_xs], dma=r_xs)
            if ch == 0:
                P.op("sp", lambda e=e: nc.sync.dma_start(out=bug[:], in_=bugd[e]), writes=[r_bug], dma=r_bug)
                P.op("sp", lambda e=e: nc.sync.dma_start(out=bdb[:], in_=bdd[e]), writes=[r_bdb], dma=r_bdb)
            wv = wugd[e].rearrange("(kc p) n -> p kc n", p=128)
            for c in range(KC):
                w = wi % NW; wi += 1
                if ch == 0:
                    P.op("pool", lambda w=w, c=c, wv=wv: G_.dma_start(out=wg[w][:], in_=wv[:, :, c * 128:(c + 1) * 128]), writes=[r_wg[w]], dma=r_wg[w])
                    P.op("pool", lambda w=w, c=c, wv=wv: G_.dma_start(out=wu[w][:], in_=wv[:, :, D + c * 128:D + (c + 1) * 128]), writes=[r_wu[w]], dma=r_wu[w])
                    P.op("pool", lambda e=e, c=c: G_.dma_start(out=wd[:, c, :], in_=wdd[e, c * 128:(c + 1) * 128, :]), writes=[r_wd[c]], dma=r_wd[c])
                    if NCH > 1:
                        P.op("act", lambda w=w, c=c: S.dma_start(out=cug[c], in_=wg[w][:].rearrange("p a b -> p (a b)")), reads=[r_wg[w]], writes=[r_cug[c]], dma=r_wgst[w])
                        P.op("act", lambda w=w, c=c: S.dma_start(out=cug[16 + c], in_=wu[w][:].rearrange("p a b -> p (a b)")), reads=[r_wu[w]], writes=[r_cug[16 + c]], dma=r_wust[w])
                        P.op("act", lambda c=c: S.dma_start(out=cdn[c], in_=wd[:, c, :]), reads=[r_wd[c]], writes=[r_cdn[c]], dma=r_wdst[c])
                else:
                    P.op("sp", lambda w=w, c=c: nc.sync.dma_start(out=wg[w][:].rearrange("p a b -> p (a b)"), in_=cug[c]), reads=[r_cug[c]], writes=[r_wg[w]], dma=r_wg[w])
                    P.op("sp", lambda w=w, c=c: nc.sync.dma_start(out=wu[w][:].rearrange("p a b -> p (a b)"), in_=cug[16 + c]), reads=[r_cug[16 + c]], writes=[r_wu[w]], dma=r_wu[w])
                    P.op("sp", lambda c=c: nc.sync.dma_start(out=wd[:, c, :], in_=cdn[c]), reads=[r_cdn[c]], writes=[r_wd[c]], dma=r_wd[c])
                for sg in range(NSG):
                    ss = slice(sg * SGW, (sg + 1) * SGW)
                    bg_, bu_ = sg * 2, sg * 2 + 1
                    for kc in range(KC):
                        P.op("pe", lambda w=w, kc=kc, ss=ss, bg_=bg_: T.matmul(bank[bg_][:, 0:SGW], lhsT=wg[w][:, kc, :], rhs=xs[:, kc, ss], start=(kc == 0), stop=(kc == KC - 1)),
                             reads=[r_wg[w], r_xs], writes=[rb[bg_]])
                    for kc in range(KC):
                        P.op("pe", lambda w=w, kc=kc, ss=ss, bu_=bu_: T.matmul(bank[bu_][:, 0:SGW], lhsT=wu[w][:, kc, :], rhs=xs[:, kc, ss], start=(kc == 0), stop=(kc == KC - 1)),
                             reads=[r_wu[w], r_xs], writes=[rb[bu_]])
                    q = ei % 2; ei += 1
                    P.op("dve", lambda q=q, c=c, bg_=bg_: V.tensor_scalar(out=gcl[q][:], in0=bank[bg_][:, 0:SGW], scalar1=bug[:, c:c + 1], scalar2=7.0, op0=ALU.add, op1=ALU.min),
                         reads=[r_bug], writes=[rb[bg_], r_gcl[q]])
                    P.op("dve", lambda q=q, c=c, bu_=bu_: V.tensor_scalar(out=ucl[q][:], in0=bank[bu_][:, 0:SGW], scalar1=bug[:, 16 + c:17 + c], scalar2=7.0, op0=ALU.add, op1=ALU.min),
                         reads=[r_bug], writes=[rb[bu_], r_ucl[q]])
                    P.op("act", lambda q=q: S.activation(out=sig[q][:], in_=gcl[q][:], func=AF.Sigmoid, scale=1.702), reads=[r_gcl[q]], writes=[r_sig[q]])
                    P.op("dve", lambda q=q: V.tensor_scalar(out=ucl[q][:], in0=ucl[q][:], scalar1=-7.0, scalar2=1.0, op0=ALU.max, op1=ALU.add), reads=[r_ucl[q]], writes=[r_ucl[q]])
                    P.op("dve", lambda q=q: V.tensor_tensor(out=gcl[q][:], in0=gcl[q][:], in1=sig[q][:], op=ALU.mult), reads=[r_gcl[q], r_sig[q]], writes=[r_gcl[q]])
                    P.op("dve", lambda q=q, c=c, ss=ss: V.tensor_tensor(out=actT[:, c, ss], in0=gcl[q][:], in1=ucl[q][:], op=ALU.mult), reads=[r_gcl[q], r_ucl[q]], writes=[r_actT[c]])
            for sb in range(NSB):
                yq = sb % 2
                for dg in range(4):
                    bk = 6 + (dg % 2)
                    for kc in range(KC):
                        P.op("pe", lambda sb=sb, dg=dg, kc=kc, bk=bk: T.matmul(bank[bk][:, :], lhsT=actT[:, kc, sb * 128:(sb + 1) * 128], rhs=wd[:, kc, dg * 512:(dg + 1) * 512],
                                                                             start=(kc == 0), stop=(kc == KC - 1)), reads=[r_actT[kc], r_wd[kc]], writes=[rb[bk]])
                    P.op("dve", lambda yq=yq, dg=dg, bk=bk: V.tensor_tensor(out=yst[yq][:, dg * 512:(dg + 1) * 512], in0=bank[bk][:, :], in1=bdb[:, dg * 512:(dg + 1) * 512], op=ALU.add),
                         reads=[r_bdb], writes=[rb[bk], r_yst[yq]])
                f = P.op("sp", lambda e=e, sb=sb, yq=yq, so=so: nc.sync.dma_start(out=yd[e, so + sb * 128:so + (sb + 1) * 128, :], in_=yst[yq][:]), reads=[r_yst[yq]], dma=r_yst[yq])
                finals.append(f)
        P.emit(final_ops=finals[-2:])
    return nc


def build_l4(TL, CAP):
    NTL = TL // 128
    nc = new_nc()
    NCK = (CAP + 127) // 128
    CW = CAP // NCK
    yd = nc.dram_tensor("yb", [NE, CAP, D], BF16, kind="ExternalInput").ap()
    Gd = nc.dram_tensor("G", [TL, NE], F32, kind="ExternalInput").ap()
    hrd = nc.dram_tensor("hres", [TL, D], F32, kind="ExternalInput").ap()
    bcd = nc.dram_tensor("bc", [128, 4 * D], F32, kind="ExternalInput").ap()
    cf32d = nc.dram_tensor("cf32", [128, 768], F32, kind="ExternalInput").ap()
    cbfd = nc.dram_tensor("cbf", [128, 384], BF16, kind="ExternalInput").ap()
    outd = nc.dram_tensor("out", [TL, D], F32, kind="ExternalOutput").ap()
    V, S, G_, T = nc.vector, nc.scalar, nc.gpsimd, nc.tensor
    with contextlib.ExitStack() as st:
        P = Prog(nc, st)
        R = P.res
        bank = [P.ps(f"bank{i}", [128, 512]) for i in range(8)]
        rb = [R(f"bank{i}") for i in range(8)]
        bc = P.sb("bct", [128, 4 * D], F32); r_bc = R()
        c32 = P.sb("c32", [128, 768], F32); r_c32 = R()
        cb = P.sb("cb", [128, 384], BF16); r_cb = R()
        IOTA = c32[:, 128:128 + CAP]
        Gt = P.sb("Gt", [128, NTL, NE], F32); r_G = R()
        mask32 = P.sb("mask32", [128, NTL, NE], F32); r_mask = R()
        maskb = P.sb("maskb", [128, NTL, NE], BF16); r_maskb = R()
        rk = P.sb("rk", [128, NTL, NE], F32); r_rk = R()
        acc = P.sb("acc", [128, NTL, D], F32); r_acc = [R() for _ in range(NTL)]
        yb = [P.sb(f"yb{i}", [128, NCK, D], BF16) for i in range(2)]; r_yb = [R(), R()]
        selw = [P.sb(f"selw{i}", [128, CAP], BF16) for i in range(2)]; r_selw = [R(), R()]
        swT = [P.sb(f"swT{i}", [128, NCK, 128], BF16) for i in range(2)]; r_swT = [R(), R()]
        hr = P.sb("hr", [128, D], F32); r_hr = R()
        tmp = P.sb("tmp", [128, D], F32); r_tmp = R()
        ot = [P.sb(f"ot{i}", [128, D], F32) for i in range(2)]; r_ot = [R(), R()]
        sm = [P.sb(f"sm{i}", [128, 8], F32) for i in range(2)]; r_sm = [R(), R()]
        P.op("sp", lambda: nc.sync.dma_start(out=bc[:], in_=bcd), writes=[r_bc], dma=r_bc)
        P.op("sp", lambda: nc.sync.dma_start(out=c32[:], in_=cf32d), writes=[r_c32], dma=r_c32)
        P.op("sp", lambda: nc.sync.dma_start(out=cb[:], in_=cbfd), writes=[r_cb], dma=r_cb)
        P.op("sp", lambda: nc.sync.dma_start(out=Gt[:], in_=Gd.rearrange("(i p) e -> p i e", p=128)), writes=[r_G], dma=r_G)
        P.op("pool", lambda: G_.memset(acc[:], 0.0), writes=r_acc)
        P.op("dve", lambda: V.scalar_tensor_tensor(out=bc[:, D:2 * D], in0=bc[:, 2 * D:3 * D], scalar=1.0, in1=bc[:, D:2 * D], op0=ALU.add, op1=ALU.mult),
             reads=[r_bc], writes=[r_bc])
        g2b, gsfb, shfb = bc[:, 0:D], bc[:, D:2 * D], bc[:, 3 * D:4 * D]
        P.op("dve", lambda: V.tensor_scalar(out=mask32[:], in0=Gt[:], scalar1=0.0, scalar2=None, op0=ALU.is_gt), reads=[r_G], writes=[r_mask])
        P.op("pool", lambda: G_.tensor_copy(out=maskb[:], in_=mask32[:]), reads=[r_mask], writes=[r_maskb])
        for i in range(NTL):
            rank_ops(P, nc, bank[6], rb[6], maskb, r_maskb, i, cb, r_cb, mask32[:, i, :], r_mask, rk[:, i, :], r_rk, lo=0)
        ui = 0
        for e in range(NE):
            k = e % 2
            if CAP % 128 == 0:
                P.op("sp", lambda k=k, e=e: nc.sync.dma_start(out=yb[k][:], in_=yd[e].rearrange("(ck p) d -> p ck d", p=128)), writes=[r_yb[k]], dma=r_yb[k])
            else:
                P.op("sp", lambda k=k, e=e: nc.sync.dma_start(out=yb[k][0:CW, 0, :], in_=yd[e]), writes=[r_yb[k]], dma=r_yb[k])
            for i in range(NTL):
                q = ui % 2; ui += 1
                P.op("dve", lambda q=q, i=i, e=e: V.tensor_scalar(out=selw[q][:], in0=IOTA, scalar1=rk[:, i, e:e + 1], scalar2=Gt[:, i, e:e + 1], op0=ALU.is_equal, op1=ALU.mult),
                     reads=[r_c32, r_rk, r_G], writes=[r_selw[q]])
                tbk = 4 + q
                tview = bank[tbk][0:CW, :].bitcast(BF16)
                for ck in range(NCK):
                    P.op("pe", lambda q=q, ck=ck, tview=tview: T.transpose(out=tview[:, ck * 128:(ck + 1) * 128], in_=selw[q][:, ck * CW:(ck + 1) * CW], identity=cb[:, 0:128]),
                         reads=[r_selw[q], r_cb], writes=[rb[tbk]])
                P.op("act", lambda q=q, tview=tview: S.copy(out=swT[q][0:CW, :, :], in_=tview[:, 0:NCK * 128].rearrange("p (a b) -> p a b", a=NCK)), writes=[rb[tbk], r_swT[q]])
                for dg in range(4):
                    for ck in range(NCK):
                        P.op("pe", lambda k=k, q=q, ck=ck, dg=dg: T.matmul(bank[dg][:, :], lhsT=swT[q][0:CW, ck, :], rhs=yb[k][0:CW, ck, dg * 512:(dg + 1) * 512],
                                                                         start=(ck == 0), stop=(ck == NCK - 1)), reads=[r_swT[q], r_yb[k]], writes=[rb[dg]])
                    ds = slice(dg * 512, (dg + 1) * 512)
                    P.op("dve", lambda i=i, dg=dg, ds=ds: V.tensor_tensor(out=acc[:, i, ds], in0=bank[dg][:, :], in1=acc[:, i, ds], op=ALU.add),
                         reads=[r_acc[i]], writes=[rb[dg], r_acc[i]])
        finals = []
        for i in range(NTL):
            p = i % 2
            tsl = slice(i * 128, (i + 1) * 128)
            smt, r_smt = sm[p], r_sm[p]
            P.op("sp", lambda tsl=tsl: nc.sync.dma_start(out=hr[:], in_=hrd[tsl, :]), writes=[r_hr], dma=r_hr)
            P.op("dve", lambda i=i: V.tensor_tensor(out=acc[:, i, :], in0=acc[:, i, :], in1=g2b, op=ALU.mult), reads=[r_bc, r_acc[i]], writes=[r_acc[i]])
            P.op("pool", lambda i=i: G_.tensor_tensor(out=acc[:, i, :], in0=acc[:, i, :], in1=hr[:], op=ALU.add), reads=[r_hr, r_acc[i]], writes=[r_acc[i]])
            P.op("pool", lambda i=i: G_.tensor_tensor(out=tmp[:], in0=acc[:, i, :], in1=acc[:, i, :], op=ALU.mult), reads=[r_acc[i]], writes=[r_tmp])
            P.op("dve", lambda smt=smt: V.tensor_reduce(out=smt[:, 0:1], in_=tmp[:], axis=AX.X, op=ALU.add), reads=[r_tmp], writes=[r_smt])
            P.op("act", lambda smt=smt: S.activation(out=smt[:, 1:2], in_=smt[:, 0:1], func=AF.Ln, bias=EPS, scale=1.0 / D), reads=[r_smt], writes=[r_smt])
            P.op("act", lambda smt=smt: S.activation(out=smt[:, 2:3], in_=smt[:, 1:2], func=AF.Exp, scale=-0.5), reads=[r_smt], writes=[r_smt])
            P.op("dve", lambda smt=smt, i=i: V.scalar_tensor_tensor(out=tmp[:], in0=acc[:, i, :], scalar=smt[:, 2:3], in1=gsfb, op0=ALU.mult, op1=ALU.mult),
                 reads=[r_acc[i], r_smt, r_bc, r_tmp], writes=[r_tmp])
            P.op("pool", lambda p=p: G_.tensor_tensor(out=ot[p][:], in0=tmp[:], in1=shfb, op=ALU.add), reads=[r_tmp, r_bc], writes=[r_ot[p]])
            f = P.op("sp", lambda p=p, tsl=tsl: nc.sync.dma_start(out=outd[tsl, :], in_=ot[p][:]), reads=[r_ot[p]], dma=r_ot[p])
            finals.append(f)
        P.emit(final_ops=finals[-2:])
    return nc


TL_FULL = SEQ // NCORES
CAP_FULL = 640
NCH_FULL = 5


_DBG = {}


def _bcst(v):
    return np.broadcast_to(np.asarray(v)[None, :], (128, v.shape[0]))


def _run(nc, in_maps):
    return run_bass_kernel_spmd(nc, in_maps, core_ids=list(range(NCORES))).results


def kernel(x, c, w_ada, b_ada, norm_mix, w_in, b_i, b_f, fox_b_f, fox_q_norm, fox_k_norm,
           mlstm_out_norm, fox_out_norm, w_out, norm_ffn, w_router, b_router, w_up_gate,
           b_up_gate, w_down, b_down, w_ada_final, b_ada_final, norm_final):
    f32 = lambda a: np.asarray(a, dtype=np.float32)
    x = f32(x); c = f32(c)
    seq = x.shape[1]
    TL = seq // NCORES
    CAP = CAP_FULL
    mod = run_l0(c, f32(w_ada), f32(b_ada), f32(w_ada_final), f32(b_ada_final))
    sh1, sc1, g1, sh2, sc2, g2 = [mod[i * D:(i + 1) * D] for i in range(6)]
    shf, scf = mod[6 * D:7 * D], mod[7 * D:8 * D]
    xT = np.ascontiguousarray(x[0].T)
    w_in0 = f32(w_in)[0]
    in1 = [l1_inputs(j, xT, w_in0, f32(norm_mix)[0], sc1, sh1, f32(b_i)[0], f32(b_f)[0], f32(fox_b_f)[0],
                     f32(fox_q_norm)[0], f32(fox_k_norm)[0], f32(fox_out_norm)[0], f32(mlstm_out_norm)[0]) for j in range(NCORES)]
    r1 = _run(build_l1(seq), in1)
    del in1, xT
    mix = np.empty((seq, D), dtype=ml_dtypes.bfloat16)
    for j in range(NCORES):
        h, half = j // 2, j % 2
        o = np.asarray(r1[j]["hout"])
        mix[:, 256 * h + 128 * half:256 * h + 128 * half + 128] = o[:, 0:128]
        mix[:, 1024 + 128 * j:1024 + 128 * j + 128] = o[:, 128:256]
    mixT = np.ascontiguousarray(mix.T)
    w_out0 = f32(w_out)[0]
    in2 = [l2_inputs(np.ascontiguousarray(mixT[:, g * TL:(g + 1) * TL]), np.ascontiguousarray(x[0, g * TL:(g + 1) * TL]), w_out0, g1,
                     f32(norm_ffn)[0], sc2, sh2, f32(w_router)[0], f32(b_router)[0]) for g in range(NCORES)]
    r2 = _run(build_l2(TL, CAP), in2)
    cf32, cbf = in2[0]["cf32"], in2[0]["cbf"]
    del in2
    in3 = []
    for cidx in range(NCORES):
        es = list(range(EPC * cidx, EPC * cidx + EPC))
        XT = np.ascontiguousarray(np.stack([np.concatenate([np.asarray(r2[g]["XT"])[e] for g in range(NCORES)], axis=1) for e in es]))
        in3.append({"XT": XT, "w_ug": np.ascontiguousarray(f32(w_up_gate)[0, es[0]:es[-1] + 1]),
                    "b_ug": np.ascontiguousarray(np.stack([f32(b_up_gate)[0, e].reshape(32, 128).T for e in es])),
                    "w_d": np.ascontiguousarray(f32(w_down)[0, es[0]:es[-1] + 1]),
                    "b_d": np.ascontiguousarray(np.stack([_bcst(f32(b_down)[0, e]) for e in es]))})
    r3 = _run(build_l3(NCORES * CAP, NCH_FULL), in3)
    del in3
    bc4 = np.ascontiguousarray(np.concatenate([_bcst(g2), _bcst(f32(norm_final)), _bcst(scf), _bcst(shf)], axis=1), dtype=np.float32)
    in4 = []
    for g in range(NCORES):
        yb = np.ascontiguousarray(np.stack([np.asarray(r3[e // EPC]["y"])[e % EPC][g * CAP:(g + 1) * CAP] for e in range(NE)]))
        in4.append({"yb": yb, "G": np.asarray(r2[g]["G"]), "hres": np.asarray(r2[g]["hres"]), "bc": bc4, "cf32": cf32, "cbf": cbf})
    r4 = _run(build_l4(TL, CAP), in4)
    out = np.concatenate([np.asarray(r["out"]) for r in r4], axis=0)[None]
    _DBG.update(mod=mod, mix=mix, G=np.concatenate([np.asarray(r2[g]["G"]) for g in range(NCORES)]), hres=np.concatenate([np.asarray(r2[g]["hres"]) for g in range(NCORES)]))
    return out.astype(np.float32)
```

```python
import contextlib
import numpy as np
import ml_dtypes
import concourse.bass as bass
import concourse.mybir as mybir
from concourse.bass_utils import run_bass_kernel_spmd

F32 = mybir.dt.float32
BF16 = mybir.dt.bfloat16
ALU = mybir.AluOpType
AF = mybir.ActivationFunctionType
AX = mybir.AxisListType
NCORES = 8

D = 2048
KC = D // 128
SEQ = 8192
EPS = 1e-6


class Res:
    __slots__ = ("name", "w", "r", "sem", "cnt")

    def __init__(self, name):
        self.name = name
        self.w = None
        self.r = []
        self.sem = None
        self.cnt = 0


class Prog:
    ENG = ("pe", "act", "dve", "pool", "sp")

    def __init__(self, nc, stack):
        self.nc = nc
        self.stack = stack
        self.ops = []
        self.e = {"pe": nc.tensor, "act": nc.scalar, "dve": nc.vector, "pool": nc.gpsimd, "sp": nc.sync}
        self.nres = 0

    def res(self, name=None):
        self.nres += 1
        return Res(name or f"r{self.nres}")

    def sb(self, name, shape, dt):
        t = self.stack.enter_context(self.nc.sbuf_tensor(name, list(shape), dt))
        return t

    def ps(self, name, shape, dt=F32):
        return self.stack.enter_context(self.nc.psum_tensor(name, list(shape), dt))

    deferred = None

    def op(self, eng, fn, reads=(), writes=(), dma=None):
        if self.deferred is not None:
            self.deferred.append((eng, fn, tuple(reads), tuple(writes), dma))
            return None
        i = len(self.ops)
        deps = set()
        for r in reads:
            if r.w is not None:
                deps.add(r.w)
        for w in writes:
            if w.w is not None:
                deps.add(w.w)
            deps.update(w.r)
        for r in reads:
            r.r.append(i)
        for w in writes:
            w.w = i
            w.r = []
        self.ops.append(dict(eng=eng, fn=fn, deps=deps, dma=dma, wset=set(id(w) for w in writes),
                             rset=set(id(r) for r in reads)))
        return i

    def emit(self, final_ops=()):
        nc = self.nc
        ops = self.ops
        def stream(o):
            return ("dma", id(o["dma"])) if o["dma"] is not None else o["eng"]
        seen = {e: {} for e in self.ENG}
        pos = {}
        kept = []
        signal = set()
        for i, o in enumerate(ops):
            k = {}
            for d in o["deps"]:
                od = ops[d]
                sd = stream(od)
                if sd == o["eng"] and od["dma"] is None:
                    if o["eng"] == "pe":
                        continue
                    if not (od["wset"] & o["rset"]):
                        continue
                k[sd] = max(k.get(sd, -1), d)
            kk = []
            for sd, d in k.items():
                if seen[o["eng"]].get(sd, -1) >= d:
                    continue
                seen[o["eng"]][sd] = d
                kk.append((sd, d))
                signal.add(d)
            kept.append(kk)
        for d in final_ops:
            signal.add(d)
        for i, o in enumerate(ops):
            if o["dma"] is not None:
                signal.add(i)
        sems = {}
        def sem_of(sd):
            if sd not in sems:
                sems[sd] = self.stack.enter_context(nc.semaphore(f"s{len(sems)}"))
            return sems[sd]
        val = {}
        cur = {}
        for i, o in enumerate(ops):
            if i in signal:
                sd = stream(o)
                inc = 16 if o["dma"] is not None else 1
                cur[sd] = cur.get(sd, 0) + inc
                val[i] = cur[sd]
        for i, o in enumerate(ops):
            eng = self.e[o["eng"]]
            for sd, d in kept[i]:
                eng.wait_ge(sem_of(sd), val[d])
            inst = o["fn"]()
            if i in signal:
                sd = stream(o)
                inst.then_inc(sem_of(sd), 16 if o["dma"] is not None else 1)
        for d in final_ops:
            self.e["sp"].wait_ge(sem_of(stream(ops[d])), val[d])


def new_nc():
    return bass.Bass("TRN2", target_bir_lowering=False)


def build_l0():
    nc = new_nc()
    NW = 2048
    c_in = nc.dram_tensor("c", [128, KC], F32, kind="ExternalInput").ap()
    w_in = nc.dram_tensor("w", [D, NW], F32, kind="ExternalInput").ap()
    b_in = nc.dram_tensor("b", [1, NW], F32, kind="ExternalInput").ap()
    out = nc.dram_tensor("mod", [1, NW], F32, kind="ExternalOutput").ap()
    with contextlib.ExitStack() as st:
        P = Prog(nc, st)
        ct = P.sb("ct", [128, KC], F32); r_ct = P.res()
        ca = P.sb("ca", [128, KC], F32); r_ca = P.res()
        bt = P.sb("bt", [1, NW], F32); r_bt = P.res()
        ot = P.sb("ot", [1, NW], F32); r_ot = P.res()
        wt = [P.sb(f"wt{i}", [128, KC, 512], F32) for i in range(2)]
        r_wt = [P.res() for _ in range(2)]
        pst = [P.ps(f"ps{i}", [1, 512]) for i in range(2)]
        r_ps = [P.res() for _ in range(2)]
        P.op("sp", lambda: nc.sync.dma_start(out=ct[:], in_=c_in), writes=[r_ct], dma=r_ct)
        P.op("sp", lambda: nc.sync.dma_start(out=bt[:], in_=b_in), writes=[r_bt], dma=r_bt)
        P.op("act", lambda: nc.scalar.activation(out=ca[:], in_=ct[:], func=AF.Silu), reads=[r_ct], writes=[r_ca])
        wv = w_in.rearrange("(kc p) n -> p kc n", p=128)
        for g in range(4):
            b = g % 2
            P.op("sp", lambda g=g, b=b: nc.sync.dma_start(out=wt[b][:], in_=wv[:, :, g * 512:(g + 1) * 512]),
                 writes=[r_wt[b]], dma=r_wt[b])
            for kc in range(KC):
                P.op("pe", lambda b=b, kc=kc: nc.tensor.matmul(pst[b][:], lhsT=ca[:, kc:kc + 1], rhs=wt[b][:, kc, :],
                                                               start=(kc == 0), stop=(kc == KC - 1)),
                     reads=[r_ca, r_wt[b]], writes=[r_ps[b]])
            P.op("dve", lambda g=g, b=b: nc.vector.tensor_tensor(out=ot[:, g * 512:(g + 1) * 512], in0=pst[b][:],
                                                                 in1=bt[:, g * 512:(g + 1) * 512], op=ALU.add),
                 reads=[r_ps[b], r_bt], writes=[r_ot])
        f = P.op("sp", lambda: nc.sync.dma_start(out=out, in_=ot[:]), reads=[r_ot], dma=r_ot)
        P.emit(final_ops=[f])
    return nc


def run_l0(c, w_ada, b_ada, w_ada_final, b_ada_final):
    wcat = np.concatenate([w_ada[0], w_ada_final], axis=1)
    bcat = np.concatenate([b_ada[0], b_ada_final], axis=0)
    cl = np.ascontiguousarray(c[0].reshape(KC, 128).T)
    in_maps = []
    for j in range(NCORES):
        in_maps.append({"c": cl, "w": np.ascontiguousarray(wcat[:, j * 2048:(j + 1) * 2048]),
                        "b": np.ascontiguousarray(bcat[None, j * 2048:(j + 1) * 2048])})
    res = run_bass_kernel_spmd(build_l0(), in_maps, core_ids=list(range(NCORES)))
    return np.concatenate([r["mod"][0] for r in res.results])


NC1 = 1027


def build_l1(seq):
    NT = seq // 128
    nc = new_nc()
    xT = nc.dram_tensor("xT", [D, seq], F32, kind="ExternalInput").ap()
    w1 = nc.dram_tensor("w1", [D, NC1], F32, kind="ExternalInput").ap()
    v16d = nc.dram_tensor("v16", [128, 48], F32, kind="ExternalInput").ap()
    scald = nc.dram_tensor("scal", [1, 3], F32, kind="ExternalInput").ap()
    gvecd = nc.dram_tensor("gvec", [128, 512], F32, kind="ExternalInput").ap()
    cf32d = nc.dram_tensor("cf32", [128, 256], F32, kind="ExternalInput").ap()
    cbfd = nc.dram_tensor("cbf", [128, 384], BF16, kind="ExternalInput").ap()
    hout = nc.dram_tensor("hout", [seq, 256], BF16, kind="ExternalOutput").ap()
    V, S, G, T = nc.vector, nc.scalar, nc.gpsimd, nc.tensor
    with contextlib.ExitStack() as st:
        P = Prog(nc, st)
        R = P.res
        bank = [P.ps(f"bank{i}", [128, 512]) for i in range(8)]
        rb = [R(f"bank{i}") for i in range(8)]
        v16 = P.sb("v16t", [128, 48], F32); r_v16 = R()
        gs = P.sb("gs", [128, 16], F32); r_gs = R()
        scal = P.sb("scalt", [1, 3], F32); r_scal = R()
        gv = P.sb("gv", [128, 512], F32); r_gv = R()
        c32 = P.sb("c32", [128, 256], F32); r_c32 = R()
        cb = P.sb("cb", [128, 384], BF16); r_cb = R()
        UT32, ONE32 = c32[:, 0:128], c32[:, 128:256]
        IDB, UTB, ONEB = cb[:, 0:128], cb[:, 128:256], cb[:, 256:384]
        wst = [P.sb(f"wst{i}", [128, NC1], F32) for i in range(2)]; r_wst = [R(), R()]
        Wb = P.sb("Wb", [128, KC, NC1 + 1], BF16); r_Wb = R()
        shWrow = P.sb("shWrow", [1, NC1], F32); r_shWrow = R()
        shWb = P.sb("shWb", [128, NC1], F32); r_shWb = R()
        XG = 256
        xst = [P.sb(f"xst{i}", [128, KC, XG], F32) for i in range(2)]; r_xst = [R(), R()]
        xb = [P.sb(f"xb{i}", [128, KC, XG], BF16) for i in range(2)]; r_xb = [R(), R()]
        sq = [P.sb(f"sq{i}", [128, KC, XG], BF16) for i in range(2)]; r_sq = [R(), R()]
        pj = [P.sb(f"pj{i}", [128, NC1], F32) for i in range(2)]; r_pj = [R(), R()]
        kT = P.sb("kT", [128, seq], BF16); r_kT = [R() for _ in range(NT)]
        vaug = P.sb("vaug", [128, NT, 130], BF16); r_v = [R() for _ in range(NT)]
        cmat = P.sb("cmat", [128, NT], F32); r_cmat = R()
        nU = P.sb("nU", [128, NT], F32); r_nU = R()
        biasm = [P.sb(f"biasm{i}", [128, NT], F32) for i in range(2)]; r_bias = [R(), R()]
        two = lambda nm, shp, dt: ([P.sb(f"{nm}{i}", shp, dt) for i in range(2)], [R(), R()])
        qn, r_qn = two("qn", [128, 128], BF16)
        kn, r_kn = two("kn", [128, 128], BF16)
        qT, r_qT = two("qT", [128, 128], BF16)
        mqb, r_mqb = two("mqb", [128, 128], BF16)
        kab, r_kab = two("kab", [128, 128], BF16)
        mqT, r_mqT = two("mqT", [128, 128], BF16)
        kaT, r_kaT = two("kaT", [128, 128], BF16)
        vm, r_vm = two("vm", [128, 258], BF16)
        sg, r_sg = two("sg", [128, 128], F32)
        SmT, r_SmT = two("SmT", [128, 128], BF16)
        outt, r_outt = two("outt", [128, 256], BF16)
        NPT = 3
        PT = [P.sb(f"PT{i}", [128, 128], BF16) for i in range(NPT)]; r_PT = [R() for _ in range(NPT)]
        junk = P.sb("junk", [128, 256], F32); r_junk = R()
        junk2 = P.sb("junk2", [128, 256], F32); r_junk2 = R()
        sm, r_sm = two("sm", [128, 32], F32)
        Cst = P.sb("Cst", [128, 257], F32); r_C = R()
        Ctmp = P.sb("Ctmp", [128, 257], F32); r_Ctmp = R()
        Cb = P.sb("Cb", [128, 258], BF16); r_Cb = R()
        h_m = P.sb("h_m", [128, 256], F32); r_hm = R()
        hfu = P.sb("hfu", [128, 128], F32); r_hfu = R()
        otmp = P.sb("otmp", [128, 128], F32); r_otmp = R()

        P.op("sp", lambda: nc.sync.dma_start(out=v16[:], in_=v16d), writes=[r_v16], dma=r_v16)
        P.op("sp", lambda: nc.sync.dma_start(out=scal[:], in_=scald), writes=[r_scal], dma=r_scal)
        P.op("sp", lambda: nc.sync.dma_start(out=gv[:], in_=gvecd), writes=[r_gv], dma=r_gv)
        P.op("sp", lambda: nc.sync.dma_start(out=c32[:], in_=cf32d), writes=[r_c32], dma=r_c32)
        P.op("sp", lambda: nc.sync.dma_start(out=cb[:], in_=cbfd), writes=[r_cb], dma=r_cb)
        P.op("dve", lambda: V.tensor_scalar(out=gs[:], in0=v16[:, 16:32], scalar1=1.0, scalar2=None, op0=ALU.add),
             reads=[r_v16], writes=[r_gs])
        P.op("dve", lambda: V.tensor_tensor(out=gs[:], in0=gs[:], in1=v16[:, 0:16], op=ALU.mult),
             reads=[r_v16, r_gs], writes=[r_gs])
        P.op("dve", lambda: V.tensor_scalar(out=gv[:, 0:128], in0=gv[:, 0:128], scalar1=128.0 ** -0.5, scalar2=None,
                                            op0=ALU.mult), reads=[r_gv], writes=[r_gv])
        P.op("pool", lambda: G.memset(vaug[:], 1.0), writes=r_v)
        for i in range(2):
            P.op("pool", lambda i=i: G.memset(vm[i][:], 1.0), writes=[r_vm[i]])
        P.op("pool", lambda: G.memset(Cst[:], 0.0), writes=[r_C])
        P.op("pool", lambda: G.memset(Cb[:], 0.0), writes=[r_Cb])
        P.op("pool", lambda: G.memset(nU[:], 0.0), writes=[r_nU])
        groups = [(0, 512, 0), (512, 1024, 1), (1024, NC1, 2)]
        for kc in range(KC):
            b = kc % 2
            P.op("sp", lambda kc=kc, b=b: nc.sync.dma_start(out=wst[b][:], in_=w1[kc * 128:(kc + 1) * 128, :]),
                 writes=[r_wst[b]], dma=r_wst[b])
            for (c0, c1, bk) in groups:
                P.op("pe", lambda kc=kc, b=b, c0=c0, c1=c1, bk=bk: T.matmul(
                    bank[bk][0:1, 0:c1 - c0], lhsT=v16[:, 32 + kc:33 + kc], rhs=wst[b][:, c0:c1],
                    start=(kc == 0), stop=(kc == KC - 1)), reads=[r_v16, r_wst[b]], writes=[rb[bk]])
            P.op("dve", lambda kc=kc, b=b: V.tensor_scalar(out=Wb[:, kc, 0:NC1], in0=wst[b][:], scalar1=gs[:, kc:kc + 1],
                                                          scalar2=None, op0=ALU.mult),
                 reads=[r_wst[b], r_gs], writes=[r_Wb])
        for (c0, c1, bk) in groups:
            P.op("dve", lambda c0=c0, c1=c1, bk=bk: V.tensor_copy(out=shWrow[:, c0:c1], in_=bank[bk][0:1, 0:c1 - c0]),
                 reads=[], writes=[rb[bk], r_shWrow])
        P.op("dve", lambda: V.tensor_tensor(out=shWrow[:, 1024:1027], in0=shWrow[:, 1024:1027], in1=scal[:, 0:3], op=ALU.add),
             reads=[r_scal, r_shWrow], writes=[r_shWrow])
        for (c0, c1, bk) in groups:
            P.op("pe", lambda c0=c0, c1=c1, bk=bk: T.matmul(bank[bk][:, 0:c1 - c0], lhsT=c32[0:1, 128:256],
                                                          rhs=shWrow[0:1, c0:c1], start=True, stop=True),
                 reads=[r_c32, r_shWrow], writes=[rb[bk]])
            P.op("dve", lambda c0=c0, c1=c1, bk=bk: V.tensor_copy(out=shWb[:, c0:c1], in_=bank[bk][:, 0:c1 - c0]),
                 writes=[rb[bk], r_shWb])

        xv = xT.rearrange("(kc p) t -> p kc t", p=128)
        trp = bank[2][:, 256:512].bitcast(BF16)
        psS = bank[2]
        final = []
        ptk = 0
        pend = []

        def stageA1(i):
            p = i % 2
            gb = (i // 2) % 2
            if i % 2 == 0:
                gi = i // 2
                P.op("sp", lambda gi=gi, gb=gb: nc.sync.dma_start(out=xst[gb][:], in_=xv[:, :, gi * XG:(gi + 1) * XG]),
                     writes=[r_xst[gb]], dma=r_xst[gb])
                P.op("act", lambda gb=gb: S.copy(out=xb[gb][:], in_=xst[gb][:]), reads=[r_xst[gb]], writes=[r_xb[gb]])
                P.op("pool", lambda gb=gb: G.tensor_tensor(out=sq[gb][:], in0=xst[gb][:], in1=xst[gb][:], op=ALU.mult),
                     reads=[r_xst[gb]], writes=[r_sq[gb]])
            ts = slice((i % 2) * 128, (i % 2) * 128 + 128)
            smt, r_smt = sm[p], r_sm[p]
            for kc in range(KC):
                P.op("pe", lambda gb=gb, kc=kc, ts=ts: T.matmul(psS[:, 8:9], lhsT=sq[gb][:, kc, ts], rhs=cb[:, 256:257],
                                                              start=(kc == 0), stop=(kc == KC - 1)),
                     reads=[r_sq[gb], r_cb], writes=[rb[2]])
            P.op("act", lambda smt=smt: S.activation(out=smt[:, 0:1], in_=psS[:, 8:9], func=AF.Ln, bias=EPS, scale=1.0 / D),
                 writes=[rb[2], r_smt])
            P.op("act", lambda smt=smt: S.activation(out=smt[:, 1:2], in_=smt[:, 0:1], func=AF.Exp, scale=-0.5),
                 reads=[r_smt], writes=[r_smt])
            for (c0, c1, bk) in groups:
                for kc in range(KC):
                    P.op("pe", lambda gb=gb, kc=kc, ts=ts, c0=c0, c1=c1, bk=bk: T.matmul(
                        bank[bk][:, 0:c1 - c0], lhsT=xb[gb][:, kc, ts], rhs=Wb[:, kc, c0:c1],
                        start=(kc == 0), stop=(kc == KC - 1)), reads=[r_xb[gb], r_Wb], writes=[rb[bk]])
            for (c0, c1, bk) in groups:
                P.op("dve", lambda p=p, smt=smt, c0=c0, c1=c1, bk=bk: V.scalar_tensor_tensor(
                    out=pj[p][:, c0:c1], in0=bank[bk][:, 0:c1 - c0], scalar=smt[:, 1:2], in1=shWb[:, c0:c1],
                    op0=ALU.mult, op1=ALU.add), reads=[r_smt, r_shWb], writes=[rb[bk], r_pj[p]])
            pjt, r_pjt = pj[p], r_pj[p]
            P.deferred = pend
            P.op("act", lambda pjt=pjt, smt=smt: S.activation(out=smt[:, 2:4], in_=pjt[:, 1024:1026], func=AF.Exp, scale=2.0 / 15.0),
                 reads=[r_pjt], writes=[r_smt])
            P.op("dve", lambda smt=smt: V.tensor_scalar(out=smt[:, 2:4], in0=smt[:, 2:4], scalar1=1.0, scalar2=None, op0=ALU.add),
                 reads=[r_smt], writes=[r_smt])
            P.op("dve", lambda smt=smt: V.reciprocal(out=smt[:, 2:4], in_=smt[:, 2:4]), reads=[r_smt], writes=[r_smt])
            P.op("dve", lambda smt=smt: V.tensor_scalar(out=smt[:, 4:6], in0=smt[:, 2:4], scalar1=-30.0, scalar2=15.0,
                                                       op0=ALU.mult, op1=ALU.add), reads=[r_smt], writes=[r_smt])
            P.op("dve", lambda smt=smt, pjt=pjt: V.tensor_copy(out=smt[:, 6:7], in_=pjt[:, 1026:1027]), reads=[r_pjt, r_smt], writes=[r_smt])
            P.op("act", lambda smt=smt: S.activation(out=smt[:, 8:10], in_=smt[:, 5:7], func=AF.Exp, scale=-1.0),
                 reads=[r_smt], writes=[r_smt])
            P.op("act", lambda smt=smt: S.activation(out=smt[:, 8:10], in_=smt[:, 8:10], func=AF.Ln, bias=1.0, scale=1.0),
                 reads=[r_smt], writes=[r_smt])
            P.op("pool", lambda pjt=pjt: G.tensor_tensor(out=junk[:], in0=pjt[:, 0:256], in1=pjt[:, 0:256], op=ALU.mult),
                 reads=[r_pjt], writes=[r_junk])
            P.op("dve", lambda smt=smt: V.tensor_reduce(out=smt[:, 18:20], in_=junk[:].rearrange("p (a b) -> p a b", a=2), axis=AX.X, op=ALU.add),
                 reads=[r_junk, r_smt], writes=[r_smt])
            P.op("act", lambda smt=smt: S.activation(out=smt[:, 18:20], in_=smt[:, 18:20], func=AF.Ln, bias=EPS, scale=1.0 / 128), reads=[r_smt], writes=[r_smt])
            P.op("act", lambda smt=smt: S.activation(out=smt[:, 20:22], in_=smt[:, 18:20], func=AF.Exp, scale=-0.5), reads=[r_smt], writes=[r_smt])
            P.op("dve", lambda p=p, pjt=pjt, smt=smt: V.scalar_tensor_tensor(out=qn[p][:], in0=pjt[:, 0:128], scalar=smt[:, 20:21], in1=gv[:, 0:128],
                                                                          op0=ALU.mult, op1=ALU.mult), reads=[r_pjt, r_smt, r_gv], writes=[r_qn[p]])
            P.op("dve", lambda p=p, pjt=pjt, smt=smt: V.scalar_tensor_tensor(out=kn[p][:], in0=pjt[:, 128:256], scalar=smt[:, 21:22], in1=gv[:, 128:256],
                                                                          op0=ALU.mult, op1=ALU.mult), reads=[r_pjt, r_smt, r_gv], writes=[r_kn[p]])
            P.op("pool", lambda p=p, pjt=pjt: G.tensor_scalar(out=mqb[p][:], in0=pjt[:, 384:512], scalar1=128.0 ** -0.5, scalar2=None, op0=ALU.mult),
                 reads=[r_pjt], writes=[r_mqb[p]])
            P.op("pool", lambda p=p, pjt=pjt: G.tensor_copy(out=vm[p][:, 0:256], in_=pjt[:, 640:896]), reads=[r_pjt], writes=[r_vm[p]])
            P.op("pool", lambda i=i, pjt=pjt: G.tensor_copy(out=vaug[:, i, 0:128], in_=pjt[:, 256:384]), reads=[r_pjt], writes=[r_v[i]])
            P.op("act", lambda p=p, pjt=pjt: S.activation(out=sg[p][:], in_=pjt[:, 896:1024], func=AF.Exp, scale=-1.0), reads=[r_pjt], writes=[r_sg[p]])
            P.op("pool", lambda p=p: G.tensor_scalar(out=sg[p][:], in0=sg[p][:], scalar1=1.0, scalar2=None, op0=ALU.add), reads=[r_sg[p]], writes=[r_sg[p]])
            P.op("dve", lambda p=p: V.reciprocal(out=sg[p][:], in_=sg[p][:]), reads=[r_sg[p]], writes=[r_sg[p]])
            P.deferred = None

        def flush(n=None):
            k = len(pend) if n is None else min(n, len(pend))
            for _ in range(k):
                a = pend.pop(0)
                P.op(a[0], a[1], reads=a[2], writes=a[3], dma=a[4])

        def stageA2(i):
            p = i % 2
            gb = (i // 2) % 2
            ts = slice((i % 2) * 128, (i % 2) * 128 + 128)
            smt, r_smt = sm[p], r_sm[p]
            pjt, r_pjt = pj[p], r_pj[p]
            A_, B_, DEC_ = smt[:, 15:16], smt[:, 16:17], smt[:, 17:18]
            P.op("pe", lambda smt=smt: T.matmul(psS[:, 10:12], lhsT=UT32, rhs=smt[:, 8:10], start=True, stop=True),
                 reads=[r_c32, r_smt], writes=[rb[2]])
            P.op("pe", lambda smt=smt: T.matmul(psS[:, 12:14], lhsT=ONE32, rhs=smt[:, 8:10], start=True, stop=True),
                 reads=[r_c32, r_smt], writes=[rb[2]])
            P.op("dve", lambda smt=smt: V.tensor_copy(out=smt[:, 10:14], in_=psS[:, 10:14]), reads=[r_smt], writes=[rb[2], r_smt])
            P.op("dve", lambda smt=smt: V.tensor_tensor(out=smt[:, 14:15], in0=smt[:, 4:5], in1=smt[:, 10:11], op=ALU.add),
                 reads=[r_smt], writes=[r_smt])
            P.op("act", lambda smt=smt: S.activation(out=smt[:, 15:16], in_=smt[:, 14:15], func=AF.Exp), reads=[r_smt], writes=[r_smt])
            P.op("act", lambda smt=smt: S.activation(out=smt[:, 16:17], in_=smt[:, 10:11], func=AF.Exp, scale=-1.0), reads=[r_smt], writes=[r_smt])
            P.op("act", lambda smt=smt: S.activation(out=smt[:, 17:18], in_=smt[:, 12:13], func=AF.Exp, scale=-1.0), reads=[r_smt], writes=[r_smt])
            A_, B_, DEC_ = smt[:, 15:16], smt[:, 16:17], smt[:, 17:18]
            P.op("dve", lambda smt=smt, i=i: V.tensor_copy(out=cmat[:, i:i + 1], in_=smt[:, 11:12]), reads=[r_smt], writes=[r_cmat])
            P.op("dve", lambda smt=smt, i=i: V.tensor_scalar(out=nU[:, 0:i + 1], in0=nU[:, 0:i + 1], scalar1=smt[:, 13:14], scalar2=None,
                                                            op0=ALU.add), reads=[r_smt, r_nU], writes=[r_nU])
            P.op("dve", lambda p=p, i=i: V.tensor_tensor(out=biasm[p][:, 0:i + 1], in0=cmat[:, 0:i + 1], in1=nU[:, 0:i + 1], op=ALU.subtract),
                 reads=[r_cmat, r_nU], writes=[r_bias[p]])
            for k, (src, r_src) in enumerate([(qn[p], r_qn[p]), (kn[p], r_kn[p])]):
                P.op("pe", lambda k=k, src=src: T.transpose(out=trp[:, k * 128:(k + 1) * 128], in_=src[:], identity=IDB),
                     reads=[r_src, r_cb], writes=[rb[2]])
            P.op("act", lambda p=p: S.copy(out=qT[p][:], in_=trp[:, 0:128]), writes=[rb[2], r_qT[p]])
            P.op("act", lambda i=i: S.copy(out=kT[:, i * 128:(i + 1) * 128], in_=trp[:, 128:256]), writes=[rb[2], r_kT[i]])
            P.op("dve", lambda p=p, pjt=pjt, A_=A_: V.tensor_scalar(out=kab[p][:], in0=pjt[:, 512:640], scalar1=A_, scalar2=None, op0=ALU.mult),
                 reads=[r_pjt, r_smt], writes=[r_kab[p]])
            for k, (src, r_src) in [(2, (mqb[p], r_mqb[p])), (3, (kab[p], r_kab[p]))]:
                P.op("pe", lambda k=k, src=src: T.transpose(out=trp[:, k * 128:(k + 1) * 128], in_=src[:], identity=IDB),
                     reads=[r_src, r_cb], writes=[rb[2]])
            P.op("dve", lambda p=p: V.tensor_copy(out=mqT[p][:], in_=trp[:, 256:384]), writes=[rb[2], r_mqT[p]])
            P.op("dve", lambda p=p: V.tensor_copy(out=kaT[p][:], in_=trp[:, 384:512]), writes=[rb[2], r_kaT[p]])
            P.op("pe", lambda p=p: T.matmul(bank[3][:, 0:257], lhsT=kab[p][:], rhs=vm[p][:, 0:257], start=True, stop=True),
                 reads=[r_kab[p], r_vm[p]], writes=[rb[3]])
            P.op("pe", lambda p=p: T.matmul(bank[4][:, 0:128], lhsT=kaT[p][:], rhs=mqT[p][:], start=True, stop=True),
                 reads=[r_kaT[p], r_mqT[p]], writes=[rb[4]])
            P.op("dve", lambda p=p: V.tensor_tensor(out=SmT[p][:], in0=bank[4][:, 0:128], in1=UT32, op=ALU.mult),
                 reads=[r_c32], writes=[rb[4], r_SmT[p]])
            P.op("pe", lambda p=p: T.matmul(bank[4][:, 128:385], lhsT=mqT[p][:], rhs=Cb[:, 0:257], start=True, stop=False),
                 reads=[r_mqT[p], r_Cb], writes=[rb[4]])
            P.op("pe", lambda p=p: T.matmul(bank[4][:, 128:385], lhsT=SmT[p][:], rhs=vm[p][:, 0:257], start=False, stop=True),
                 reads=[r_SmT[p], r_vm[p]], writes=[rb[4]])
            P.op("dve", lambda: V.tensor_tensor(out=Ctmp[:], in0=bank[3][:, 0:257], in1=Cst[:], op=ALU.add),
                 reads=[r_C], writes=[rb[3], r_Ctmp])
            P.op("dve", lambda DEC_=DEC_: V.tensor_scalar(out=Cst[:], in0=Ctmp[:], scalar1=DEC_, scalar2=None, op0=ALU.mult),
                 reads=[r_Ctmp, r_smt], writes=[r_C])
            P.op("pool", lambda: G.tensor_copy(out=Cb[:, 0:257], in_=Cst[:]), reads=[r_C], writes=[r_Cb])
            P.op("dve", lambda smt=smt, B_=B_: V.tensor_scalar(out=smt[:, 22:23], in0=bank[4][:, 384:385], scalar1=B_, scalar2=None,
                                                             op0=ALU.mult), reads=[r_smt], writes=[rb[4], r_smt])
            P.op("dve", lambda smt=smt: V.tensor_scalar(out=smt[:, 30:31], in0=smt[:, 22:23], scalar1=-1.0, scalar2=1.0,
                                                       op0=ALU.mult, op1=ALU.max), reads=[r_smt], writes=[r_smt])
            P.op("dve", lambda smt=smt: V.tensor_tensor(out=smt[:, 22:23], in0=smt[:, 22:23], in1=smt[:, 30:31], op=ALU.max),
                 reads=[r_smt], writes=[r_smt])
            P.op("dve", lambda smt=smt: V.reciprocal(out=smt[:, 23:24], in_=smt[:, 22:23]), reads=[r_smt], writes=[r_smt])
            P.op("dve", lambda smt=smt, B_=B_: V.tensor_tensor(out=smt[:, 24:25], in0=smt[:, 23:24], in1=B_, op=ALU.mult), reads=[r_smt], writes=[r_smt])
            P.op("dve", lambda smt=smt: V.tensor_scalar(out=h_m[:], in0=bank[4][:, 128:384], scalar1=smt[:, 24:25], scalar2=None, op0=ALU.mult),
                 reads=[r_smt], writes=[rb[4], r_hm])
            P.op("pool", lambda: G.tensor_tensor(out=junk2[:], in0=h_m[:], in1=h_m[:], op=ALU.mult), reads=[r_hm], writes=[r_junk2])
            P.op("dve", lambda smt=smt: V.tensor_reduce(out=smt[:, 25:26], in_=junk2[:], axis=AX.X, op=ALU.add), reads=[r_junk2, r_smt], writes=[r_smt])
            P.op("act", lambda smt=smt: S.activation(out=smt[:, 25:26], in_=smt[:, 25:26], func=AF.Ln, bias=EPS, scale=1.0 / 256), reads=[r_smt], writes=[r_smt])
            P.op("act", lambda smt=smt: S.activation(out=smt[:, 26:27], in_=smt[:, 25:26], func=AF.Exp, scale=-0.5), reads=[r_smt], writes=[r_smt])
            P.op("dve", lambda smt=smt: V.scalar_tensor_tensor(out=otmp[:], in0=h_m[:, 0:128], scalar=smt[:, 26:27], in1=gv[:, 384:512],
                                                             op0=ALU.mult, op1=ALU.mult), reads=[r_hm, r_smt, r_gv], writes=[r_otmp])
            P.op("dve", lambda p=p: V.tensor_tensor(out=outt[p][:, 0:128], in0=otmp[:], in1=sg[p][:], op=ALU.mult),
                 reads=[r_otmp, r_sg[p]], writes=[r_outt[p]])
            P.deferred = None

        def stageB(i):
            nonlocal ptk
            p = i % 2
            gb = (i // 2) % 2
            ts = slice((i % 2) * 128, (i % 2) * 128 + 128)
            smt, r_smt = sm[p], r_sm[p]
            pjt, r_pjt = pj[p], r_pj[p]
            A_, B_, DEC_ = smt[:, 15:16], smt[:, 16:17], smt[:, 17:18]
            def qk(sb, p=p):
                pb = 5 + (sb % 2)
                P.op("pe", lambda: T.matmul(bank[pb][:, 0:128], lhsT=kT[:, sb * 128:(sb + 1) * 128], rhs=qT[p][:], start=True, stop=True),
                     reads=[r_kT[sb], r_qT[p]], writes=[rb[pb]])
            def pv(sb, k, p=p, i=i):
                pb = 5 + (sb % 2)
                P.op("act", lambda: S.activation(out=PT[k][:], in_=bank[pb][:, 0:128], func=AF.Exp, bias=biasm[p][:, sb:sb + 1], scale=1.0),
                     reads=[r_bias[p]], writes=[rb[pb], r_PT[k]])
                if sb == i:
                    P.op("pool", lambda: G.tensor_tensor(out=PT[k][:], in0=PT[k][:], in1=UTB, op=ALU.mult), reads=[r_cb, r_PT[k]], writes=[r_PT[k]])
                P.op("pe", lambda: T.matmul(bank[7][:, 0:129], lhsT=PT[k][:], rhs=vaug[:, sb, 0:129], start=(sb == 0), stop=(sb == i)),
                     reads=[r_PT[k], r_v[sb]], writes=[rb[7]])
            share = (len(pend) + i) // (i + 1)
            qk(0)
            for sb in range(i + 1):
                if sb + 1 <= i:
                    qk(sb + 1)
                pv(sb, ptk % NPT)
                ptk += 1
                flush(share)
            flush()
            P.op("dve", lambda smt=smt: V.reciprocal(out=smt[:, 27:28], in_=bank[7][:, 128:129]), reads=[r_smt], writes=[rb[7], r_smt])
            P.op("dve", lambda smt=smt: V.tensor_scalar(out=hfu[:], in0=bank[7][:, 0:128], scalar1=smt[:, 27:28], scalar2=None, op0=ALU.mult),
                 reads=[r_smt], writes=[rb[7], r_hfu])
            P.op("pool", lambda: G.tensor_tensor(out=junk2[:, 0:128], in0=hfu[:], in1=hfu[:], op=ALU.mult), reads=[r_hfu], writes=[r_junk2])
            P.op("dve", lambda smt=smt: V.tensor_reduce(out=smt[:, 28:29], in_=junk2[:, 0:128], axis=AX.X, op=ALU.add), reads=[r_junk2, r_smt], writes=[r_smt])
            P.op("act", lambda smt=smt: S.activation(out=smt[:, 28:29], in_=smt[:, 28:29], func=AF.Ln, bias=EPS, scale=1.0 / 128), reads=[r_smt], writes=[r_smt])
            P.op("act", lambda smt=smt: S.activation(out=smt[:, 29:30], in_=smt[:, 28:29], func=AF.Exp, scale=-0.5), reads=[r_smt], writes=[r_smt])
            P.op("dve", lambda p=p, smt=smt: V.scalar_tensor_tensor(out=outt[p][:, 128:256], in0=hfu[:], scalar=smt[:, 29:30], in1=gv[:, 256:384],
                                                                  op0=ALU.mult, op1=ALU.mult), reads=[r_hfu, r_smt, r_gv], writes=[r_outt[p]])
            f = P.op("sp", lambda p=p, i=i: nc.sync.dma_start(out=hout[i * 128:(i + 1) * 128, :], in_=outt[p][:]), reads=[r_outt[p]], dma=r_outt[p])
            final.append(f)
        stageA1(0)
        flush()
        stageA2(0)
        for i in range(NT):
            if i + 1 < NT:
                stageA1(i + 1)
            stageB(i)
            if i + 1 < NT:
                stageA2(i + 1)
        P.emit(final_ops=final[-2:])
    return nc


W_OFF = dict(mq=0, mk=512, mv=1024, mo=2048, mi=3072, mf=3076, fq=3080, fk=4104, fv=5128, ff=6152)


def l1_inputs(j, xT, w_in, norm_mix, sc1, sh1, b_i, b_f, fox_b_f, gq, gk, gfo, gmo):
    h, half = j // 2, j % 2
    o = W_OFF
    cols = np.concatenate([
        np.arange(o["fq"] + 128 * j, o["fq"] + 128 * j + 128), np.arange(o["fk"] + 128 * j, o["fk"] + 128 * j + 128),
        np.arange(o["fv"] + 128 * j, o["fv"] + 128 * j + 128), np.arange(o["mq"] + 128 * h, o["mq"] + 128 * h + 128),
        np.arange(o["mk"] + 128 * h, o["mk"] + 128 * h + 128),
        np.arange(o["mv"] + 256 * h + 128 * half, o["mv"] + 256 * h + 128 * half + 128),
        np.arange(o["mv"] + 256 * h + 128 * (1 - half), o["mv"] + 256 * h + 128 * (1 - half) + 128),
        np.arange(o["mo"] + 256 * h + 128 * half, o["mo"] + 256 * h + 128 * half + 128),
        [o["mi"] + h, o["mf"] + h, o["ff"] + j]]).astype(np.int64)
    lay = lambda v: v.reshape(KC, 128).T
    v16 = np.ascontiguousarray(np.concatenate([lay(norm_mix), lay(sc1), lay(sh1)], axis=1), dtype=np.float32)
    scal = np.array([[b_i[h], b_f[h], fox_b_f[j]]], np.float32)
    bc = lambda v: np.broadcast_to(v[None, :], (128, v.shape[0]))
    gvec = np.ascontiguousarray(np.concatenate([bc(gq), bc(gk), bc(gfo[j]), bc(gmo[h, 128 * half:128 * half + 128])], axis=1), dtype=np.float32)
    ut = np.triu(np.ones((128, 128), np.float32))
    cf32 = np.ascontiguousarray(np.concatenate([ut, np.ones((128, 128), np.float32)], axis=1))
    cbf = np.ascontiguousarray(np.concatenate([np.eye(128, dtype=np.float32), ut, np.ones((128, 128), np.float32)], axis=1)).astype(ml_dtypes.bfloat16)
    return {"xT": xT, "w1": np.ascontiguousarray(w_in[:, cols]), "v16": v16, "scal": scal, "gvec": gvec, "cf32": cf32, "cbf": cbf}


NE = 32


def rank_ops(P, nc, bankr, rbr, maskb, r_maskb, i, cb, r_cb, mask32, r_mask, rk_out, r_rk, lo=32):
    V, T = nc.vector, nc.tensor
    for ii in range(i + 1):
        lhs = cb[:, 256:384] if ii < i else cb[:, 128:256]
        P.op("pe", lambda ii=ii, lhs=lhs: T.matmul(bankr[:, lo:lo + NE], lhsT=lhs, rhs=maskb[:, ii, :], start=(ii == 0), stop=(ii == i)),
             reads=[r_cb, r_maskb], writes=[rbr])
    P.op("dve", lambda: V.scalar_tensor_tensor(out=rk_out, in0=bankr[:, lo:lo + NE], scalar=1.0, in1=mask32, op0=ALU.add, op1=ALU.mult),
         reads=[r_mask], writes=[rbr, r_rk])
    P.op("dve", lambda: V.tensor_scalar(out=rk_out, in0=rk_out, scalar1=-1.0, scalar2=None, op0=ALU.add), reads=[r_rk], writes=[r_rk])


def build_l2(TL, CAP):
    NTL = TL // 128
    nc = new_nc()
    mixT = nc.dram_tensor("mixT", [D, TL], BF16, kind="ExternalInput").ap()
    xd = nc.dram_tensor("x", [TL, D], F32, kind="ExternalInput").ap()
    wod = nc.dram_tensor("w_out", [D, D], F32, kind="ExternalInput").ap()
    bcd = nc.dram_tensor("bc", [128, 3 * D + NE], F32, kind="ExternalInput").ap()
    sh2d = nc.dram_tensor("sh2", [128, D], F32, kind="ExternalInput").ap()
    wrd = nc.dram_tensor("w_r", [D, NE], F32, kind="ExternalInput").ap()
    cf32d = nc.dram_tensor("cf32", [128, 128 + 640], F32, kind="ExternalInput").ap()
    cbfd = nc.dram_tensor("cbf", [128, 384], BF16, kind="ExternalInput").ap()
    hres_o = nc.dram_tensor("hres", [TL, D], F32, kind="ExternalOutput").ap()
    G_o = nc.dram_tensor("G", [TL, NE], F32, kind="ExternalOutput").ap()
    XT_o = nc.dram_tensor("XT", [NE, D, CAP], BF16, kind="ExternalOutput").ap()
    V, S, G_, T = nc.vector, nc.scalar, nc.gpsimd, nc.tensor
    with contextlib.ExitStack() as st:
        P = Prog(nc, st)
        R = P.res
        bank = [P.ps(f"bank{i}", [128, 512]) for i in range(8)]
        rb = [R(f"bank{i}") for i in range(8)]
        wobf = P.sb("wob", [128, KC * D], BF16); r_wob = R()
        wob = wobf[:, :].rearrange("p (kc n) -> p kc n", kc=KC)
        bc = P.sb("bct", [128, 3 * D + NE], F32); r_bc = R()
        g1b, gs2b, brb = bc[:, 0:D], bc[:, D:2 * D], bc[:, 3 * D:3 * D + NE]
        sh2b = P.sb("sh2b", [128, D], F32); r_sh2 = R()
        wr = P.sb("wr", [128, KC, NE], F32); r_wr = R()
        c32 = P.sb("c32", [128, 768], F32); r_c32 = R()
        cb = P.sb("cb", [128, 384], BF16); r_cb = R()
        ID32, IOTA = c32[:, 0:128], c32[:, 128:128 + CAP]
        mx = [P.sb(f"mx{i}", [128, KC, 128], BF16) for i in range(2)]; r_mx = [R(), R()]
        _xt = P.sb("xt0", [128, D], F32); _rxt = R(); xt = [_xt, _xt]; r_xt = [_rxt, _rxt]
        hres = [P.sb(f"hres{i}", [128, D], F32) for i in range(2)]; r_hres = [R(), R()]
        h2f = P.sb("h2f", [128, D], F32); r_h2f = R()
        h2b = P.sb("h2b", [128, NTL, D], BF16); r_h2b = [R() for _ in range(NTL)]
        h2T = P.sb("h2T", [128, KC, 128], F32); r_h2T = R()
        maskb = P.sb("maskb", [128, NTL, NE], BF16); r_maskb = R()
        rk = P.sb("rk", [128, NTL, NE], F32); r_rk = R()
        sm = [P.sb(f"sm{i}", [128, 16], F32) for i in range(2)]; r_sm = [R(), R()]
        lg = [P.sb(f"lg{i}", [128, 4 * NE], F32) for i in range(2)]; r_lg = [R(), R()]
        assert 2 * NTL * CAP + 2 * KC * CAP <= KC * D
        sel = [wobf[:, q * NTL * CAP:(q + 1) * NTL * CAP].rearrange("p (i s) -> p i s", i=NTL) for q in range(2)]; r_sel = [R(), R()]
        xb0 = 2 * NTL * CAP
        xte = [wobf[:, xb0 + q * KC * CAP:xb0 + (q + 1) * KC * CAP].rearrange("p (fc s) -> p fc s", fc=KC) for q in range(2)]; r_xte = [R(), R()]

        r_wobk = [R() for _ in range(KC)]
        for kc in range(KC):
            P.op("pool", lambda kc=kc: G_.dma_start(out=wob[:, kc, :], in_=wod[kc * 128:(kc + 1) * 128, :]), writes=[r_wobk[kc]], dma=r_wobk[kc])
        P.op("sp", lambda: nc.sync.dma_start(out=bc[:], in_=bcd), writes=[r_bc], dma=r_bc)
        P.op("sp", lambda: nc.sync.dma_start(out=sh2b[:], in_=sh2d), writes=[r_sh2], dma=r_sh2)
        P.op("sp", lambda: nc.sync.dma_start(out=wr[:], in_=wrd.rearrange("(kc p) n -> p kc n", p=128)), writes=[r_wr], dma=r_wr)
        P.op("sp", lambda: nc.sync.dma_start(out=c32[:], in_=cf32d), writes=[r_c32], dma=r_c32)
        P.op("sp", lambda: nc.sync.dma_start(out=cb[:], in_=cbfd), writes=[r_cb], dma=r_cb)
        P.op("dve", lambda: V.scalar_tensor_tensor(out=bc[:, D:2 * D], in0=bc[:, 2 * D:3 * D], scalar=1.0, in1=bc[:, D:2 * D], op0=ALU.add, op1=ALU.mult),
             reads=[r_bc], writes=[r_bc])
        mv = mixT.rearrange("(kc p) t -> p kc t", p=128)
        finals = []
        for i in range(NTL):
            p = i % 2
            tsl = slice(i * 128, (i + 1) * 128)
            smt, r_smt = sm[p], r_sm[p]
            lgt, r_lgt = lg[p], r_lg[p]
            P.op("sp", lambda p=p, tsl=tsl: nc.sync.dma_start(out=mx[p][:], in_=mv[:, :, tsl]), writes=[r_mx[p]], dma=r_mx[p])
            P.op("sp", lambda p=p, tsl=tsl: nc.sync.dma_start(out=xt[p][:], in_=xd[tsl, :]), writes=[r_xt[p]], dma=r_xt[p])
            for dg in range(4):
                for kc in range(KC):
                    P.op("pe", lambda p=p, dg=dg, kc=kc: T.matmul(bank[dg][:, :], lhsT=mx[p][:, kc, :], rhs=wob[:, kc, dg * 512:(dg + 1) * 512],
                                                                 start=(kc == 0), stop=(kc == KC - 1)), reads=[r_mx[p], r_wobk[kc]], writes=[rb[dg]])
            for dg in range(4):
                ds = slice(dg * 512, (dg + 1) * 512)
                P.op("dve", lambda p=p, dg=dg, ds=ds: V.tensor_tensor(out=hres[p][:, ds], in0=bank[dg][:, :], in1=g1b[:, ds], op=ALU.mult),
                     reads=[r_bc], writes=[rb[dg], r_hres[p]])
                P.op("pool", lambda p=p, ds=ds: G_.tensor_tensor(out=hres[p][:, ds], in0=hres[p][:, ds], in1=xt[p][:, ds], op=ALU.add),
                     reads=[r_xt[p], r_hres[p]], writes=[r_hres[p]])
            f = P.op("sp", lambda p=p, tsl=tsl: nc.sync.dma_start(out=hres_o[tsl, :], in_=hres[p][:]), reads=[r_hres[p]], dma=r_hres[p])
            finals.append(f)
            P.op("pool", lambda p=p: G_.tensor_tensor(out=h2f[:], in0=hres[p][:], in1=hres[p][:], op=ALU.mult), reads=[r_hres[p]], writes=[r_h2f])
            P.op("dve", lambda smt=smt: V.tensor_reduce(out=smt[:, 0:1], in_=h2f[:], axis=AX.X, op=ALU.add), reads=[r_h2f], writes=[r_smt])
            P.op("act", lambda smt=smt: S.activation(out=smt[:, 1:2], in_=smt[:, 0:1], func=AF.Ln, bias=EPS, scale=1.0 / D), reads=[r_smt], writes=[r_smt])
            P.op("act", lambda smt=smt: S.activation(out=smt[:, 2:3], in_=smt[:, 1:2], func=AF.Exp, scale=-0.5), reads=[r_smt], writes=[r_smt])
            P.op("dve", lambda p=p, smt=smt: V.scalar_tensor_tensor(out=h2f[:], in0=hres[p][:], scalar=smt[:, 2:3], in1=gs2b, op0=ALU.mult, op1=ALU.mult),
                 reads=[r_hres[p], r_smt, r_bc, r_h2f], writes=[r_h2f])
            P.op("pool", lambda: G_.tensor_tensor(out=h2f[:], in0=h2f[:], in1=sh2b[:], op=ALU.add), reads=[r_sh2, r_h2f], writes=[r_h2f])
            P.op("act", lambda i=i: S.copy(out=h2b[:, i, :], in_=h2f[:]), reads=[r_h2f], writes=[r_h2b[i]])
            for q4 in range(4):
                tb = 4 + (q4 % 2)
                for k4 in range(4):
                    kc = q4 * 4 + k4
                    P.op("pe", lambda tb=tb, k4=k4, kc=kc: T.transpose(out=bank[tb][:, k4 * 128:(k4 + 1) * 128], in_=h2f[:, kc * 128:(kc + 1) * 128], identity=ID32),
                         reads=[r_h2f, r_c32], writes=[rb[tb]])
                eng = "act" if q4 % 2 == 0 else "dve"
                if eng == "act":
                    P.op("act", lambda tb=tb, q4=q4: S.copy(out=h2T[:, q4 * 4:(q4 + 1) * 4, :], in_=bank[tb][:, :].rearrange("p (a b) -> p a b", a=4)),
                         writes=[rb[tb], r_h2T])
                else:
                    P.op("dve", lambda tb=tb, q4=q4: V.tensor_copy(out=h2T[:, q4 * 4:(q4 + 1) * 4, :], in_=bank[tb][:, :].rearrange("p (a b) -> p a b", a=4)),
                         writes=[rb[tb], r_h2T])
            for kc in range(KC):
                P.op("pe", lambda kc=kc: T.matmul(bank[6][:, 0:NE], lhsT=h2T[:, kc, :], rhs=wr[:, kc, :], start=(kc == 0), stop=(kc == KC - 1)),
                     reads=[r_h2T, r_wr], writes=[rb[6]])
            P.op("dve", lambda lgt=lgt: V.tensor_tensor(out=lgt[:, 0:NE], in0=bank[6][:, 0:NE], in1=brb, op=ALU.add), reads=[r_bc], writes=[rb[6], r_lgt])
            P.op("dve", lambda lgt=lgt, smt=smt: V.max(out=smt[:, 8:16], in_=lgt[:, 0:NE]), reads=[r_lgt], writes=[r_smt])
            P.op("dve", lambda lgt=lgt, smt=smt: V.tensor_scalar(out=lgt[:, NE:2 * NE], in0=lgt[:, 0:NE], scalar1=smt[:, 11:12], scalar2=None, op0=ALU.is_ge),
                 reads=[r_lgt, r_smt], writes=[r_lgt])
            P.op("dve", lambda smt=smt: V.tensor_scalar(out=smt[:, 3:4], in0=smt[:, 8:9], scalar1=-1.0, scalar2=None, op0=ALU.mult), reads=[r_smt], writes=[r_smt])
            P.op("act", lambda lgt=lgt, smt=smt: S.activation(out=lgt[:, 2 * NE:3 * NE], in_=lgt[:, 0:NE], func=AF.Exp, bias=smt[:, 3:4], scale=1.0),
                 reads=[r_lgt, r_smt], writes=[r_lgt])
            P.op("dve", lambda lgt=lgt: V.tensor_tensor(out=lgt[:, 2 * NE:3 * NE], in0=lgt[:, 2 * NE:3 * NE], in1=lgt[:, NE:2 * NE], op=ALU.mult), reads=[r_lgt], writes=[r_lgt])
            P.op("dve", lambda lgt=lgt, smt=smt: V.tensor_reduce(out=smt[:, 4:5], in_=lgt[:, 2 * NE:3 * NE], axis=AX.X, op=ALU.add), reads=[r_lgt], writes=[r_smt])
            P.op("dve", lambda smt=smt: V.reciprocal(out=smt[:, 5:6], in_=smt[:, 4:5]), reads=[r_smt], writes=[r_smt])
            P.op("dve", lambda lgt=lgt, smt=smt: V.tensor_scalar(out=lgt[:, 3 * NE:4 * NE], in0=lgt[:, 2 * NE:3 * NE], scalar1=smt[:, 5:6], scalar2=None, op0=ALU.mult),
                 reads=[r_lgt, r_smt], writes=[r_lgt])
            f = P.op("sp", lambda lgt=lgt, tsl=tsl: nc.sync.dma_start(out=G_o[tsl, :], in_=lgt[:, 3 * NE:4 * NE]), reads=[r_lgt], dma=r_lgt)
            finals.append(f)
            P.op("pool", lambda lgt=lgt, i=i: G_.tensor_copy(out=maskb[:, i, :], in_=lgt[:, NE:2 * NE]), reads=[r_lgt], writes=[r_maskb])
            rank_ops(P, nc, bank[6], rb[6], maskb, r_maskb, i, cb, r_cb, lgt[:, NE:2 * NE], r_lgt, rk[:, i, :], r_rk)
        gbanks = [0, 1, 2, 3, 7, 4, 5]
        gi = 0
        ncc = (CAP + 511) // 512
        ccw = CAP // ncc
        P.op("dve", lambda: V.memset(sel[0][:, 0, 0:2], 0.0), writes=r_wobk + r_sel + r_xte)
        for e in range(NE):
            q = e % 2
            for i in range(NTL):
                P.op("dve", lambda q=q, i=i, e=e: V.tensor_scalar(out=sel[q][:, i, :], in0=IOTA, scalar1=rk[:, i, e:e + 1], scalar2=None, op0=ALU.is_equal),
                     reads=[r_c32, r_rk], writes=[r_sel[q]])
            for fc in range(KC):
                for cc in range(ncc):
                    cs = slice(cc * ccw, (cc + 1) * ccw)
                    gbk = gbanks[gi % len(gbanks)]; gi += 1
                    for i in range(NTL):
                        P.op("pe", lambda gbk=gbk, q=q, i=i, fc=fc, cs=cs: T.matmul(bank[gbk][:, 0:ccw], lhsT=h2b[:, i, fc * 128:(fc + 1) * 128], rhs=sel[q][:, i, cs],
                                                                                start=(i == 0), stop=(i == NTL - 1)), reads=[r_h2b[i], r_sel[q]], writes=[rb[gbk]])
                    if gi % 2 == 0:
                        P.op("act", lambda gbk=gbk, q=q, fc=fc, cs=cs: S.copy(out=xte[q][:, fc, cs], in_=bank[gbk][:, 0:ccw]), writes=[rb[gbk], r_xte[q]])
                    else:
                        P.op("dve", lambda gbk=gbk, q=q, fc=fc, cs=cs: V.tensor_copy(out=xte[q][:, fc, cs], in_=bank[gbk][:, 0:ccw]), writes=[rb[gbk], r_xte[q]])
            f = P.op("sp", lambda q=q, e=e: nc.sync.dma_start(out=XT_o[e].rearrange("(fc p) s -> p fc s", p=128), in_=xte[q][:]), reads=[r_xte[q]], dma=r_xte[q])
            finals.append(f)
        P.emit(final_ops=finals)
    return nc


def l2_inputs(mixT_g, x_g, w_out, g1, norm_ffn, sc2, sh2, w_router, b_router):
    bcst = lambda v: np.broadcast_to(v[None, :], (128, v.shape[0]))
    bc = np.ascontiguousarray(np.concatenate([bcst(g1), bcst(norm_ffn), bcst(sc2), bcst(b_router)], axis=1), dtype=np.float32)
    sl = np.tril(np.ones((128, 128), np.float32), -1).T
    cf32 = np.ascontiguousarray(np.concatenate([np.eye(128, dtype=np.float32), bcst(np.arange(640, dtype=np.float32))], axis=1))
    cbf = np.ascontiguousarray(np.concatenate([np.eye(128, dtype=np.float32), sl, np.ones((128, 128), np.float32)], axis=1)).astype(ml_dtypes.bfloat16)
    return {"mixT": mixT_g, "x": x_g, "w_out": w_out, "bc": bc, "sh2": np.ascontiguousarray(bcst(sh2), dtype=np.float32),
            "w_r": w_router, "cf32": cf32, "cbf": cbf}


EPC = 4


def build_l3(NS, NCH=1):
    nc = new_nc()
    NSC = NS // NCH
    NSG = (NSC + 511) // 512
    SGW = NSC // NSG
    NSB = NSC // 128
    xtd = nc.dram_tensor("XT", [EPC, D, NS], BF16, kind="ExternalInput").ap()
    wugd = nc.dram_tensor("w_ug", [EPC, D, 2 * D], F32, kind="ExternalInput").ap()
    bugd = nc.dram_tensor("b_ug", [EPC, 128, 32], F32, kind="ExternalInput").ap()
    wdd = nc.dram_tensor("w_d", [EPC, D, D], F32, kind="ExternalInput").ap()
    bdd = nc.dram_tensor("b_d", [EPC, 128, D], F32, kind="ExternalInput").ap()
    yd = nc.dram_tensor("y", [EPC, NS, D], BF16, kind="ExternalOutput").ap()
    cug = nc.dram_tensor("cache_ug", [32, 128, KC * 128], BF16).ap() if NCH > 1 else None
    cdn = nc.dram_tensor("cache_dn", [KC, 128, D], BF16).ap() if NCH > 1 else None
    V, S, G_, T = nc.vector, nc.scalar, nc.gpsimd, nc.tensor
    with contextlib.ExitStack() as st:
        P = Prog(nc, st)
        R = P.res
        bank = [P.ps(f"bank{i}", [128, 512]) for i in range(8)]
        rb = [R(f"bank{i}") for i in range(8)]
        xs = P.sb("xs", [128, KC, NSC], BF16); r_xs = R()
        actT = P.sb("actT", [128, KC, NSC], BF16); r_actT = [R() for _ in range(KC)]
        wd = P.sb("wd", [128, KC, D], BF16); r_wd = [R() for _ in range(KC)]
        NW = 3
        wg = [P.sb(f"wg{i}", [128, KC, 128], BF16) for i in range(NW)]; r_wg = [R() for _ in range(NW)]
        wu = [P.sb(f"wu{i}", [128, KC, 128], BF16) for i in range(NW)]; r_wu = [R() for _ in range(NW)]
        bug = P.sb("bug", [128, 32], F32); r_bug = R()
        bdb = P.sb("bdb", [128, D], F32); r_bdb = R()
        gcl = [P.sb(f"gcl{i}", [128, SGW], F32) for i in range(2)]; r_gcl = [R(), R()]
        sig = [P.sb(f"sig{i}", [128, SGW], F32) for i in range(2)]; r_sig = [R(), R()]
        ucl = [P.sb(f"ucl{i}", [128, SGW], F32) for i in range(2)]; r_ucl = [R(), R()]
        yst = [P.sb(f"yst{i}", [128, D], BF16) for i in range(2)]; r_yst = [R(), R()]
        r_cug = [R() for _ in range(32)]
        r_cdn = [R() for _ in range(KC)]
        r_wgst = [R() for _ in range(NW)]; r_wust = [R() for _ in range(NW)]; r_wdst = [R() for _ in range(KC)]
        finals = []
        wi = 0
        ei = 0
        for v in range(EPC * NCH):
            e, ch = v // NCH, v % NCH
            so = ch * NSC
            P.op("sp", lambda e=e, so=so: nc.sync.dma_start(out=xs[:], in_=xtd[e][:, so:so + NSC].rearrange("(kc p) s -> p kc s", p=128)), writes=[r_xs], dma=r_xs)
            if ch == 0:
                P.op("sp", lambda e=e: nc.sync.dma_start(out=bug[:], in_=bugd[e]), writes=[r_bug], dma=r_bug)
                P.op("sp", lambda e=e: nc.sync.dma_start(out=bdb[:], in_=bdd[e]), writes=[r_bdb], dma=r_bdb)
            wv = wugd[e].rearrange("(kc p) n -> p kc n", p=128)
            for c in range(KC):
                w = wi % NW; wi += 1
                if ch == 0:
                    P.op("pool", lambda w=w, c=c, wv=wv: G_.dma_start(out=wg[w][:], in_=wv[:, :, c * 128:(c + 1) * 128]), writes=[r_wg[w]], dma=r_wg[w])
                    P.op("pool", lambda w=w, c=c, wv=wv: G_.dma_start(out=wu[w][:], in_=wv[:, :, D + c * 128:D + (c + 1) * 128]), writes=[r_wu[w]], dma=r_wu[w])
                    P.op("pool", lambda e=e, c=c: G_.dma_start(out=wd[:, c, :], in_=wdd[e, c * 128:(c + 1) * 128, :]), writes=[r_wd[c]], dma=r_wd[c])
                    if NCH > 1:
                        P.op("act", lambda w=w, c=c: S.dma_start(out=cug[c], in_=wg[w][:].rearrange("p a b -> p (a b)")), reads=[r_wg[w]], writes=[r_cug[c]], dma=r_wgst[w])
                        P.op("act", lambda w=w, c=c: S.dma_start(out=cug[16 + c], in_=wu[w][:].rearrange("p a b -> p (a b)")), reads=[r_wu[w]], writes=[r_cug[16 + c]], dma=r_wust[w])
                        P.op("act", lambda c=c: S.dma_start(out=cdn[c], in_=wd[:, c, :]), reads=[r_wd[c]], writes=[r_cdn[c]], dma=r_wdst[c])
                else:
                    P.op("sp", lambda w=w, c=c: nc.sync.dma_start(out=wg[w][:].rearrange("p a b -> p (a b)"), in_=cug[c]), reads=[r_cug[c]], writes=[r_wg[w]], dma=r_wg[w])
                    P.op("sp", lambda w=w, c=c: nc.sync.dma_start(out=wu[w][:].rearrange("p a b -> p (a b)"), in_=cug[16 + c]), reads=[r_cug[16 + c]], writes=[r_wu[w]], dma=r_wu[w])
                    P.op("sp", lambda c=c: nc.sync.dma_start(out=wd[:, c, :], in_=cdn[c]), reads=[r_cdn[c]], writes=[r_wd[c]], dma=r_wd[c])
                for sg in range(NSG):
                    ss = slice(sg * SGW, (sg + 1) * SGW)
                    bg_, bu_ = sg * 2, sg * 2 + 1
                    for kc in range(KC):
                        P.op("pe", lambda w=w, kc=kc, ss=ss, bg_=bg_: T.matmul(bank[bg_][:, 0:SGW], lhsT=wg[w][:, kc, :], rhs=xs[:, kc, ss], start=(kc == 0), stop=(kc == KC - 1)),
                             reads=[r_wg[w], r_xs], writes=[rb[bg_]])
                    for kc in range(KC):
                        P.op("pe", lambda w=w, kc=kc, ss=ss, bu_=bu_: T.matmul(bank[bu_][:, 0:SGW], lhsT=wu[w][:, kc, :], rhs=xs[:, kc, ss], start=(kc == 0), stop=(kc == KC - 1)),
                             reads=[r_wu[w], r_xs], writes=[rb[bu_]])
                    q = ei % 2; ei += 1
                    P.op("dve", lambda q=q, c=c, bg_=bg_: V.tensor_scalar(out=gcl[q][:], in0=bank[bg_][:, 0:SGW], scalar1=bug[:, c:c + 1], scalar2=7.0, op0=ALU.add, op1=ALU.min),
                         reads=[r_bug], writes=[rb[bg_], r_gcl[q]])
                    P.op("dve", lambda q=q, c=c, bu_=bu_: V.tensor_scalar(out=ucl[q][:], in0=bank[bu_][:, 0:SGW], scalar1=bug[:, 16 + c:17 + c], scalar2=7.0, op0=ALU.add, op1=ALU.min),
                         reads=[r_bug], writes=[rb[bu_], r_ucl[q]])
                    P.op("act", lambda q=q: S.activation(out=sig[q][:], in_=gcl[q][:], func=AF.Sigmoid, scale=1.702), reads=[r_gcl[q]], writes=[r_sig[q]])
                    P.op("dve", lambda q=q: V.tensor_scalar(out=ucl[q][:], in0=ucl[q][:], scalar1=-7.0, scalar2=1.0, op0=ALU.max, op1=ALU.add), reads=[r_ucl[q]], writes=[r_ucl[q]])
                    P.op("dve", lambda q=q: V.tensor_tensor(out=gcl[q][:], in0=gcl[q][:], in1=sig[q][:], op=ALU.mult), reads=[r_gcl[q], r_sig[q]], writes=[r_gcl[q]])
                    P.op("dve", lambda q=q, c=c, ss=ss: V.tensor_tensor(out=actT[:, c, ss], in0=gcl[q][:], in1=ucl[q][:], op=ALU.mult), reads=[r_gcl[q], r_ucl[q]], writes=[r_actT[c]])
            for sb in range(NSB):
                yq = sb % 2
                for dg in range(4):
                    bk = 6 + (dg % 2)
                    for kc in range(KC):
                        P.op("pe", lambda sb=sb, dg=dg, kc=kc, bk=bk: T.matmul(bank[bk][:, :], lhsT=actT[:, kc, sb * 128:(sb + 1) * 128], rhs=wd[:, kc, dg * 512:(dg + 1) * 512],
                                                                             start=(kc == 0), stop=(kc == KC - 1)), reads=[r_actT[kc], r_wd[kc]], writes=[rb[bk]])
                    P.op("dve", lambda yq=yq, dg=dg, bk=bk: V.tensor_tensor(out=yst[yq][:, dg * 512:(dg + 1) * 512], in0=bank[bk][:, :], in1=bdb[:, dg * 512:(dg + 1) * 512], op=ALU.add),
                         reads=[r_bdb], writes=[rb[bk], r_yst[yq]])
                f = P.op("sp", lambda e=e, sb=sb, yq=yq, so=so: nc.sync.dma_start(out=yd[e, so + sb * 128:so + (sb + 1) * 128, :], in_=yst[yq][:]), reads=[r_yst[yq]], dma=r_yst[yq])
                finals.append(f)
        P.emit(final_ops=finals[-2:])
    return nc


def build_l4(TL, CAP):
    NTL = TL // 128
    nc = new_nc()
    NCK = (CAP + 127) // 128
    CW = CAP // NCK
    yd = nc.dram_tensor("yb", [NE, CAP, D], BF16, kind="ExternalInput").ap()
    Gd = nc.dram_tensor("G", [TL, NE], F32, kind="ExternalInput").ap()
    hrd = nc.dram_tensor("hres", [TL, D], F32, kind="ExternalInput").ap()
    bcd = nc.dram_tensor("bc", [128, 4 * D], F32, kind="ExternalInput").ap()
    cf32d = nc.dram_tensor("cf32", [128, 768], F32, kind="ExternalInput").ap()
    cbfd = nc.dram_tensor("cbf", [128, 384], BF16, kind="ExternalInput").ap()
    outd = nc.dram_tensor("out", [TL, D], F32, kind="ExternalOutput").ap()
    V, S, G_, T = nc.vector, nc.scalar, nc.gpsimd, nc.tensor
    with contextlib.ExitStack() as st:
        P = Prog(nc, st)
        R = P.res
        bank = [P.ps(f"bank{i}", [128, 512]) for i in range(8)]
        rb = [R(f"bank{i}") for i in range(8)]
        bc = P.sb("bct", [128, 4 * D], F32); r_bc = R()
        c32 = P.sb("c32", [128, 768], F32); r_c32 = R()
        cb = P.sb("cb", [128, 384], BF16); r_cb = R()
        IOTA = c32[:, 128:128 + CAP]
        Gt = P.sb("Gt", [128, NTL, NE], F32); r_G = R()
        mask32 = P.sb("mask32", [128, NTL, NE], F32); r_mask = R()
        maskb = P.sb("maskb", [128, NTL, NE], BF16); r_maskb = R()
        rk = P.sb("rk", [128, NTL, NE], F32); r_rk = R()
        acc = P.sb("acc", [128, NTL, D], F32); r_acc = [R() for _ in range(NTL)]
        yb = [P.sb(f"yb{i}", [128, NCK, D], BF16) for i in range(2)]; r_yb = [R(), R()]
        selw = [P.sb(f"selw{i}", [128, CAP], BF16) for i in range(2)]; r_selw = [R(), R()]
        swT = [P.sb(f"swT{i}", [128, NCK, 128], BF16) for i in range(2)]; r_swT = [R(), R()]
        hr = P.sb("hr", [128, D], F32); r_hr = R()
        tmp = P.sb("tmp", [128, D], F32); r_tmp = R()
        ot = [P.sb(f"ot{i}", [128, D], F32) for i in range(2)]; r_ot = [R(), R()]
        sm = [P.sb(f"sm{i}", [128, 8], F32) for i in range(2)]; r_sm = [R(), R()]
        P.op("sp", lambda: nc.sync.dma_start(out=bc[:], in_=bcd), writes=[r_bc], dma=r_bc)
        P.op("sp", lambda: nc.sync.dma_start(out=c32[:], in_=cf32d), writes=[r_c32], dma=r_c32)
        P.op("sp", lambda: nc.sync.dma_start(out=cb[:], in_=cbfd), writes=[r_cb], dma=r_cb)
        P.op("sp", lambda: nc.sync.dma_start(out=Gt[:], in_=Gd.rearrange("(i p) e -> p i e", p=128)), writes=[r_G], dma=r_G)
        P.op("pool", lambda: G_.memset(acc[:], 0.0), writes=r_acc)
        P.op("dve", lambda: V.scalar_tensor_tensor(out=bc[:, D:2 * D], in0=bc[:, 2 * D:3 * D], scalar=1.0, in1=bc[:, D:2 * D], op0=ALU.add, op1=ALU.mult),
             reads=[r_bc], writes=[r_bc])
        g2b, gsfb, shfb = bc[:, 0:D], bc[:, D:2 * D], bc[:, 3 * D:4 * D]
        P.op("dve", lambda: V.tensor_scalar(out=mask32[:], in0=Gt[:], scalar1=0.0, scalar2=None, op0=ALU.is_gt), reads=[r_G], writes=[r_mask])
        P.op("pool", lambda: G_.tensor_copy(out=maskb[:], in_=mask32[:]), reads=[r_mask], writes=[r_maskb])
        for i in range(NTL):
            rank_ops(P, nc, bank[6], rb[6], maskb, r_maskb, i, cb, r_cb, mask32[:, i, :], r_mask, rk[:, i, :], r_rk, lo=0)
        ui = 0
        for e in range(NE):
            k = e % 2
            if CAP % 128 == 0:
                P.op("sp", lambda k=k, e=e: nc.sync.dma_start(out=yb[k][:], in_=yd[e].rearrange("(ck p) d -> p ck d", p=128)), writes=[r_yb[k]], dma=r_yb[k])
            else:
                P.op("sp", lambda k=k, e=e: nc.sync.dma_start(out=yb[k][0:CW, 0, :], in_=yd[e]), writes=[r_yb[k]], dma=r_yb[k])
            for i in range(NTL):
                q = ui % 2; ui += 1
                P.op("dve", lambda q=q, i=i, e=e: V.tensor_scalar(out=selw[q][:], in0=IOTA, scalar1=rk[:, i, e:e + 1], scalar2=Gt[:, i, e:e + 1], op0=ALU.is_equal, op1=ALU.mult),
                     reads=[r_c32, r_rk, r_G], writes=[r_selw[q]])
                tbk = 4 + q
                tview = bank[tbk][0:CW, :].bitcast(BF16)
                for ck in range(NCK):
                    P.op("pe", lambda q=q, ck=ck, tview=tview: T.transpose(out=tview[:, ck * 128:(ck + 1) * 128], in_=selw[q][:, ck * CW:(ck + 1) * CW], identity=cb[:, 0:128]),
                         reads=[r_selw[q], r_cb], writes=[rb[tbk]])
                P.op("act", lambda q=q, tview=tview: S.copy(out=swT[q][0:CW, :, :], in_=tview[:, 0:NCK * 128].rearrange("p (a b) -> p a b", a=NCK)), writes=[rb[tbk], r_swT[q]])
                for dg in range(4):
                    for ck in range(NCK):
                        P.op("pe", lambda k=k, q=q, ck=ck, dg=dg: T.matmul(bank[dg][:, :], lhsT=swT[q][0:CW, ck, :], rhs=yb[k][0:CW, ck, dg * 512:(dg + 1) * 512],
                                                                         start=(ck == 0), stop=(ck == NCK - 1)), reads=[r_swT[q], r_yb[k]], writes=[rb[dg]])
                    ds = slice(dg * 512, (dg + 1) * 512)
                    P.op("dve", lambda i=i, dg=dg, ds=ds: V.tensor_tensor(out=acc[:, i, ds], in0=bank[dg][:, :], in1=acc[:, i, ds], op=ALU.add),
                         reads=[r_acc[i]], writes=[rb[dg], r_acc[i]])
        finals = []
        for i in range(NTL):
            p = i % 2
            tsl = slice(i * 128, (i + 1) * 128)
            smt, r_smt = sm[p], r_sm[p]
            P.op("sp", lambda tsl=tsl: nc.sync.dma_start(out=hr[:], in_=hrd[tsl, :]), writes=[r_hr], dma=r_hr)
            P.op("dve", lambda i=i: V.tensor_tensor(out=acc[:, i, :], in0=acc[:, i, :], in1=g2b, op=ALU.mult), reads=[r_bc, r_acc[i]], writes=[r_acc[i]])
            P.op("pool", lambda i=i: G_.tensor_tensor(out=acc[:, i, :], in0=acc[:, i, :], in1=hr[:], op=ALU.add), reads=[r_hr, r_acc[i]], writes=[r_acc[i]])
            P.op("pool", lambda i=i: G_.tensor_tensor(out=tmp[:], in0=acc[:, i, :], in1=acc[:, i, :], op=ALU.mult), reads=[r_acc[i]], writes=[r_tmp])
            P.op("dve", lambda smt=smt: V.tensor_reduce(out=smt[:, 0:1], in_=tmp[:], axis=AX.X, op=ALU.add), reads=[r_tmp], writes=[r_smt])
            P.op("act", lambda smt=smt: S.activation(out=smt[:, 1:2], in_=smt[:, 0:1], func=AF.Ln, bias=EPS, scale=1.0 / D), reads=[r_smt], writes=[r_smt])
            P.op("act", lambda smt=smt: S.activation(out=smt[:, 2:3], in_=smt[:, 1:2], func=AF.Exp, scale=-0.5), reads=[r_smt], writes=[r_smt])
            P.op("dve", lambda smt=smt, i=i: V.scalar_tensor_tensor(out=tmp[:], in0=acc[:, i, :], scalar=smt[:, 2:3], in1=gsfb, op0=ALU.mult, op1=ALU.mult),
                 reads=[r_acc[i], r_smt, r_bc, r_tmp], writes=[r_tmp])
            P.op("pool", lambda p=p: G_.tensor_tensor(out=ot[p][:], in0=tmp[:], in1=shfb, op=ALU.add), reads=[r_tmp, r_bc], writes=[r_ot[p]])
            f = P.op("sp", lambda p=p, tsl=tsl: nc.sync.dma_start(out=outd[tsl, :], in_=ot[p][:]), reads=[r_ot[p]], dma=r_ot[p])
            finals.append(f)
        P.emit(final_ops=finals[-2:])
    return nc


TL_FULL = SEQ // NCORES
CAP_FULL = 640
NCH_FULL = 5


_DBG = {}


def _bcst(v):
    return np.broadcast_to(np.asarray(v)[None, :], (128, v.shape[0]))


def _run(nc, in_maps):
    return run_bass_kernel_spmd(nc, in_maps, core_ids=list(range(NCORES))).results


def kernel(x, c, w_ada, b_ada, norm_mix, w_in, b_i, b_f, fox_b_f, fox_q_norm, fox_k_norm,
           mlstm_out_norm, fox_out_norm, w_out, norm_ffn, w_router, b_router, w_up_gate,
           b_up_gate, w_down, b_down, w_ada_final, b_ada_final, norm_final):
    f32 = lambda a: np.asarray(a, dtype=np.float32)
    x = f32(x); c = f32(c)
    seq = x.shape[1]
    TL = seq // NCORES
    CAP = CAP_FULL
    mod = run_l0(c, f32(w_ada), f32(b_ada), f32(w_ada_final), f32(b_ada_final))
    sh1, sc1, g1, sh2, sc2, g2 = [mod[i * D:(i + 1) * D] for i in range(6)]
    shf, scf = mod[6 * D:7 * D], mod[7 * D:8 * D]
    xT = np.ascontiguousarray(x[0].T)
    w_in0 = f32(w_in)[0]
    in1 = [l1_inputs(j, xT, w_in0, f32(norm_mix)[0], sc1, sh1, f32(b_i)[0], f32(b_f)[0], f32(fox_b_f)[0],
                     f32(fox_q_norm)[0], f32(fox_k_norm)[0], f32(fox_out_norm)[0], f32(mlstm_out_norm)[0]) for j in range(NCORES)]
    r1 = _run(build_l1(seq), in1)
    del in1, xT
    mix = np.empty((seq, D), dtype=ml_dtypes.bfloat16)
    for j in range(NCORES):
        h, half = j // 2, j % 2
        o = np.asarray(r1[j]["hout"])
        mix[:, 256 * h + 128 * half:256 * h + 128 * half + 128] = o[:, 0:128]
        mix[:, 1024 + 128 * j:1024 + 128 * j + 128] = o[:, 128:256]
    mixT = np.ascontiguousarray(mix.T)
    w_out0 = f32(w_out)[0]
    in2 = [l2_inputs(np.ascontiguousarray(mixT[:, g * TL:(g + 1) * TL]), np.ascontiguousarray(x[0, g * TL:(g + 1) * TL]), w_out0, g1,
                     f32(norm_ffn)[0], sc2, sh2, f32(w_router)[0], f32(b_router)[0]) for g in range(NCORES)]
    r2 = _run(build_l2(TL, CAP), in2)
    cf32, cbf = in2[0]["cf32"], in2[0]["cbf"]
    del in2
    in3 = []
    for cidx in range(NCORES):
        es = list(range(EPC * cidx, EPC * cidx + EPC))
        XT = np.ascontiguousarray(np.stack([np.concatenate([np.asarray(r2[g]["XT"])[e] for g in range(NCORES)], axis=1) for e in es]))
        in3.append({"XT": XT, "w_ug": np.ascontiguousarray(f32(w_up_gate)[0, es[0]:es[-1] + 1]),
                    "b_ug": np.ascontiguousarray(np.stack([f32(b_up_gate)[0, e].reshape(32, 128).T for e in es])),
                    "w_d": np.ascontiguousarray(f32(w_down)[0, es[0]:es[-1] + 1]),
                    "b_d": np.ascontiguousarray(np.stack([_bcst(f32(b_down)[0, e]) for e in es]))})
    r3 = _run(build_l3(NCORES * CAP, NCH_FULL), in3)
    del in3
    bc4 = np.ascontiguousarray(np.concatenate([_bcst(g2), _bcst(f32(norm_final)), _bcst(scf), _bcst(shf)], axis=1), dtype=np.float32)
    in4 = []
    for g in range(NCORES):
        yb = np.ascontiguousarray(np.stack([np.asarray(r3[e // EPC]["y"])[e % EPC][g * CAP:(g + 1) * CAP] for e in range(NE)]))
        in4.append({"yb": yb, "G": np.asarray(r2[g]["G"]), "hres": np.asarray(r2[g]["hres"]), "bc": bc4, "cf32": cf32, "cbf": cbf})
    r4 = _run(build_l4(TL, CAP), in4)
    out = np.concatenate([np.asarray(r["out"]) for r in r4], axis=0)[None]
    _DBG.update(mod=mod, mix=mix, G=np.concatenate([np.asarray(r2[g]["G"]) for g in range(NCORES)]), hres=np.concatenate([np.asarray(r2[g]["hres"]) for g in range(NCORES)]))
    return out.astype(np.float32)
```

```python
import contextlib
import numpy as np
import ml_dtypes
import concourse.bass as bass
import concourse.mybir as mybir
from concourse.bass_utils import run_bass_kernel_spmd

F32 = mybir.dt.float32
BF16 = mybir.dt.bfloat16
ALU = mybir.AluOpType
AF = mybir.ActivationFunctionType
AX = mybir.AxisListType
NCORES = 8

D = 2048
KC = D // 128
SEQ = 8192
EPS = 1e-6


class Res:
    __slots__ = ("name", "w", "r", "sem", "cnt")

    def __init__(self, name):
        self.name = name
        self.w = None
        self.r = []
        self.sem = None
        self.cnt = 0


class Prog:
    ENG = ("pe", "act", "dve", "pool", "sp")

    def __init__(self, nc, stack):
        self.nc = nc
        self.stack = stack
        self.ops = []
        self.e = {"pe": nc.tensor, "act": nc.scalar, "dve": nc.vector, "pool": nc.gpsimd, "sp": nc.sync}
        self.nres = 0

    def res(self, name=None):
        self.nres += 1
        return Res(name or f"r{self.nres}")

    def sb(self, name, shape, dt):
        t = self.stack.enter_context(self.nc.sbuf_tensor(name, list(shape), dt))
        return t

    def ps(self, name, shape, dt=F32):
        return self.stack.enter_context(self.nc.psum_tensor(name, list(shape), dt))

    deferred = None

    def op(self, eng, fn, reads=(), writes=(), dma=None):
        if self.deferred is not None:
            self.deferred.append((eng, fn, tuple(reads), tuple(writes), dma))
            return None
        i = len(self.ops)
        deps = set()
        for r in reads:
            if r.w is not None:
                deps.add(r.w)
        for w in writes:
            if w.w is not None:
                deps.add(w.w)
            deps.update(w.r)
        for r in reads:
            r.r.append(i)
        for w in writes:
            w.w = i
            w.r = []
        self.ops.append(dict(eng=eng, fn=fn, deps=deps, dma=dma, wset=set(id(w) for w in writes),
                             rset=set(id(r) for r in reads)))
        return i

    def emit(self, final_ops=()):
        nc = self.nc
        ops = self.ops
        def stream(o):
            return ("dma", id(o["dma"])) if o["dma"] is not None else o["eng"]
        seen = {e: {} for e in self.ENG}
        pos = {}
        kept = []
        signal = set()
        for i, o in enumerate(ops):
            k = {}
            for d in o["deps"]:
                od = ops[d]
                sd = stream(od)
                if sd == o["eng"] and od["dma"] is None:
                    if o["eng"] == "pe":
                        continue
                    if not (od["wset"] & o["rset"]):
                        continue
                k[sd] = max(k.get(sd, -1), d)
            kk = []
            for sd, d in k.items():
                if seen[o["eng"]].get(sd, -1) >= d:
                    continue
                seen[o["eng"]][sd] = d
                kk.append((sd, d))
                signal.add(d)
            kept.append(kk)
        for d in final_ops:
            signal.add(d)
        for i, o in enumerate(ops):
            if o["dma"] is not None:
                signal.add(i)
        sems = {}
        def sem_of(sd):
            if sd not in sems:
                sems[sd] = self.stack.enter_context(nc.semaphore(f"s{len(sems)}"))
            return sems[sd]
        val = {}
        cur = {}
        for i, o in enumerate(ops):
            if i in signal:
                sd = stream(o)
                inc = 16 if o["dma"] is not None else 1
                cur[sd] = cur.get(sd, 0) + inc
                val[i] = cur[sd]
        for i, o in enumerate(ops):
            eng = self.e[o["eng"]]
            for sd, d in kept[i]:
                eng.wait_ge(sem_of(sd), val[d])
            inst = o["fn"]()
            if i in signal:
                sd = stream(o)
                inst.then_inc(sem_of(sd), 16 if o["dma"] is not None else 1)
        for d in final_ops:
            self.e["sp"].wait_ge(sem_of(stream(ops[d])), val[d])


def new_nc():
    return bass.Bass("TRN2", target_bir_lowering=False)


def build_l0():
    nc = new_nc()
    NW = 2048
    c_in = nc.dram_tensor("c", [128, KC], F32, kind="ExternalInput").ap()
    w_in = nc.dram_tensor("w", [D, NW], F32, kind="ExternalInput").ap()
    b_in = nc.dram_tensor("b", [1, NW], F32, kind="ExternalInput").ap()
    out = nc.dram_tensor("mod", [1, NW], F32, kind="ExternalOutput").ap()
    with contextlib.ExitStack() as st:
        P = Prog(nc, st)
        ct = P.sb("ct", [128, KC], F32); r_ct = P.res()
        ca = P.sb("ca", [128, KC], F32); r_ca = P.res()
        bt = P.sb("bt", [1, NW], F32); r_bt = P.res()
        ot = P.sb("ot", [1, NW], F32); r_ot = P.res()
        wt = [P.sb(f"wt{i}", [128, KC, 512], F32) for i in range(2)]
        r_wt = [P.res() for _ in range(2)]
        pst = [P.ps(f"ps{i}", [1, 512]) for i in range(2)]
        r_ps = [P.res() for _ in range(2)]
        P.op("sp", lambda: nc.sync.dma_start(out=ct[:], in_=c_in), writes=[r_ct], dma=r_ct)
        P.op("sp", lambda: nc.sync.dma_start(out=bt[:], in_=b_in), writes=[r_bt], dma=r_bt)
        P.op("act", lambda: nc.scalar.activation(out=ca[:], in_=ct[:], func=AF.Silu), reads=[r_ct], writes=[r_ca])
        wv = w_in.rearrange("(kc p) n -> p kc n", p=128)
        for g in range(4):
            b = g % 2
            P.op("sp", lambda g=g, b=b: nc.sync.dma_start(out=wt[b][:], in_=wv[:, :, g * 512:(g + 1) * 512]),
                 writes=[r_wt[b]], dma=r_wt[b])
            for kc in range(KC):
                P.op("pe", lambda b=b, kc=kc: nc.tensor.matmul(pst[b][:], lhsT=ca[:, kc:kc + 1], rhs=wt[b][:, kc, :],
                                                               start=(kc == 0), stop=(kc == KC - 1)),
                     reads=[r_ca, r_wt[b]], writes=[r_ps[b]])
            P.op("dve", lambda g=g, b=b: nc.vector.tensor_tensor(out=ot[:, g * 512:(g + 1) * 512], in0=pst[b][:],
                                                                 in1=bt[:, g * 512:(g + 1) * 512], op=ALU.add),
                 reads=[r_ps[b], r_bt], writes=[r_ot])
        f = P.op("sp", lambda: nc.sync.dma_start(out=out, in_=ot[:]), reads=[r_ot], dma=r_ot)
        P.emit(final_ops=[f])
    return nc


def run_l0(c, w_ada, b_ada, w_ada_final, b_ada_final):
    wcat = np.concatenate([w_ada[0], w_ada_final], axis=1)
    bcat = np.concatenate([b_ada[0], b_ada_final], axis=0)
    cl = np.ascontiguousarray(c[0].reshape(KC, 128).T)
    in_maps = []
    for j in range(NCORES):
        in_maps.append({"c": cl, "w": np.ascontiguousarray(wcat[:, j * 2048:(j + 1) * 2048]),
                        "b": np.ascontiguousarray(bcat[None, j * 2048:(j + 1) * 2048])})
    res = run_bass_kernel_spmd(build_l0(), in_maps, core_ids=list(range(NCORES)))
    return np.concatenate([r["mod"][0] for r in res.results])


NC1 = 1027


def build_l1(seq):
    NT = seq // 128
    nc = new_nc()
    xT = nc.dram_tensor("xT", [D, seq], F32, kind="ExternalInput").ap()
    w1 = nc.dram_tensor("w1", [D, NC1], F32, kind="ExternalInput").ap()
    v16d = nc.dram_tensor("v16", [128, 48], F32, kind="ExternalInput").ap()
    scald = nc.dram_tensor("scal", [1, 3], F32, kind="ExternalInput").ap()
    gvecd = nc.dram_tensor("gvec", [128, 512], F32, kind="ExternalInput").ap()
    cf32d = nc.dram_tensor("cf32", [128, 256], F32, kind="ExternalInput").ap()
    cbfd = nc.dram_tensor("cbf", [128, 384], BF16, kind="ExternalInput").ap()
    hout = nc.dram_tensor("hout", [seq, 256], BF16, kind="ExternalOutput").ap()
    V, S, G, T = nc.vector, nc.scalar, nc.gpsimd, nc.tensor
    with contextlib.ExitStack() as st:
        P = Prog(nc, st)
        R = P.res
        bank = [P.ps(f"bank{i}", [128, 512]) for i in range(8)]
        rb = [R(f"bank{i}") for i in range(8)]
        v16 = P.sb("v16t", [128, 48], F32); r_v16 = R()
        gs = P.sb("gs", [128, 16], F32); r_gs = R()
        scal = P.sb("scalt", [1, 3], F32); r_scal = R()
        gv = P.sb("gv", [128, 512], F32); r_gv = R()
        c32 = P.sb("c32", [128, 256], F32); r_c32 = R()
        cb = P.sb("cb", [128, 384], BF16); r_cb = R()
        UT32, ONE32 = c32[:, 0:128], c32[:, 128:256]
        IDB, UTB, ONEB = cb[:, 0:128], cb[:, 128:256], cb[:, 256:384]
        wst = [P.sb(f"wst{i}", [128, NC1], F32) for i in range(2)]; r_wst = [R(), R()]
        Wb = P.sb("Wb", [128, KC, NC1 + 1], BF16); r_Wb = R()
        shWrow = P.sb("shWrow", [1, NC1], F32); r_shWrow = R()
        shWb = P.sb("shWb", [128, NC1], F32); r_shWb = R()
        XG = 256
        xst = [P.sb(f"xst{i}", [128, KC, XG], F32) for i in range(2)]; r_xst = [R(), R()]
        xb = [P.sb(f"xb{i}", [128, KC, XG], BF16) for i in range(2)]; r_xb = [R(), R()]
        sq = [P.sb(f"sq{i}", [128, KC, XG], BF16) for i in range(2)]; r_sq = [R(), R()]
        pj = [P.sb(f"pj{i}", [128, NC1], F32) for i in range(2)]; r_pj = [R(), R()]
        kT = P.sb("kT", [128, seq], BF16); r_kT = [R() for _ in range(NT)]
        vaug = P.sb("vaug", [128, NT, 130], BF16); r_v = [R() for _ in range(NT)]
        cmat = P.sb("cmat", [128, NT], F32); r_cmat = R()
        nU = P.sb("nU", [128, NT], F32); r_nU = R()
        biasm = [P.sb(f"biasm{i}", [128, NT], F32) for i in range(2)]; r_bias = [R(), R()]
        two = lambda nm, shp, dt: ([P.sb(f"{nm}{i}", shp, dt) for i in range(2)], [R(), R()])
        qn, r_qn = two("qn", [128, 128], BF16)
        kn, r_kn = two("kn", [128, 128], BF16)
        qT, r_qT = two("qT", [128, 128], BF16)
        mqb, r_mqb = two("mqb", [128, 128], BF16)
        kab, r_kab = two("kab", [128, 128], BF16)
        mqT, r_mqT = two("mqT", [128, 128], BF16)
        kaT, r_kaT = two("kaT", [128, 128], BF16)
        vm, r_vm = two("vm", [128, 258], BF16)
        sg, r_sg = two("sg", [128, 128], F32)
        SmT, r_SmT = two("SmT", [128, 128], BF16)
        outt, r_outt = two("outt", [128, 256], BF16)
        NPT = 3
        PT = [P.sb(f"PT{i}", [128, 128], BF16) for i in range(NPT)]; r_PT = [R() for _ in range(NPT)]
        junk = P.sb("junk", [128, 256], F32); r_junk = R()
        junk2 = P.sb("junk2", [128, 256], F32); r_junk2 = R()
        sm, r_sm = two("sm", [128, 32], F32)
        Cst = P.sb("Cst", [128, 257], F32); r_C = R()
        Ctmp = P.sb("Ctmp", [128, 257], F32); r_Ctmp = R()
        Cb = P.sb("Cb", [128, 258], BF16); r_Cb = R()
        h_m = P.sb("h_m", [128, 256], F32); r_hm = R()
        hfu = P.sb("hfu", [128, 128], F32); r_hfu = R()
        otmp = P.sb("otmp", [128, 128], F32); r_otmp = R()

        P.op("sp", lambda: nc.sync.dma_start(out=v16[:], in_=v16d), writes=[r_v16], dma=r_v16)
        P.op("sp", lambda: nc.sync.dma_start(out=scal[:], in_=scald), writes=[r_scal], dma=r_scal)
        P.op("sp", lambda: nc.sync.dma_start(out=gv[:], in_=gvecd), writes=[r_gv], dma=r_gv)
        P.op("sp", lambda: nc.sync.dma_start(out=c32[:], in_=cf32d), writes=[r_c32], dma=r_c32)
        P.op("sp", lambda: nc.sync.dma_start(out=cb[:], in_=cbfd), writes=[r_cb], dma=r_cb)
        P.op("dve", lambda: V.tensor_scalar(out=gs[:], in0=v16[:, 16:32], scalar1=1.0, scalar2=None, op0=ALU.add),
             reads=[r_v16], writes=[r_gs])
        P.op("dve", lambda: V.tensor_tensor(out=gs[:], in0=gs[:], in1=v16[:, 0:16], op=ALU.mult),
             reads=[r_v16, r_gs], writes=[r_gs])
        P.op("dve", lambda: V.tensor_scalar(out=gv[:, 0:128], in0=gv[:, 0:128], scalar1=128.0 ** -0.5, scalar2=None,
                                            op0=ALU.mult), reads=[r_gv], writes=[r_gv])
        P.op("pool", lambda: G.memset(vaug[:], 1.0), writes=r_v)
        for i in range(2):
            P.op("pool", lambda i=i: G.memset(vm[i][:], 1.0), writes=[r_vm[i]])
        P.op("pool", lambda: G.memset(Cst[:], 0.0), writes=[r_C])
        P.op("pool", lambda: G.memset(Cb[:], 0.0), writes=[r_Cb])
        P.op("pool", lambda: G.memset(nU[:], 0.0), writes=[r_nU])
        groups = [(0, 512, 0), (512, 1024, 1), (1024, NC1, 2)]
        for kc in range(KC):
            b = kc % 2
            P.op("sp", lambda kc=kc, b=b: nc.sync.dma_start(out=wst[b][:], in_=w1[kc * 128:(kc + 1) * 128, :]),
                 writes=[r_wst[b]], dma=r_wst[b])
            for (c0, c1, bk) in groups:
                P.op("pe", lambda kc=kc, b=b, c0=c0, c1=c1, bk=bk: T.matmul(
                    bank[bk][0:1, 0:c1 - c0], lhsT=v16[:, 32 + kc:33 + kc], rhs=wst[b][:, c0:c1],
                    start=(kc == 0), stop=(kc == KC - 1)), reads=[r_v16, r_wst[b]], writes=[rb[bk]])
            P.op("dve", lambda kc=kc, b=b: V.tensor_scalar(out=Wb[:, kc, 0:NC1], in0=wst[b][:], scalar1=gs[:, kc:kc + 1],
                                                          scalar2=None, op0=ALU.mult),
                 reads=[r_wst[b], r_gs], writes=[r_Wb])
        for (c0, c1, bk) in groups:
            P.op("dve", lambda c0=c0, c1=c1, bk=bk: V.tensor_copy(out=shWrow[:, c0:c1], in_=bank[bk][0:1, 0:c1 - c0]),
                 reads=[], writes=[rb[bk], r_shWrow])
        P.op("dve", lambda: V.tensor_tensor(out=shWrow[:, 1024:1027], in0=shWrow[:, 1024:1027], in1=scal[:, 0:3], op=ALU.add),
             reads=[r_scal, r_shWrow], writes=[r_shWrow])
        for (c0, c1, bk) in groups:
            P.op("pe", lambda c0=c0, c1=c1, bk=bk: T.matmul(bank[bk][:, 0:c1 - c0], lhsT=c32[0:1, 128:256],
                                                          rhs=shWrow[0:1, c0:c1], start=True, stop=True),
                 reads=[r_c32, r_shWrow], writes=[rb[bk]])
            P.op("dve", lambda c0=c0, c1=c1, bk=bk: V.tensor_copy(out=shWb[:, c0:c1], in_=bank[bk][:, 0:c1 - c0]),
                 writes=[rb[bk], r_shWb])

        xv = xT.rearrange("(kc p) t -> p kc t", p=128)
        trp = bank[2][:, 256:512].bitcast(BF16)
        psS = bank[2]
        final = []
        ptk = 0
        pend = []

        def stageA1(i):
            p = i % 2
            gb = (i // 2) % 2
            if i % 2 == 0:
                gi = i // 2
                P.op("sp", lambda gi=gi, gb=gb: nc.sync.dma_start(out=xst[gb][:], in_=xv[:, :, gi * XG:(gi + 1) * XG]),
                     writes=[r_xst[gb]], dma=r_xst[gb])
                P.op("act", lambda gb=gb: S.copy(out=xb[gb][:], in_=xst[gb][:]), reads=[r_xst[gb]], writes=[r_xb[gb]])
                P.op("pool", lambda gb=gb: G.tensor_tensor(out=sq[gb][:], in0=xst[gb][:], in1=xst[gb][:], op=ALU.mult),
                     reads=[r_xst[gb]], writes=[r_sq[gb]])
            ts = slice((i % 2) * 128, (i % 2) * 128 + 128)
            smt, r_smt = sm[p], r_sm[p]
            for kc in range(KC):
                P.op("pe", lambda gb=gb, kc=kc, ts=ts: T.matmul(psS[:, 8:9], lhsT=sq[gb][:, kc, ts], rhs=cb[:, 256:257],
                                                              start=(kc == 0), stop=(kc == KC - 1)),
                     reads=[r_sq[gb], r_cb], writes=[rb[2]])
            P.op("act", lambda smt=smt: S.activation(out=smt[:, 0:1], in_=psS[:, 8:9], func=AF.Ln, bias=EPS, scale=1.0 / D),
                 writes=[rb[2], r_smt])
            P.op("act", lambda smt=smt: S.activation(out=smt[:, 1:2], in_=smt[:, 0:1], func=AF.Exp, scale=-0.5),
                 reads=[r_smt], writes=[r_smt])
            for (c0, c1, bk) in groups:
                for kc in range(KC):
                    P.op("pe", lambda gb=gb, kc=kc, ts=ts, c0=c0, c1=c1, bk=bk: T.matmul(
                        bank[bk][:, 0:c1 - c0], lhsT=xb[gb][:, kc, ts], rhs=Wb[:, kc, c0:c1],
                        start=(kc == 0), stop=(kc == KC - 1)), reads=[r_xb[gb], r_Wb], writes=[rb[bk]])
            for (c0, c1, bk) in groups:
                P.op("dve", lambda p=p, smt=smt, c0=c0, c1=c1, bk=bk: V.scalar_tensor_tensor(
                    out=pj[p][:, c0:c1], in0=bank[bk][:, 0:c1 - c0], scalar=smt[:, 1:2], in1=shWb[:, c0:c1],
                    op0=ALU.mult, op1=ALU.add), reads=[r_smt, r_shWb], writes=[rb[bk], r_pj[p]])
            pjt, r_pjt = pj[p], r_pj[p]
            P.deferred = pend
            P.op("act", lambda pjt=pjt, smt=smt: S.activation(out=smt[:, 2:4], in_=pjt[:, 1024:1026], func=AF.Exp, scale=2.0 / 15.0),
                 reads=[r_pjt], writes=[r_smt])
            P.op("dve", lambda smt=smt: V.tensor_scalar(out=smt[:, 2:4], in0=smt[:, 2:4], scalar1=1.0, scalar2=None, op0=ALU.add),
                 reads=[r_smt], writes=[r_smt])
            P.op("dve", lambda smt=smt: V.reciprocal(out=smt[:, 2:4], in_=smt[:, 2:4]), reads=[r_smt], writes=[r_smt])
            P.op("dve", lambda smt=smt: V.tensor_scalar(out=smt[:, 4:6], in0=smt[:, 2:4], scalar1=-30.0, scalar2=15.0,
                                                       op0=ALU.mult, op1=ALU.add), reads=[r_smt], writes=[r_smt])
            P.op("dve", lambda smt=smt, pjt=pjt: V.tensor_copy(out=smt[:, 6:7], in_=pjt[:, 1026:1027]), reads=[r_pjt, r_smt], writes=[r_smt])
            P.op("act", lambda smt=smt: S.activation(out=smt[:, 8:10], in_=smt[:, 5:7], func=AF.Exp, scale=-1.0),
                 reads=[r_smt], writes=[r_smt])
            P.op("act", lambda smt=smt: S.activation(out=smt[:, 8:10], in_=smt[:, 8:10], func=AF.Ln, bias=1.0, scale=1.0),
                 reads=[r_smt], writes=[r_smt])
            P.op("pool", lambda pjt=pjt: G.tensor_tensor(out=junk[:], in0=pjt[:, 0:256], in1=pjt[:, 0:256], op=ALU.mult),
                 reads=[r_pjt], writes=[r_junk])
            P.op("dve", lambda smt=smt: V.tensor_reduce(out=smt[:, 18:20], in_=junk[:].rearrange("p (a b) -> p a b", a=2), axis=AX.X, op=ALU.add),
                 reads=[r_junk, r_smt], writes=[r_smt])
            P.op("act", lambda smt=smt: S.activation(out=smt[:, 18:20], in_=smt[:, 18:20], func=AF.Ln, bias=EPS, scale=1.0 / 128), reads=[r_smt], writes=[r_smt])
            P.op("act", lambda smt=smt: S.activation(out=smt[:, 20:22], in_=smt[:, 18:20], func=AF.Exp, scale=-0.5), reads=[r_smt], writes=[r_smt])
            P.op("dve", lambda p=p, pjt=pjt, smt=smt: V.scalar_tensor_tensor(out=qn[p][:], in0=pjt[:, 0:128], scalar=smt[:, 20:21], in1=gv[:, 0:128],
                                                                          op0=ALU.mult, op1=ALU.mult), reads=[r_pjt, r_smt, r_gv], writes=[r_qn[p]])
            P.op("dve", lambda p=p, pjt=pjt, smt=smt: V.scalar_tensor_tensor(out=kn[p][:], in0=pjt[:, 128:256], scalar=smt[:, 21:22], in1=gv[:, 128:256],
                                                                          op0=ALU.mult, op1=ALU.mult), reads=[r_pjt, r_smt, r_gv], writes=[r_kn[p]])
            P.op("pool", lambda p=p, pjt=pjt: G.tensor_scalar(out=mqb[p][:], in0=pjt[:, 384:512], scalar1=128.0 ** -0.5, scalar2=None, op0=ALU.mult),
                 reads=[r_pjt], writes=[r_mqb[p]])
            P.op("pool", lambda p=p, pjt=pjt: G.tensor_copy(out=vm[p][:, 0:256], in_=pjt[:, 640:896]), reads=[r_pjt], writes=[r_vm[p]])
            P.op("pool", lambda i=i, pjt=pjt: G.tensor_copy(out=vaug[:, i, 0:128], in_=pjt[:, 256:384]), reads=[r_pjt], writes=[r_v[i]])
            P.op("act", lambda p=p, pjt=pjt: S.activation(out=sg[p][:], in_=pjt[:, 896:1024], func=AF.Exp, scale=-1.0), reads=[r_pjt], writes=[r_sg[p]])
            P.op("pool", lambda p=p: G.tensor_scalar(out=sg[p][:], in0=sg[p][:], scalar1=1.0, scalar2=None, op0=ALU.add), reads=[r_sg[p]], writes=[r_sg[p]])
            P.op("dve", lambda p=p: V.reciprocal(out=sg[p][:], in_=sg[p][:]), reads=[r_sg[p]], writes=[r_sg[p]])
            P.deferred = None

        def flush(n=None):
            k = len(pend) if n is None else min(n, len(pend))
            for _ in range(k):
                a = pend.pop(0)
                P.op(a[0], a[1], reads=a[2], writes=a[3], dma=a[4])

        def stageA2(i):
            p = i % 2
            gb = (i // 2) % 2
            ts = slice((i % 2) * 128, (i % 2) * 128 + 128)
            smt, r_smt = sm[p], r_sm[p]
            pjt, r_pjt = pj[p], r_pj[p]
            A_, B_, DEC_ = smt[:, 15:16], smt[:, 16:17], smt[:, 17:18]
            P.op("pe", lambda smt=smt: T.matmul(psS[:, 10:12], lhsT=UT32, rhs=smt[:, 8:10], start=True, stop=True),
                 reads=[r_c32, r_smt], writes=[rb[2]])
            P.op("pe", lambda smt=smt: T.matmul(psS[:, 12:14], lhsT=ONE32, rhs=smt[:, 8:10], start=True, stop=True),
                 reads=[r_c32, r_smt], writes=[rb[2]])
            P.op("dve", lambda smt=smt: V.tensor_copy(out=smt[:, 10:14], in_=psS[:, 10:14]), reads=[r_smt], writes=[rb[2], r_smt])
            P.op("dve", lambda smt=smt: V.tensor_tensor(out=smt[:, 14:15], in0=smt[:, 4:5], in1=smt[:, 10:11], op=ALU.add),
                 reads=[r_smt], writes=[r_smt])
            P.op("act", lambda smt=smt: S.activation(out=smt[:, 15:16], in_=smt[:, 14:15], func=AF.Exp), reads=[r_smt], writes=[r_smt])
            P.op("act", lambda smt=smt: S.activation(out=smt[:, 16:17], in_=smt[:, 10:11], func=AF.Exp, scale=-1.0), reads=[r_smt], writes=[r_smt])
            P.op("act", lambda smt=smt: S.activation(out=smt[:, 17:18], in_=smt[:, 12:13], func=AF.Exp, scale=-1.0), reads=[r_smt], writes=[r_smt])
            A_, B_, DEC_ = smt[:, 15:16], smt[:, 16:17], smt[:, 17:18]
            P.op("dve", lambda smt=smt, i=i: V.tensor_copy(out=cmat[:, i:i + 1], in_=smt[:, 11:12]), reads=[r_smt], writes=[r_cmat])
            P.op("dve", lambda smt=smt, i=i: V.tensor_scalar(out=nU[:, 0:i + 1], in0=nU[:, 0:i + 1], scalar1=smt[:, 13:14], scalar2=None,
                                                            op0=ALU.add), reads=[r_smt, r_nU], writes=[r_nU])
            P.op("dve", lambda p=p, i=i: V.tensor_tensor(out=biasm[p][:, 0:i + 1], in0=cmat[:, 0:i + 1], in1=nU[:, 0:i + 1], op=ALU.subtract),
                 reads=[r_cmat, r_nU], writes=[r_bias[p]])
            for k, (src, r_src) in enumerate([(qn[p], r_qn[p]), (kn[p], r_kn[p])]):
                P.op("pe", lambda k=k, src=src: T.transpose(out=trp[:, k * 128:(k + 1) * 128], in_=src[:], identity=IDB),
                     reads=[r_src, r_cb], writes=[rb[2]])
            P.op("act", lambda p=p: S.copy(out=qT[p][:], in_=trp[:, 0:128]), writes=[rb[2], r_qT[p]])
            P.op("act", lambda i=i: S.copy(out=kT[:, i * 128:(i + 1) * 128], in_=trp[:, 128:256]), writes=[rb[2], r_kT[i]])
            P.op("dve", lambda p=p, pjt=pjt, A_=A_: V.tensor_scalar(out=kab[p][:], in0=pjt[:, 512:640], scalar1=A_, scalar2=None, op0=ALU.mult),
                 reads=[r_pjt, r_smt], writes=[r_kab[p]])
            for k, (src, r_src) in [(2, (mqb[p], r_mqb[p])), (3, (kab[p], r_kab[p]))]:
                P.op("pe", lambda k=k, src=src: T.transpose(out=trp[:, k * 128:(k + 1) * 128], in_=src[:], identity=IDB),
                     reads=[r_src, r_cb], writes=[rb[2]])
            P.op("dve", lambda p=p: V.tensor_copy(out=mqT[p][:], in_=trp[:, 256:384]), writes=[rb[2], r_mqT[p]])
            P.op("dve", lambda p=p: V.tensor_copy(out=kaT[p][:], in_=trp[:, 384:512]), writes=[rb[2], r_kaT[p]])
            P.op("pe", lambda p=p: T.matmul(bank[3][:, 0:257], lhsT=kab[p][:], rhs=vm[p][:, 0:257], start=True, stop=True),
                 reads=[r_kab[p], r_vm[p]], writes=[rb[3]])
            P.op("pe", lambda p=p: T.matmul(bank[4][:, 0:128], lhsT=kaT[p][:], rhs=mqT[p][:], start=True, stop=True),
                 reads=[r_kaT[p], r_mqT[p]], writes=[rb[4]])
            P.op("dve", lambda p=p: V.tensor_tensor(out=SmT[p][:], in0=bank[4][:, 0:128], in1=UT32, op=ALU.mult),
                 reads=[r_c32], writes=[rb[4], r_SmT[p]])
            P.op("pe", lambda p=p: T.matmul(bank[4][:, 128:385], lhsT=mqT[p][:], rhs=Cb[:, 0:257], start=True, stop=False),
                 reads=[r_mqT[p], r_Cb], writes=[rb[4]])
            P.op("pe", lambda p=p: T.matmul(bank[4][:, 128:385], lhsT=SmT[p][:], rhs=vm[p][:, 0:257], start=False, stop=True),
                 reads=[r_SmT[p], r_vm[p]], writes=[rb[4]])
            P.op("dve", lambda: V.tensor_tensor(out=Ctmp[:], in0=bank[3][:, 0:257], in1=Cst[:], op=ALU.add),
                 reads=[r_C], writes=[rb[3], r_Ctmp])
            P.op("dve", lambda DEC_=DEC_: V.tensor_scalar(out=Cst[:], in0=Ctmp[:], scalar1=DEC_, scalar2=None, op0=ALU.mult),
                 reads=[r_Ctmp, r_smt], writes=[r_C])
            P.op("pool", lambda: G.tensor_copy(out=Cb[:, 0:257], in_=Cst[:]), reads=[r_C], writes=[r_Cb])
            P.op("dve", lambda smt=smt, B_=B_: V.tensor_scalar(out=smt[:, 22:23], in0=bank[4][:, 384:385], scalar1=B_, scalar2=None,
                                                             op0=ALU.mult), reads=[r_smt], writes=[rb[4], r_smt])
            P.op("dve", lambda smt=smt: V.tensor_scalar(out=smt[:, 30:31], in0=smt[:, 22:23], scalar1=-1.0, scalar2=1.0,
                                                       op0=ALU.mult, op1=ALU.max), reads=[r_smt], writes=[r_smt])
            P.op("dve", lambda smt=smt: V.tensor_tensor(out=smt[:, 22:23], in0=smt[:, 22:23], in1=smt[:, 30:31], op=ALU.max),
                 reads=[r_smt], writes=[r_smt])
            P.op("dve", lambda smt=smt: V.reciprocal(out=smt[:, 23:24], in_=smt[:, 22:23]), reads=[r_smt], writes=[r_smt])
            P.op("dve", lambda smt=smt, B_=B_: V.tensor_tensor(out=smt[:, 24:25], in0=smt[:, 23:24], in1=B_, op=ALU.mult), reads=[r_smt], writes=[r_smt])
            P.op("dve", lambda smt=smt: V.tensor_scalar(out=h_m[:], in0=bank[4][:, 128:384], scalar1=smt[:, 24:25], scalar2=None, op0=ALU.mult),
                 reads=[r_smt], writes=[rb[4], r_hm])
            P.op("pool", lambda: G.tensor_tensor(out=junk2[:], in0=h_m[:], in1=h_m[:], op=ALU.mult), reads=[r_hm], writes=[r_junk2])
            P.op("dve", lambda smt=smt: V.tensor_reduce(out=smt[:, 25:26], in_=junk2[:], axis=AX.X, op=ALU.add), reads=[r_junk2, r_smt], writes=[r_smt])
            P.op("act", lambda smt=smt: S.activation(out=smt[:, 25:26], in_=smt[:, 25:26], func=AF.Ln, bias=EPS, scale=1.0 / 256), reads=[r_smt], writes=[r_smt])
            P.op("act", lambda smt=smt: S.activation(out=smt[:, 26:27], in_=smt[:, 25:26], func=AF.Exp, scale=-0.5), reads=[r_smt], writes=[r_smt])
            P.op("dve", lambda smt=smt: V.scalar_tensor_tensor(out=otmp[:], in0=h_m[:, 0:128], scalar=smt[:, 26:27], in1=gv[:, 384:512],
                                                             op0=ALU.mult, op1=ALU.mult), reads=[r_hm, r_smt, r_gv], writes=[r_otmp])
            P.op("dve", lambda p=p: V.tensor_tensor(out=outt[p][:, 0:128], in0=otmp[:], in1=sg[p][:], op=ALU.mult),
                 reads=[r_otmp, r_sg[p]], writes=[r_outt[p]])
            P.deferred = None

        def stageB(i):
            nonlocal ptk
            p = i % 2
            gb = (i // 2) % 2
            ts = slice((i % 2) * 128, (i % 2) * 128 + 128)
            smt, r_smt = sm[p], r_sm[p]
            pjt, r_pjt = pj[p], r_pj[p]
            A_, B_, DEC_ = smt[:, 15:16], smt[:, 16:17], smt[:, 17:18]
            def qk(sb, p=p):
                pb = 5 + (sb % 2)
                P.op("pe", lambda: T.matmul(bank[pb][:, 0:128], lhsT=kT[:, sb * 128:(sb + 1) * 128], rhs=qT[p][:], start=True, stop=True),
                     reads=[r_kT[sb], r_qT[p]], writes=[rb[pb]])
            def pv(sb, k, p=p, i=i):
                pb = 5 + (sb % 2)
                P.op("act", lambda: S.activation(out=PT[k][:], in_=bank[pb][:, 0:128], func=AF.Exp, bias=biasm[p][:, sb:sb + 1], scale=1.0),
                     reads=[r_bias[p]], writes=[rb[pb], r_PT[k]])
                if sb == i:
                    P.op("pool", lambda: G.tensor_tensor(out=PT[k][:], in0=PT[k][:], in1=UTB, op=ALU.mult), reads=[r_cb, r_PT[k]], writes=[r_PT[k]])
                P.op("pe", lambda: T.matmul(bank[7][:, 0:129], lhsT=PT[k][:], rhs=vaug[:, sb, 0:129], start=(sb == 0), stop=(sb == i)),
                     reads=[r_PT[k], r_v[sb]], writes=[rb[7]])
            share = (len(pend) + i) // (i + 1)
            qk(0)
            for sb in range(i + 1):
                if sb + 1 <= i:
                    qk(sb + 1)
                pv(sb, ptk % NPT)
                ptk += 1
                flush(share)
            flush()
            P.op("dve", lambda smt=smt: V.reciprocal(out=smt[:, 27:28], in_=bank[7][:, 128:129]), reads=[r_smt], writes=[rb[7], r_smt])
            P.op("dve", lambda smt=smt: V.tensor_scalar(out=hfu[:], in0=bank[7][:, 0:128], scalar1=smt[:, 27:28], scalar2=None, op0=ALU.mult),
                 reads=[r_smt], writes=[rb[7], r_hfu])
            P.op("pool", lambda: G.tensor_tensor(out=junk2[:, 0:128], in0=hfu[:], in1=hfu[:], op=ALU.mult), reads=[r_hfu], writes=[r_junk2])
            P.op("dve", lambda smt=smt: V.tensor_reduce(out=smt[:, 28:29], in_=junk2[:, 0:128], axis=AX.X, op=ALU.add), reads=[r_junk2, r_smt], writes=[r_smt])
            P.op("act", lambda smt=smt: S.activation(out=smt[:, 28:29], in_=smt[:, 28:29], func=AF.Ln, bias=EPS, scale=1.0 / 128), reads=[r_smt], writes=[r_smt])
            P.op("act", lambda smt=smt: S.activation(out=smt[:, 29:30], in_=smt[:, 28:29], func=AF.Exp, scale=-0.5), reads=[r_smt], writes=[r_smt])
            P.op("dve", lambda p=p, smt=smt: V.scalar_tensor_tensor(out=outt[p][:, 128:256], in0=hfu[:], scalar=smt[:, 29:30], in1=gv[:, 256:384],
                                                                  op0=ALU.mult, op1=ALU.mult), reads=[r_hfu, r_smt, r_gv], writes=[r_outt[p]])
            f = P.op("sp", lambda p=p, i=i: nc.sync.dma_start(out=hout[i * 128:(i + 1) * 128, :], in_=outt[p][:]), reads=[r_outt[p]], dma=r_outt[p])
            final.append(f)
        stageA1(0)
        flush()
        stageA2(0)
        for i in range(NT):
            if i + 1 < NT:
                stageA1(i + 1)
            stageB(i)
            if i + 1 < NT:
                stageA2(i + 1)
        P.emit(final_ops=final[-2:])
    return nc


W_OFF = dict(mq=0, mk=512, mv=1024, mo=2048, mi=3072, mf=3076, fq=3080, fk=4104, fv=5128, ff=6152)


def l1_inputs(j, xT, w_in, norm_mix, sc1, sh1, b_i, b_f, fox_b_f, gq, gk, gfo, gmo):
    h, half = j // 2, j % 2
    o = W_OFF
    cols = np.concatenate([
        np.arange(o["fq"] + 128 * j, o["fq"] + 128 * j + 128), np.arange(o["fk"] + 128 * j, o["fk"] + 128 * j + 128),
        np.arange(o["fv"] + 128 * j, o["fv"] + 128 * j + 128), np.arange(o["mq"] + 128 * h, o["mq"] + 128 * h + 128),
        np.arange(o["mk"] + 128 * h, o["mk"] + 128 * h + 128),
        np.arange(o["mv"] + 256 * h + 128 * half, o["mv"] + 256 * h + 128 * half + 128),
        np.arange(o["mv"] + 256 * h + 128 * (1 - half), o["mv"] + 256 * h + 128 * (1 - half) + 128),
        np.arange(o["mo"] + 256 * h + 128 * half, o["mo"] + 256 * h + 128 * half + 128),
        [o["mi"] + h, o["mf"] + h, o["ff"] + j]]).astype(np.int64)
    lay = lambda v: v.reshape(KC, 128).T
    v16 = np.ascontiguousarray(np.concatenate([lay(norm_mix), lay(sc1), lay(sh1)], axis=1), dtype=np.float32)
    scal = np.array([[b_i[h], b_f[h], fox_b_f[j]]], np.float32)
    bc = lambda v: np.broadcast_to(v[None, :], (128, v.shape[0]))
    gvec = np.ascontiguousarray(np.concatenate([bc(gq), bc(gk), bc(gfo[j]), bc(gmo[h, 128 * half:128 * half + 128])], axis=1), dtype=np.float32)
    ut = np.triu(np.ones((128, 128), np.float32))
    cf32 = np.ascontiguousarray(np.concatenate([ut, np.ones((128, 128), np.float32)], axis=1))
    cbf = np.ascontiguousarray(np.concatenate([np.eye(128, dtype=np.float32), ut, np.ones((128, 128), np.float32)], axis=1)).astype(ml_dtypes.bfloat16)
    return {"xT": xT, "w1": np.ascontiguousarray(w_in[:, cols]), "v16": v16, "scal": scal, "gvec": gvec, "cf32": cf32, "cbf": cbf}


NE = 32


def rank_ops(P, nc, bankr, rbr, maskb, r_maskb, i, cb, r_cb, mask32, r_mask, rk_out, r_rk, lo=32):
    V, T = nc.vector, nc.tensor
    for ii in range(i + 1):
        lhs = cb[:, 256:384] if ii < i else cb[:, 128:256]
        P.op("pe", lambda ii=ii, lhs=lhs: T.matmul(bankr[:, lo:lo + NE], lhsT=lhs, rhs=maskb[:, ii, :], start=(ii == 0), stop=(ii == i)),
             reads=[r_cb, r_maskb], writes=[rbr])
    P.op("dve", lambda: V.scalar_tensor_tensor(out=rk_out, in0=bankr[:, lo:lo + NE], scalar=1.0, in1=mask32, op0=ALU.add, op1=ALU.mult),
         reads=[r_mask], writes=[rbr, r_rk])
    P.op("dve", lambda: V.tensor_scalar(out=rk_out, in0=rk_out, scalar1=-1.0, scalar2=None, op0=ALU.add), reads=[r_rk], writes=[r_rk])


def build_l2(TL, CAP):
    NTL = TL // 128
    nc = new_nc()
    mixT = nc.dram_tensor("mixT", [D, TL], BF16, kind="ExternalInput").ap()
    xd = nc.dram_tensor("x", [TL, D], F32, kind="ExternalInput").ap()
    wod = nc.dram_tensor("w_out", [D, D], F32, kind="ExternalInput").ap()
    bcd = nc.dram_tensor("bc", [128, 3 * D + NE], F32, kind="ExternalInput").ap()
    sh2d = nc.dram_tensor("sh2", [128, D], F32, kind="ExternalInput").ap()
    wrd = nc.dram_tensor("w_r", [D, NE], F32, kind="ExternalInput").ap()
    cf32d = nc.dram_tensor("cf32", [128, 128 + 640], F32, kind="ExternalInput").ap()
    cbfd = nc.dram_tensor("cbf", [128, 384], BF16, kind="ExternalInput").ap()
    hres_o = nc.dram_tensor("hres", [TL, D], F32, kind="ExternalOutput").ap()
    G_o = nc.dram_tensor("G", [TL, NE], F32, kind="ExternalOutput").ap()
    XT_o = nc.dram_tensor("XT", [NE, D, CAP], BF16, kind="ExternalOutput").ap()
    V, S, G_, T = nc.vector, nc.scalar, nc.gpsimd, nc.tensor
    with contextlib.ExitStack() as st:
        P = Prog(nc, st)
        R = P.res
        bank = [P.ps(f"bank{i}", [128, 512]) for i in range(8)]
        rb = [R(f"bank{i}") for i in range(8)]
        wobf = P.sb("wob", [128, KC * D], BF16); r_wob = R()
        wob = wobf[:, :].rearrange("p (kc n) -> p kc n", kc=KC)
        bc = P.sb("bct", [128, 3 * D + NE], F32); r_bc = R()
        g1b, gs2b, brb = bc[:, 0:D], bc[:, D:2 * D], bc[:, 3 * D:3 * D + NE]
        sh2b = P.sb("sh2b", [128, D], F32); r_sh2 = R()
        wr = P.sb("wr", [128, KC, NE], F32); r_wr = R()
        c32 = P.sb("c32", [128, 768], F32); r_c32 = R()
        cb = P.sb("cb", [128, 384], BF16); r_cb = R()
        ID32, IOTA = c32[:, 0:128], c32[:, 128:128 + CAP]
        mx = [P.sb(f"mx{i}", [128, KC, 128], BF16) for i in range(2)]; r_mx = [R(), R()]
        _xt = P.sb("xt0", [128, D], F32); _rxt = R(); xt = [_xt, _xt]; r_xt = [_rxt, _rxt]
        hres = [P.sb(f"hres{i}", [128, D], F32) for i in range(2)]; r_hres = [R(), R()]
        h2f = P.sb("h2f", [128, D], F32); r_h2f = R()
        h2b = P.sb("h2b", [128, NTL, D], BF16); r_h2b = [R() for _ in range(NTL)]
        h2T = P.sb("h2T", [128, KC, 128], F32); r_h2T = R()
        maskb = P.sb("maskb", [128, NTL, NE], BF16); r_maskb = R()
        rk = P.sb("rk", [128, NTL, NE], F32); r_rk = R()
        sm = [P.sb(f"sm{i}", [128, 16], F32) for i in range(2)]; r_sm = [R(), R()]
        lg = [P.sb(f"lg{i}", [128, 4 * NE], F32) for i in range(2)]; r_lg = [R(), R()]
        assert 2 * NTL * CAP + 2 * KC * CAP <= KC * D
        sel = [wobf[:, q * NTL * CAP:(q + 1) * NTL * CAP].rearrange("p (i s) -> p i s", i=NTL) for q in range(2)]; r_sel = [R(), R()]
        xb0 = 2 * NTL * CAP
        xte = [wobf[:, xb0 + q * KC * CAP:xb0 + (q + 1) * KC * CAP].rearrange("p (fc s) -> p fc s", fc=KC) for q in range(2)]; r_xte = [R(), R()]

        r_wobk = [R() for _ in range(KC)]
        for kc in range(KC):
            P.op("pool", lambda kc=kc: G_.dma_start(out=wob[:, kc, :], in_=wod[kc * 128:(kc + 1) * 128, :]), writes=[r_wobk[kc]], dma=r_wobk[kc])
        P.op("sp", lambda: nc.sync.dma_start(out=bc[:], in_=bcd), writes=[r_bc], dma=r_bc)
        P.op("sp", lambda: nc.sync.dma_start(out=sh2b[:], in_=sh2d), writes=[r_sh2], dma=r_sh2)
        P.op("sp", lambda: nc.sync.dma_start(out=wr[:], in_=wrd.rearrange("(kc p) n -> p kc n", p=128)), writes=[r_wr], dma=r_wr)
        P.op("sp", lambda: nc.sync.dma_start(out=c32[:], in_=cf32d), writes=[r_c32], dma=r_c32)
        P.op("sp", lambda: nc.sync.dma_start(out=cb[:], in_=cbfd), writes=[r_cb], dma=r_cb)
        P.op("dve", lambda: V.scalar_tensor_tensor(out=bc[:, D:2 * D], in0=bc[:, 2 * D:3 * D], scalar=1.0, in1=bc[:, D:2 * D], op0=ALU.add, op1=ALU.mult),
             reads=[r_bc], writes=[r_bc])
        mv = mixT.rearrange("(kc p) t -> p kc t", p=128)
        finals = []
        for i in range(NTL):
            p = i % 2
            tsl = slice(i * 128, (i + 1) * 128)
            smt, r_smt = sm[p], r_sm[p]
            lgt, r_lgt = lg[p], r_lg[p]
            P.op("sp", lambda p=p, tsl=tsl: nc.sync.dma_start(out=mx[p][:], in_=mv[:, :, tsl]), writes=[r_mx[p]], dma=r_mx[p])
            P.op("sp", lambda p=p, tsl=tsl: nc.sync.dma_start(out=xt[p][:], in_=xd[tsl, :]), writes=[r_xt[p]], dma=r_xt[p])
            for dg in range(4):
                for kc in range(KC):
                    P.op("pe", lambda p=p, dg=dg, kc=kc: T.matmul(bank[dg][:, :], lhsT=mx[p][:, kc, :], rhs=wob[:, kc, dg * 512:(dg + 1) * 512],
                                                                 start=(kc == 0), stop=(kc == KC - 1)), reads=[r_mx[p], r_wobk[kc]], writes=[rb[dg]])
            for dg in range(4):
                ds = slice(dg * 512, (dg + 1) * 512)
                P.op("dve", lambda p=p, dg=dg, ds=ds: V.tensor_tensor(out=hres[p][:, ds], in0=bank[dg][:, :], in1=g1b[:, ds], op=ALU.mult),
                     reads=[r_bc], writes=[rb[dg], r_hres[p]])
                P.op("pool", lambda p=p, ds=ds: G_.tensor_tensor(out=hres[p][:, ds], in0=hres[p][:, ds], in1=xt[p][:, ds], op=ALU.add),
                     reads=[r_xt[p], r_hres[p]], writes=[r_hres[p]])
            f = P.op("sp", lambda p=p, tsl=tsl: nc.sync.dma_start(out=hres_o[tsl, :], in_=hres[p][:]), reads=[r_hres[p]], dma=r_hres[p])
            finals.append(f)
            P.op("pool", lambda p=p: G_.tensor_tensor(out=h2f[:], in0=hres[p][:], in1=hres[p][:], op=ALU.mult), reads=[r_hres[p]], writes=[r_h2f])
            P.op("dve", lambda smt=smt: V.tensor_reduce(out=smt[:, 0:1], in_=h2f[:], axis=AX.X, op=ALU.add), reads=[r_h2f], writes=[r_smt])
            P.op("act", lambda smt=smt: S.activation(out=smt[:, 1:2], in_=smt[:, 0:1], func=AF.Ln, bias=EPS, scale=1.0 / D), reads=[r_smt], writes=[r_smt])
            P.op("act", lambda smt=smt: S.activation(out=smt[:, 2:3], in_=smt[:, 1:2], func=AF.Exp, scale=-0.5), reads=[r_smt], writes=[r_smt])
            P.op("dve", lambda p=p, smt=smt: V.scalar_tensor_tensor(out=h2f[:], in0=hres[p][:], scalar=smt[:, 2:3], in1=gs2b, op0=ALU.mult, op1=ALU.mult),
                 reads=[r_hres[p], r_smt, r_bc, r_h2f], writes=[r_h2f])
            P.op("pool", lambda: G_.tensor_tensor(out=h2f[:], in0=h2f[:], in1=sh2b[:], op=ALU.add), reads=[r_sh2, r_h2f], writes=[r_h2f])
            P.op("act", lambda i=i: S.copy(out=h2b[:, i, :], in_=h2f[:]), reads=[r_h2f], writes=[r_h2b[i]])
            for q4 in range(4):
                tb = 4 + (q4 % 2)
                for k4 in range(4):
                    kc = q4 * 4 + k4
                    P.op("pe", lambda tb=tb, k4=k4, kc=kc: T.transpose(out=bank[tb][:, k4 * 128:(k4 + 1) * 128], in_=h2f[:, kc * 128:(kc + 1) * 128], identity=ID32),
                         reads=[r_h2f, r_c32], writes=[rb[tb]])
                eng = "act" if q4 % 2 == 0 else "dve"
                if eng == "act":
                    P.op("act", lambda tb=tb, q4=q4: S.copy(out=h2T[:, q4 * 4:(q4 + 1) * 4, :], in_=bank[tb][:, :].rearrange("p (a b) -> p a b", a=4)),
                         writes=[rb[tb], r_h2T])
                else:
                    P.op("dve", lambda tb=tb, q4=q4: V.tensor_copy(out=h2T[:, q4 * 4:(q4 + 1) * 4, :], in_=bank[tb][:, :].rearrange("p (a b) -> p a b", a=4)),
                         writes=[rb[tb], r_h2T])
            for kc in range(KC):
                P.op("pe", lambda kc=kc: T.matmul(bank[6][:, 0:NE], lhsT=h2T[:, kc, :], rhs=wr[:, kc, :], start=(kc == 0), stop=(kc == KC - 1)),
                     reads=[r_h2T, r_wr], writes=[rb[6]])
            P.op("dve", lambda lgt=lgt: V.tensor_tensor(out=lgt[:, 0:NE], in0=bank[6][:, 0:NE], in1=brb, op=ALU.add), reads=[r_bc], writes=[rb[6], r_lgt])
            P.op("dve", lambda lgt=lgt, smt=smt: V.max(out=smt[:, 8:16], in_=lgt[:, 0:NE]), reads=[r_lgt], writes=[r_smt])
            P.op("dve", lambda lgt=lgt, smt=smt: V.tensor_scalar(out=lgt[:, NE:2 * NE], in0=lgt[:, 0:NE], scalar1=smt[:, 11:12], scalar2=None, op0=ALU.is_ge),
                 reads=[r_lgt, r_smt], writes=[r_lgt])
            P.op("dve", lambda smt=smt: V.tensor_scalar(out=smt[:, 3:4], in0=smt[:, 8:9], scalar1=-1.0, scalar2=None, op0=ALU.mult), reads=[r_smt], writes=[r_smt])
            P.op("act", lambda lgt=lgt, smt=smt: S.activation(out=lgt[:, 2 * NE:3 * NE], in_=lgt[:, 0:NE], func=AF.Exp, bias=smt[:, 3:4], scale=1.0),
                 reads=[r_lgt, r_smt], writes=[r_lgt])
            P.op("dve", lambda lgt=lgt: V.tensor_tensor(out=lgt[:, 2 * NE:3 * NE], in0=lgt[:, 2 * NE:3 * NE], in1=lgt[:, NE:2 * NE], op=ALU.mult), reads=[r_lgt], writes=[r_lgt])
            P.op("dve", lambda lgt=lgt, smt=smt: V.tensor_reduce(out=smt[:, 4:5], in_=lgt[:, 2 * NE:3 * NE], axis=AX.X, op=ALU.add), reads=[r_lgt], writes=[r_smt])
            P.op("dve", lambda smt=smt: V.reciprocal(out=smt[:, 5:6], in_=smt[:, 4:5]), reads=[r_smt], writes=[r_smt])
            P.op("dve", lambda lgt=lgt, smt=smt: V.tensor_scalar(out=lgt[:, 3 * NE:4 * NE], in0=lgt[:, 2 * NE:3 * NE], scalar1=smt[:, 5:6], scalar2=None, op0=ALU.mult),
                 reads=[r_lgt, r_smt], writes=[r_lgt])
            f = P.op("sp", lambda lgt=lgt, tsl=tsl: nc.sync.dma_start(out=G_o[tsl, :], in_=lgt[:, 3 * NE:4 * NE]), reads=[r_lgt], dma=r_lgt)
            finals.append(f)
            P.op("pool", lambda lgt=lgt, i=i: G_.tensor_copy(out=maskb[:, i, :], in_=lgt[:, NE:2 * NE]), reads=[r_lgt], writes=[r_maskb])
            rank_ops(P, nc, bank[6], rb[6], maskb, r_maskb, i, cb, r_cb, lgt[:, NE:2 * NE], r_lgt, rk[:, i, :], r_rk)
        gbanks = [0, 1, 2, 3, 7, 4, 5]
        gi = 0
        ncc = (CAP + 511) // 512
        ccw = CAP // ncc
        P.op("dve", lambda: V.memset(sel[0][:, 0, 0:2], 0.0), writes=r_wobk + r_sel + r_xte)
        for e in range(NE):
            q = e % 2
            for i in range(NTL):
                P.op("dve", lambda q=q, i=i, e=e: V.tensor_scalar(out=sel[q][:, i, :], in0=IOTA, scalar1=rk[:, i, e:e + 1], scalar2=None, op0=ALU.is_equal),
                     reads=[r_c32, r_rk], writes=[r_sel[q]])
            for fc in range(KC):
                for cc in range(ncc):
                    cs = slice(cc * ccw, (cc + 1) * ccw)
                    gbk = gbanks[gi % len(gbanks)]; gi += 1
                    for i in range(NTL):
                        P.op("pe", lambda gbk=gbk, q=q, i=i, fc=fc, cs=cs: T.matmul(bank[gbk][:, 0:ccw], lhsT=h2b[:, i, fc * 128:(fc + 1) * 128], rhs=sel[q][:, i, cs],
                                                                                start=(i == 0), stop=(i == NTL - 1)), reads=[r_h2b[i], r_sel[q]], writes=[rb[gbk]])
                    if gi % 2 == 0:
                        P.op("act", lambda gbk=gbk, q=q, fc=fc, cs=cs: S.copy(out=xte[q][:, fc, cs], in_=bank[gbk][:, 0:ccw]), writes=[rb[gbk], r_xte[q]])
                    else:
                        P.op("dve", lambda gbk=gbk, q=q, fc=fc, cs=cs: V.tensor_copy(out=xte[q][:, fc, cs], in_=bank[gbk][:, 0:ccw]), writes=[rb[gbk], r_xte[q]])
            f = P.op("sp", lambda q=q, e=e: nc.sync.dma_start(out=XT_o[e].rearrange("(fc p) s -> p fc s", p=128), in_=xte[q][:]), reads=[r_xte[q]], dma=r_xte[q])
            finals.append(f)
        P.emit(final_ops=finals)
    return nc


def l2_inputs(mixT_g, x_g, w_out, g1, norm_ffn, sc2, sh2, w_router, b_router):
    bcst = lambda v: np.broadcast_to(v[None, :], (128, v.shape[0]))
    bc = np.ascontiguousarray(np.concatenate([bcst(g1), bcst(norm_ffn), bcst(sc2), bcst(b_router)], axis=1), dtype=np.float32)
    sl = np.tril(np.ones((128, 128), np.float32), -1).T
    cf32 = np.ascontiguousarray(np.concatenate([np.eye(128, dtype=np.float32), bcst(np.arange(640, dtype=np.float32))], axis=1))
    cbf = np.ascontiguousarray(np.concatenate([np.eye(128, dtype=np.float32), sl, np.ones((128, 128), np.float32)], axis=1)).astype(ml_dtypes.bfloat16)
    return {"mixT": mixT_g, "x": x_g, "w_out": w_out, "bc": bc, "sh2": np.ascontiguousarray(bcst(sh2), dtype=np.float32),
            "w_r": w_router, "cf32": cf32, "cbf": cbf}


EPC = 4


def build_l3(NS, NCH=1):
    nc = new_nc()
    NSC = NS // NCH
    NSG = (NSC + 511) // 512
    SGW = NSC // NSG
    NSB = NSC // 128
    xtd = nc.dram_tensor("XT", [EPC, D, NS], BF16, kind="ExternalInput").ap()
    wugd = nc.dram_tensor("w_ug", [EPC, D, 2 * D], F32, kind="ExternalInput").ap()
    bugd = nc.dram_tensor("b_ug", [EPC, 128, 32], F32, kind="ExternalInput").ap()
    wdd = nc.dram_tensor("w_d", [EPC, D, D], F32, kind="ExternalInput").ap()
    bdd = nc.dram_tensor("b_d", [EPC, 128, D], F32, kind="ExternalInput").ap()
    yd = nc.dram_tensor("y", [EPC, NS, D], BF16, kind="ExternalOutput").ap()
    cug = nc.dram_tensor("cache_ug", [32, 128, KC * 128], BF16).ap() if NCH > 1 else None
    cdn = nc.dram_tensor("cache_dn", [KC, 128, D], BF16).ap() if NCH > 1 else None
    V, S, G_, T = nc.vector, nc.scalar, nc.gpsimd, nc.tensor
    with contextlib.ExitStack() as st:
        P = Prog(nc, st)
        R = P.res
        bank = [P.ps(f"bank{i}", [128, 512]) for i in range(8)]
        rb = [R(f"bank{i}") for i in range(8)]
        xs = P.sb("xs", [128, KC, NSC], BF16); r_xs = R()
        actT = P.sb("actT", [128, KC, NSC], BF16); r_actT = [R() for _ in range(KC)]
        wd = P.sb("wd", [128, KC, D], BF16); r_wd = [R() for _ in range(KC)]
        NW = 3
        wg = [P.sb(f"wg{i}", [128, KC, 128], BF16) for i in range(NW)]; r_wg = [R() for _ in range(NW)]
        wu = [P.sb(f"wu{i}", [128, KC, 128], BF16) for i in range(NW)]; r_wu = [R() for _ in range(NW)]
        bug = P.sb("bug", [128, 32], F32); r_bug = R()
        bdb = P.sb("bdb", [128, D], F32); r_bdb = R()
        gcl = [P.sb(f"gcl{i}", [128, SGW], F32) for i in range(2)]; r_gcl = [R(), R()]
        sig = [P.sb(f"sig{i}", [128, SGW], F32) for i in range(2)]; r_sig = [R(), R()]
        ucl = [P.sb(f"ucl{i}", [128, SGW], F32) for i in range(2)]; r_ucl = [R(), R()]
        yst = [P.sb(f"yst{i}", [128, D], BF16) for i in range(2)]; r_yst = [R(), R()]
        r_cug = [R() for _ in range(32)]
        r_cdn = [R() for _ in range(KC)]
        r_wgst = [R() for _ in range(NW)]; r_wust = [R() for _ in range(NW)]; r_wdst = [R() for _ in range(KC)]
        finals = []
        wi = 0
        ei = 0
        for v in range(EPC * NCH):
            e, ch = v // NCH, v % NCH
            so = ch * NSC
            P.op("sp", lambda e=e, so=so: nc.sync.dma_start(out=xs[:], in_=xtd[e][:, so:so + NSC].rearrange("(kc p) s -> p kc s", p=128)), writes=[r_xs], dma=r_xs)
            if ch == 0:
                P.op("sp", lambda e=e: nc.sync.dma_start(out=bug[:], in_=bugd[e]), writes=[r_bug], dma=r_bug)
                P.op("sp", lambda e=e: nc.sync.dma_start(out=bdb[:], in_=bdd[e]), writes=[r_bdb], dma=r_bdb)
            wv = wugd[e].rearrange("(kc p) n -> p kc n", p=128)
            for c in range(KC):
                w = wi % NW; wi += 1
                if ch == 0:
                    P.op("pool", lambda w=w, c=c, wv=wv: G_.dma_start(out=wg[w][:], in_=wv[:, :, c * 128:(c + 1) * 128]), writes=[r_wg[w]], dma=r_wg[w])
                    P.op("pool", lambda w=w, c=c, wv=wv: G_.dma_start(out=wu[w][:], in_=wv[:, :, D + c * 128:D + (c + 1) * 128]), writes=[r_wu[w]], dma=r_wu[w])
                    P.op("pool", lambda e=e, c=c: G_.dma_start(out=wd[:, c, :], in_=wdd[e, c * 128:(c + 1) * 128, :]), writes=[r_wd[c]], dma=r_wd[c])
                    if NCH > 1:
                        P.op("act", lambda w=w, c=c: S.dma_start(out=cug[c], in_=wg[w][:].rearrange("p a b -> p (a b)")), reads=[r_wg[w]], writes=[r_cug[c]], dma=r_wgst[w])
                        P.op("act", lambda w=w, c=c: S.dma_start(out=cug[16 + c], in_=wu[w][:].rearrange("p a b -> p (a b)")), reads=[r_wu[w]], writes=[r_cug[16 + c]], dma=r_wust[w])
                        P.op("act", lambda c=c: S.dma_start(out=cdn[c], in_=wd[:, c, :]), reads=[r_wd[c]], writes=[r_cdn[c]], dma=r_wdst[c])
                else:
                    P.op("sp", lambda w=w, c=c: nc.sync.dma_start(out=wg[w][:].rearrange("p a b -> p (a b)"), in_=cug[c]), reads=[r_cug[c]], writes=[r_wg[w]], dma=r_wg[w])
                    P.op("sp", lambda w=w, c=c: nc.sync.dma_start(out=wu[w][:].rearrange("p a b -> p (a b)"), in_=cug[16 + c]), reads=[r_cug[16 + c]], writes=[r_wu[w]], dma=r_wu[w])
                    P.op("sp", lambda c=c: nc.sync.dma_start(out=wd[:, c, :], in_=cdn[c]), reads=[r_cdn[c]], writes=[r_wd[c]], dma=r_wd[c])
                for sg in range(NSG):
                    ss = slice(sg * SGW, (sg + 1) * SGW)
                    bg_, bu_ = sg * 2, sg * 2 + 1
                    for kc in range(KC):
                        P.op("pe", lambda w=w, kc=kc, ss=ss, bg_=bg_: T.matmul(bank[bg_][:, 0:SGW], lhsT=wg[w][:, kc, :], rhs=xs[:, kc, ss], start=(kc == 0), stop=(kc == KC - 1)),
                             reads=[r_wg[w], r_xs], writes=[rb[bg_]])
                    for kc in range(KC):
                        P.op("pe", lambda w=w, kc=kc, ss=ss, bu_=bu_: T.matmul(bank[bu_][:, 0:SGW], lhsT=wu[w][:, kc, :], rhs=xs[:, kc, ss], start=(kc == 0), stop=(kc == KC - 1)),
                             reads=[r_wu[w], r_xs], writes=[rb[bu_]])
                    q = ei % 2; ei += 1
                    P.op("dve", lambda q=q, c=c, bg_=bg_: V.tensor_scalar(out=gcl[q][:], in0=bank[bg_][:, 0:SGW], scalar1=bug[:, c:c + 1], scalar2=7.0, op0=ALU.add, op1=ALU.min),
                         reads=[r_bug], writes=[rb[bg_], r_gcl[q]])
                    P.op("dve", lambda q=q, c=c, bu_=bu_: V.tensor_scalar(out=ucl[q][:], in0=bank[bu_][:, 0:SGW], scalar1=bug[:, 16 + c:17 + c], scalar2=7.0, op0=ALU.add, op1=ALU.min),
                         reads=[r_bug], writes=[rb[bu_], r_ucl[q]])
                    P.op("act", lambda q=q: S.activation(out=sig[q][:], in_=gcl[q][:], func=AF.Sigmoid, scale=1.702), reads=[r_gcl[q]], writes=[r_sig[q]])
                    P.op("dve", lambda q=q: V.tensor_scalar(out=ucl[q][:], in0=ucl[q][:], scalar1=-7.0, scalar2=1.0, op0=ALU.max, op1=ALU.add), reads=[r_ucl[q]], writes=[r_ucl[q]])
                    P.op("dve", lambda q=q: V.tensor_tensor(out=gcl[q][:], in0=gcl[q][:], in1=sig[q][:], op=ALU.mult), reads=[r_gcl[q], r_sig[q]], writes=[r_gcl[q]])
                    P.op("dve", lambda q=q, c=c, ss=ss: V.tensor_tensor(out=actT[:, c, ss], in0=gcl[q][:], in1=ucl[q][:], op=ALU.mult), reads=[r_gcl[q], r_ucl[q]], writes=[r_actT[c]])
            for sb in range(NSB):
                yq = sb % 2
                for dg in range(4):
                    bk = 6 + (dg % 2)
                    for kc in range(KC):
                        P.op("pe", lambda sb=sb, dg=dg, kc=kc, bk=bk: T.matmul(bank[bk][:, :], lhsT=actT[:, kc, sb * 128:(sb + 1) * 128], rhs=wd[:, kc, dg * 512:(dg + 1) * 512],
                                                                             start=(kc == 0), stop=(kc == KC - 1)), reads=[r_actT[kc], r_wd[kc]], writes=[rb[bk]])
                    P.op("dve", lambda yq=yq, dg=dg, bk=bk: V.tensor_tensor(out=yst[yq][:, dg * 512:(dg + 1) * 512], in0=bank[bk][:, :], in1=bdb[:, dg * 512:(dg + 1) * 512], op=ALU.add),
                         reads=[r_bdb], writes=[rb[bk], r_yst[yq]])
                f = P.op("sp", lambda e=e, sb=sb, yq=yq, so=so: nc.sync.dma_start(out=yd[e, so + sb * 128:so + (sb + 1) * 128, :], in_=yst[yq][:]), reads=[r_yst[yq]], dma=r_yst[yq])
                finals.append(f)
        P.emit(final_ops=finals[-2:])
    return nc


def build_l4(TL, CAP):
    NTL = TL // 128
    nc = new_nc()
    NCK = (CAP + 127) // 128
    CW = CAP // NCK
    yd = nc.dram_tensor("yb", [NE, CAP, D], BF16, kind="ExternalInput").ap()
    Gd = nc.dram_tensor("G", [TL, NE], F32, kind="ExternalInput").ap()
    hrd = nc.dram_tensor("hres", [TL, D], F32, kind="ExternalInput").ap()
    bcd = nc.dram_tensor("bc", [128, 4 * D], F32, kind="ExternalInput").ap()
    cf32d = nc.dram_tensor("cf32", [128, 768], F32, kind="ExternalInput").ap()
    cbfd = nc.dram_tensor("cbf", [128, 384], BF16, kind="ExternalInput").ap()
    outd = nc.dram_tensor("out", [TL, D], F32, kind="ExternalOutput").ap()
    V, S, G_, T = nc.vector, nc.scalar, nc.gpsimd, nc.tensor
    with contextlib.ExitStack() as st:
        P = Prog(nc, st)
        R = P.res
        bank = [P.ps(f"bank{i}", [128, 512]) for i in range(8)]
        rb = [R(f"bank{i}") for i in range(8)]
        bc = P.sb("bct", [128, 4 * D], F32); r_bc = R()
        c32 = P.sb("c32", [128, 768], F32); r_c32 = R()
        cb = P.sb("cb", [128, 384], BF16); r_cb = R()
        IOTA = c32[:, 128:128 + CAP]
        Gt = P.sb("Gt", [128, NTL, NE], F32); r_G = R()
        mask32 = P.sb("mask32", [128, NTL, NE], F32); r_mask = R()
        maskb = P.sb("maskb", [128, NTL, NE], BF16); r_maskb = R()
        rk = P.sb("rk", [128, NTL, NE], F32); r_rk = R()
        acc = P.sb("acc", [128, NTL, D], F32); r_acc = [R() for _ in range(NTL)]
        yb = [P.sb(f"yb{i}", [128, NCK, D], BF16) for i in range(2)]; r_yb = [R(), R()]
        selw = [P.sb(f"selw{i}", [128, CAP], BF16) for i in range(2)]; r_selw = [R(), R()]
        swT = [P.sb(f"swT{i}", [128, NCK, 128], BF16) for i in range(2)]; r_swT = [R(), R()]
        hr = P.sb("hr", [128, D], F32); r_hr = R()
        tmp = P.sb("tmp", [128, D], F32); r_tmp = R()
        ot = [P.sb(f"ot{i}", [128, D], F32) for i in range(2)]; r_ot = [R(), R()]
        sm = [P.sb(f"sm{i}", [128, 8], F32) for i in range(2)]; r_sm = [R(), R()]
        P.op("sp", lambda: nc.sync.dma_start(out=bc[:], in_=bcd), writes=[r_bc], dma=r_bc)
        P.op("sp", lambda: nc.sync.dma_start(out=c32[:], in_=cf32d), writes=[r_c32], dma=r_c32)
        P.op("sp", lambda: nc.sync.dma_start(out=cb[:], in_=cbfd), writes=[r_cb], dma=r_cb)
        P.op("sp", lambda: nc.sync.dma_start(out=Gt[:], in_=Gd.rearrange("(i p) e -> p i e", p=128)), writes=[r_G], dma=r_G)
        P.op("pool", lambda: G_.memset(acc[:], 0.0), writes=r_acc)
        P.op("dve", lambda: V.scalar_tensor_tensor(out=bc[:, D:2 * D], in0=bc[:, 2 * D:3 * D], scalar=1.0, in1=bc[:, D:2 * D], op0=ALU.add, op1=ALU.mult),
             reads=[r_bc], writes=[r_bc])
        g2b, gsfb, shfb = bc[:, 0:D], bc[:, D:2 * D], bc[:, 3 * D:4 * D]
        P.op("dve", lambda: V.tensor_scalar(out=mask32[:], in0=Gt[:], scalar1=0.0, scalar2=None, op0=ALU.is_gt), reads=[r_G], writes=[r_mask])
        P.op("pool", lambda: G_.tensor_copy(out=maskb[:], in_=mask32[:]), reads=[r_mask], writes=[r_maskb])
        for i in range(NTL):
            rank_ops(P, nc, bank[6], rb[6], maskb, r_maskb, i, cb, r_cb, mask32[:, i, :], r_mask, rk[:, i, :], r_rk, lo=0)
        ui = 0
        for e in range(NE):
            k = e % 2
            if CAP % 128 == 0:
                P.op("sp", lambda k=k, e=e: nc.sync.dma_start(out=yb[k][:], in_=yd[e].rearrange("(ck p) d -> p ck d", p=128)), writes=[r_yb[k]], dma=r_yb[k])
            else:
                P.op("sp", lambda k=k, e=e: nc.sync.dma_start(out=yb[k][0:CW, 0, :], in_=yd[e]), writes=[r_yb[k]], dma=r_yb[k])
            for i in range(NTL):
                q = ui % 2; ui += 1
                P.op("dve", lambda q=q, i=i, e=e: V.tensor_scalar(out=selw[q][:], in0=IOTA, scalar1=rk[:, i, e:e + 1], scalar2=Gt[:, i, e:e + 1], op0=ALU.is_equal, op1=ALU.mult),
                     reads=[r_c32, r_rk, r_G], writes=[r_selw[q]])
                tbk = 4 + q
                tview = bank[tbk][0:CW, :].bitcast(BF16)
                for ck in range(NCK):
                    P.op("pe", lambda q=q, ck=ck, tview=tview: T.transpose(out=tview[:, ck * 128:(ck + 1) * 128], in_=selw[q][:, ck * CW:(ck + 1) * CW], identity=cb[:, 0:128]),
                         reads=[r_selw[q], r_cb], writes=[rb[tbk]])
                P.op("act", lambda q=q, tview=tview: S.copy(out=swT[q][0:CW, :, :], in_=tview[:, 0:NCK * 128].rearrange("p (a b) -> p a b", a=NCK)), writes=[rb[tbk], r_swT[q]])
                for dg in range(4):
                    for ck in range(NCK):
                        P.op("pe", lambda k=k, q=q, ck=ck, dg=dg: T.matmul(bank[dg][:, :], lhsT=swT[q][0:CW, ck, :], rhs=yb[k][0:CW, ck, dg * 512:(dg + 1) * 512],
                                                                         start=(ck == 0), stop=(ck == NCK - 1)), reads=[r_swT[q], r_yb[k]], writes=[rb[dg]])
                    ds = slice(dg * 512, (dg + 1) * 512)
                    P.op("dve", lambda i=i, dg=dg, ds=ds: V.tensor_tensor(out=acc[:, i, ds], in0=bank[dg][:, :], in1=acc[:, i, ds], op=ALU.add),
                         reads=[r_acc[i]], writes=[rb[dg], r_acc[i]])
        finals = []
        for i in range(NTL):
            p = i % 2
            tsl = slice(i * 128, (i + 1) * 128)
            smt, r_smt = sm[p], r_sm[p]
            P.op("sp", lambda tsl=tsl: nc.sync.dma_start(out=hr[:], in_=hrd[tsl, :]), writes=[r_hr], dma=r_hr)
            P.op("dve", lambda i=i: V.tensor_tensor(out=acc[:, i, :], in0=acc[:, i, :], in1=g2b, op=ALU.mult), reads=[r_bc, r_acc[i]], writes=[r_acc[i]])
            P.op("pool", lambda i=i: G_.tensor_tensor(out=acc[:, i, :], in0=acc[:, i, :], in1=hr[:], op=ALU.add), reads=[r_hr, r_acc[i]], writes=[r_acc[i]])
            P.op("pool", lambda i=i: G_.tensor_tensor(out=tmp[:], in0=acc[:, i, :], in1=acc[:, i, :], op=ALU.mult), reads=[r_acc[i]], writes=[r_tmp])
            P.op("dve", lambda smt=smt: V.tensor_reduce(out=smt[:, 0:1], in_=tmp[:], axis=AX.X, op=ALU.add), reads=[r_tmp], writes=[r_smt])
            P.op("act", lambda smt=smt: S.activation(out=smt[:, 1:2], in_=smt[:, 0:1], func=AF.Ln, bias=EPS, scale=1.0 / D), reads=[r_smt], writes=[r_smt])
            P.op("act", lambda smt=smt: S.activation(out=smt[:, 2:3], in_=smt[:, 1:2], func=AF.Exp, scale=-0.5), reads=[r_smt], writes=[r_smt])
            P.op("dve", lambda smt=smt, i=i: V.scalar_tensor_tensor(out=tmp[:], in0=acc[:, i, :], scalar=smt[:, 2:3], in1=gsfb, op0=ALU.mult, op1=ALU.mult),
                 reads=[r_acc[i], r_smt, r_bc, r_tmp], writes=[r_tmp])
            P.op("pool", lambda p=p: G_.tensor_tensor(out=ot[p][:], in0=tmp[:], in1=shfb, op=ALU.add), reads=[r_tmp, r_bc], writes=[r_ot[p]])
            f = P.op("sp", lambda p=p, tsl=tsl: nc.sync.dma_start(out=outd[tsl, :], in_=ot[p][:]), reads=[r_ot[p]], dma=r_ot[p])
            finals.append(f)
        P.emit(final_ops=finals[-2:])
    return nc


TL_FULL = SEQ // NCORES
CAP_FULL = 512
NCH_FULL = 4


_DBG = {}


def _bcst(v):
    return np.broadcast_to(np.asarray(v)[None, :], (128, v.shape[0]))


def _run(nc, in_maps):
    return run_bass_kernel_spmd(nc, in_maps, core_ids=list(range(NCORES))).results


def kernel(x, c, w_ada, b_ada, norm_mix, w_in, b_i, b_f, fox_b_f, fox_q_norm, fox_k_norm,
           mlstm_out_norm, fox_out_norm, w_out, norm_ffn, w_router, b_router, w_up_gate,
           b_up_gate, w_down, b_down, w_ada_final, b_ada_final, norm_final):
    f32 = lambda a: np.asarray(a, dtype=np.float32)
    x = f32(x); c = f32(c)
    seq = x.shape[1]
    TL = seq // NCORES
    CAP = CAP_FULL
    mod = run_l0(c, f32(w_ada), f32(b_ada), f32(w_ada_final), f32(b_ada_final))
    sh1, sc1, g1, sh2, sc2, g2 = [mod[i * D:(i + 1) * D] for i in range(6)]
    shf, scf = mod[6 * D:7 * D], mod[7 * D:8 * D]
    xT = np.ascontiguousarray(x[0].T)
    w_in0 = f32(w_in)[0]
    in1 = [l1_inputs(j, xT, w_in0, f32(norm_mix)[0], sc1, sh1, f32(b_i)[0], f32(b_f)[0], f32(fox_b_f)[0],
                     f32(fox_q_norm)[0], f32(fox_k_norm)[0], f32(fox_out_norm)[0], f32(mlstm_out_norm)[0]) for j in range(NCORES)]
    r1 = _run(build_l1(seq), in1)
    del in1, xT
    mix = np.empty((seq, D), dtype=ml_dtypes.bfloat16)
    for j in range(NCORES):
        h, half = j // 2, j % 2
        o = np.asarray(r1[j]["hout"])
        mix[:, 256 * h + 128 * half:256 * h + 128 * half + 128] = o[:, 0:128]
        mix[:, 1024 + 128 * j:1024 + 128 * j + 128] = o[:, 128:256]
    mixT = np.ascontiguousarray(mix.T)
    w_out0 = f32(w_out)[0]
    in2 = [l2_inputs(np.ascontiguousarray(mixT[:, g * TL:(g + 1) * TL]), np.ascontiguousarray(x[0, g * TL:(g + 1) * TL]), w_out0, g1,
                     f32(norm_ffn)[0], sc2, sh2, f32(w_router)[0], f32(b_router)[0]) for g in range(NCORES)]
    r2 = _run(build_l2(TL, CAP), in2)
    cf32, cbf = in2[0]["cf32"], in2[0]["cbf"]
    del in2
    in3 = []
    for cidx in range(NCORES):
        es = list(range(EPC * cidx, EPC * cidx + EPC))
        XT = np.ascontiguousarray(np.stack([np.concatenate([np.asarray(r2[g]["XT"])[e] for g in range(NCORES)], axis=1) for e in es]))
        in3.append({"XT": XT, "w_ug": np.ascontiguousarray(f32(w_up_gate)[0, es[0]:es[-1] + 1]),
                    "b_ug": np.ascontiguousarray(np.stack([f32(b_up_gate)[0, e].reshape(32, 128).T for e in es])),
                    "w_d": np.ascontiguousarray(f32(w_down)[0, es[0]:es[-1] + 1]),
                    "b_d": np.ascontiguousarray(np.stack([_bcst(f32(b_down)[0, e]) for e in es]))})
    r3 = _run(build_l3(NCORES * CAP, NCH_FULL), in3)
    del in3
    bc4 = np.ascontiguousarray(np.concatenate([_bcst(g2), _bcst(f32(norm_final)), _bcst(scf), _bcst(shf)], axis=1), dtype=np.float32)
    in4 = []
    for g in range(NCORES):
        yb = np.ascontiguousarray(np.stack([np.asarray(r3[e // EPC]["y"])[e % EPC][g * CAP:(g + 1) * CAP] for e in range(NE)]))
        in4.append({"yb": yb, "G": np.asarray(r2[g]["G"]), "hres": np.asarray(r2[g]["hres"]), "bc": bc4, "cf32": cf32, "cbf": cbf})
    r4 = _run(build_l4(TL, CAP), in4)
    out = np.concatenate([np.asarray(r["out"]) for r in r4], axis=0)[None]
    _DBG.update(mod=mod, mix=mix, G=np.concatenate([np.asarray(r2[g]["G"]) for g in range(NCORES)]), hres=np.concatenate([np.asarray(r2[g]["hres"]) for g in range(NCORES)]))
    return out.astype(np.float32)
```

```python
import contextlib
import numpy as np
import ml_dtypes
import concourse.bass as bass
import concourse.mybir as mybir
from concourse.bass_utils import run_bass_kernel_spmd

F32 = mybir.dt.float32
BF16 = mybir.dt.bfloat16
ALU = mybir.AluOpType
AF = mybir.ActivationFunctionType
AX = mybir.AxisListType
NCORES = 8

D = 2048
KC = D // 128
SEQ = 8192
EPS = 1e-6


class Res:
    __slots__ = ("name", "w", "r", "sem", "cnt")

    def __init__(self, name):
        self.name = name
        self.w = None
        self.r = []
        self.sem = None
        self.cnt = 0


class Prog:
    ENG = ("pe", "act", "dve", "pool", "sp")

    def __init__(self, nc, stack):
        self.nc = nc
        self.stack = stack
        self.ops = []
        self.e = {"pe": nc.tensor, "act": nc.scalar, "dve": nc.vector, "pool": nc.gpsimd, "sp": nc.sync}
        self.nres = 0

    def res(self, name=None):
        self.nres += 1
        return Res(name or f"r{self.nres}")

    def sb(self, name, shape, dt):
        t = self.stack.enter_context(self.nc.sbuf_tensor(name, list(shape), dt))
        return t

    def ps(self, name, shape, dt=F32):
        return self.stack.enter_context(self.nc.psum_tensor(name, list(shape), dt))

    deferred = None

    def op(self, eng, fn, reads=(), writes=(), dma=None):
        if self.deferred is not None:
            self.deferred.append((eng, fn, tuple(reads), tuple(writes), dma))
            return None
        i = len(self.ops)
        deps = set()
        for r in reads:
            if r.w is not None:
                deps.add(r.w)
        for w in writes:
            if w.w is not None:
                deps.add(w.w)
            deps.update(w.r)
        for r in reads:
            r.r.append(i)
        for w in writes:
            w.w = i
            w.r = []
        self.ops.append(dict(eng=eng, fn=fn, deps=deps, dma=dma, wset=set(id(w) for w in writes),
                             rset=set(id(r) for r in reads)))
        return i

    def emit(self, final_ops=()):
        nc = self.nc
        ops = self.ops
        def stream(o):
            return ("dma", id(o["dma"])) if o["dma"] is not None else o["eng"]
        seen = {e: {} for e in self.ENG}
        pos = {}
        kept = []
        signal = set()
        for i, o in enumerate(ops):
            k = {}
            for d in o["deps"]:
                od = ops[d]
                sd = stream(od)
                if sd == o["eng"] and od["dma"] is None:
                    if o["eng"] == "pe":
                        continue
                    if not (od["wset"] & o["rset"]):
                        continue
                k[sd] = max(k.get(sd, -1), d)
            kk = []
            for sd, d in k.items():
                if seen[o["eng"]].get(sd, -1) >= d:
                    continue
                seen[o["eng"]][sd] = d
                kk.append((sd, d))
                signal.add(d)
            kept.append(kk)
        for d in final_ops:
            signal.add(d)
        for i, o in enumerate(ops):
            if o["dma"] is not None:
                signal.add(i)
        sems = {}
        def sem_of(sd):
            if sd not in sems:
                sems[sd] = self.stack.enter_context(nc.semaphore(f"s{len(sems)}"))
            return sems[sd]
        val = {}
        cur = {}
        for i, o in enumerate(ops):
            if i in signal:
                sd = stream(o)
                inc = 16 if o["dma"] is not None else 1
                cur[sd] = cur.get(sd, 0) + inc
                val[i] = cur[sd]
        for i, o in enumerate(ops):
            eng = self.e[o["eng"]]
            for sd, d in kept[i]:
                eng.wait_ge(sem_of(sd), val[d])
            inst = o["fn"]()
            if i in signal:
                sd = stream(o)
                inst.then_inc(sem_of(sd), 16 if o["dma"] is not None else 1)
        for d in final_ops:
            self.e["sp"].wait_ge(sem_of(stream(ops[d])), val[d])


def new_nc():
    return bass.Bass("TRN2", target_bir_lowering=False)


def build_l0():
    nc = new_nc()
    NW = 2048
    c_in = nc.dram_tensor("c", [128, KC], F32, kind="ExternalInput").ap()
    w_in = nc.dram_tensor("w", [D, NW], F32, kind="ExternalInput").ap()
    b_in = nc.dram_tensor("b", [1, NW], F32, kind="ExternalInput").ap()
    out = nc.dram_tensor("mod", [1, NW], F32, kind="ExternalOutput").ap()
    with contextlib.ExitStack() as st:
        P = Prog(nc, st)
        ct = P.sb("ct", [128, KC], F32); r_ct = P.res()
        ca = P.sb("ca", [128, KC], F32); r_ca = P.res()
        bt = P.sb("bt", [1, NW], F32); r_bt = P.res()
        ot = P.sb("ot", [1, NW], F32); r_ot = P.res()
        wt = [P.sb(f"wt{i}", [128, KC, 512], F32) for i in range(2)]
        r_wt = [P.res() for _ in range(2)]
        pst = [P.ps(f"ps{i}", [1, 512]) for i in range(2)]
        r_ps = [P.res() for _ in range(2)]
        P.op("sp", lambda: nc.sync.dma_start(out=ct[:], in_=c_in), writes=[r_ct], dma=r_ct)
        P.op("sp", lambda: nc.sync.dma_start(out=bt[:], in_=b_in), writes=[r_bt], dma=r_bt)
        P.op("act", lambda: nc.scalar.activation(out=ca[:], in_=ct[:], func=AF.Silu), reads=[r_ct], writes=[r_ca])
        wv = w_in.rearrange("(kc p) n -> p kc n", p=128)
        for g in range(4):
            b = g % 2
            P.op("sp", lambda g=g, b=b: nc.sync.dma_start(out=wt[b][:], in_=wv[:, :, g * 512:(g + 1) * 512]),
                 writes=[r_wt[b]], dma=r_wt[b])
            for kc in range(KC):
                P.op("pe", lambda b=b, kc=kc: nc.tensor.matmul(pst[b][:], lhsT=ca[:, kc:kc + 1], rhs=wt[b][:, kc, :],
                                                               start=(kc == 0), stop=(kc == KC - 1)),
                     reads=[r_ca, r_wt[b]], writes=[r_ps[b]])
            P.op("dve", lambda g=g, b=b: nc.vector.tensor_tensor(out=ot[:, g * 512:(g + 1) * 512], in0=pst[b][:],
                                                                 in1=bt[:, g * 512:(g + 1) * 512], op=ALU.add),
                 reads=[r_ps[b], r_bt], writes=[r_ot])
        f = P.op("sp", lambda: nc.sync.dma_start(out=out, in_=ot[:]), reads=[r_ot], dma=r_ot)
        P.emit(final_ops=[f])
    return nc


def run_l0(c, w_ada, b_ada, w_ada_final, b_ada_final):
    wcat = np.concatenate([w_ada[0], w_ada_final], axis=1)
    bcat = np.concatenate([b_ada[0], b_ada_final], axis=0)
    cl = np.ascontiguousarray(c[0].reshape(KC, 128).T)
    in_maps = []
    for j in range(NCORES):
        in_maps.append({"c": cl, "w": np.ascontiguousarray(wcat[:, j * 2048:(j + 1) * 2048]),
                        "b": np.ascontiguousarray(bcat[None, j * 2048:(j + 1) * 2048])})
    res = run_bass_kernel_spmd(build_l0(), in_maps, core_ids=list(range(NCORES)))
    return np.concatenate([r["mod"][0] for r in res.results])


NC1 = 1027


def build_l1(seq):
    NT = seq // 128
    nc = new_nc()
    xT = nc.dram_tensor("xT", [D, seq], F32, kind="ExternalInput").ap()
    w1 = nc.dram_tensor("w1", [D, NC1], F32, kind="ExternalInput").ap()
    v16d = nc.dram_tensor("v16", [128, 48], F32, kind="ExternalInput").ap()
    scald = nc.dram_tensor("scal", [1, 3], F32, kind="ExternalInput").ap()
    gvecd = nc.dram_tensor("gvec", [128, 512], F32, kind="ExternalInput").ap()
    cf32d = nc.dram_tensor("cf32", [128, 256], F32, kind="ExternalInput").ap()
    cbfd = nc.dram_tensor("cbf", [128, 384], BF16, kind="ExternalInput").ap()
    hout = nc.dram_tensor("hout", [seq, 256], BF16, kind="ExternalOutput").ap()
    V, S, G, T = nc.vector, nc.scalar, nc.gpsimd, nc.tensor
    with contextlib.ExitStack() as st:
        P = Prog(nc, st)
        R = P.res
        bank = [P.ps(f"bank{i}", [128, 512]) for i in range(8)]
        rb = [R(f"bank{i}") for i in range(8)]
        v16 = P.sb("v16t", [128, 48], F32); r_v16 = R()
        gs = P.sb("gs", [128, 16], F32); r_gs = R()
        scal = P.sb("scalt", [1, 3], F32); r_scal = R()
        gv = P.sb("gv", [128, 512], F32); r_gv = R()
        c32 = P.sb("c32", [128, 256], F32); r_c32 = R()
        cb = P.sb("cb", [128, 384], BF16); r_cb = R()
        UT32, ONE32 = c32[:, 0:128], c32[:, 128:256]
        IDB, UTB, ONEB = cb[:, 0:128], cb[:, 128:256], cb[:, 256:384]
        wst = [P.sb(f"wst{i}", [128, NC1], F32) for i in range(2)]; r_wst = [R(), R()]
        Wb = P.sb("Wb", [128, KC, NC1 + 1], BF16); r_Wb = R()
        shWrow = P.sb("shWrow", [1, NC1], F32); r_shWrow = R()
        shWb = P.sb("shWb", [128, NC1], F32); r_shWb = R()
        XG = 256
        xst = [P.sb(f"xst{i}", [128, KC, XG], F32) for i in range(2)]; r_xst = [R(), R()]
        xb = [P.sb(f"xb{i}", [128, KC, XG], BF16) for i in range(2)]; r_xb = [R(), R()]
        sq = [P.sb(f"sq{i}", [128, KC, XG], BF16) for i in range(2)]; r_sq = [R(), R()]
        pj = [P.sb(f"pj{i}", [128, NC1], F32) for i in range(2)]; r_pj = [R(), R()]
        kT = P.sb("kT", [128, seq], BF16); r_kT = [R() for _ in range(NT)]
        vaug = P.sb("vaug", [128, NT, 130], BF16); r_v = [R() for _ in range(NT)]
        cmat = P.sb("cmat", [128, NT], F32); r_cmat = R()
        nU = P.sb("nU", [128, NT], F32); r_nU = R()
        biasm = [P.sb(f"biasm{i}", [128, NT], F32) for i in range(2)]; r_bias = [R(), R()]
        two = lambda nm, shp, dt: ([P.sb(f"{nm}{i}", shp, dt) for i in range(2)], [R(), R()])
        qn, r_qn = two("qn", [128, 128], BF16)
        kn, r_kn = two("kn", [128, 128], BF16)
        qT, r_qT = two("qT", [128, 128], BF16)
        mqb, r_mqb = two("mqb", [128, 128], BF16)
        kab, r_kab = two("kab", [128, 128], BF16)
        mqT, r_mqT = two("mqT", [128, 128], BF16)
        kaT, r_kaT = two("kaT", [128, 128], BF16)
        vm, r_vm = two("vm", [128, 258], BF16)
        sg, r_sg = two("sg", [128, 128], F32)
        SmT, r_SmT = two("SmT", [128, 128], BF16)
        outt, r_outt = two("outt", [128, 256], BF16)
        NPT = 3
        PT = [P.sb(f"PT{i}", [128, 128], BF16) for i in range(NPT)]; r_PT = [R() for _ in range(NPT)]
        junk = P.sb("junk", [128, 256], F32); r_junk = R()
        junk2 = P.sb("junk2", [128, 256], F32); r_junk2 = R()
        sm, r_sm = two("sm", [128, 32], F32)
        Cst = P.sb("Cst", [128, 257], F32); r_C = R()
        Ctmp = P.sb("Ctmp", [128, 257], F32); r_Ctmp = R()
        Cb = P.sb("Cb", [128, 258], BF16); r_Cb = R()
        h_m = P.sb("h_m", [128, 256], F32); r_hm = R()
        hfu = P.sb("hfu", [128, 128], F32); r_hfu = R()
        otmp = P.sb("otmp", [128, 128], F32); r_otmp = R()

        P.op("sp", lambda: nc.sync.dma_start(out=v16[:], in_=v16d), writes=[r_v16], dma=r_v16)
        P.op("sp", lambda: nc.sync.dma_start(out=scal[:], in_=scald), writes=[r_scal], dma=r_scal)
        P.op("sp", lambda: nc.sync.dma_start(out=gv[:], in_=gvecd), writes=[r_gv], dma=r_gv)
        P.op("sp", lambda: nc.sync.dma_start(out=c32[:], in_=cf32d), writes=[r_c32], dma=r_c32)
        P.op("sp", lambda: nc.sync.dma_start(out=cb[:], in_=cbfd), writes=[r_cb], dma=r_cb)
        P.op("dve", lambda: V.tensor_scalar(out=gs[:], in0=v16[:, 16:32], scalar1=1.0, scalar2=None, op0=ALU.add),
             reads=[r_v16], writes=[r_gs])
        P.op("dve", lambda: V.tensor_tensor(out=gs[:], in0=gs[:], in1=v16[:, 0:16], op=ALU.mult),
             reads=[r_v16, r_gs], writes=[r_gs])
        P.op("dve", lambda: V.tensor_scalar(out=gv[:, 0:128], in0=gv[:, 0:128], scalar1=128.0 ** -0.5, scalar2=None,
                                            op0=ALU.mult), reads=[r_gv], writes=[r_gv])
        P.op("pool", lambda: G.memset(vaug[:], 1.0), writes=r_v)
        for i in range(2):
            P.op("pool", lambda i=i: G.memset(vm[i][:], 1.0), writes=[r_vm[i]])
        P.op("pool", lambda: G.memset(Cst[:], 0.0), writes=[r_C])
        P.op("pool", lambda: G.memset(Cb[:], 0.0), writes=[r_Cb])
        P.op("pool", lambda: G.memset(nU[:], 0.0), writes=[r_nU])
        groups = [(0, 512, 0), (512, 1024, 1), (1024, NC1, 2)]
        for kc in range(KC):
            b = kc % 2
            P.op("sp", lambda kc=kc, b=b: nc.sync.dma_start(out=wst[b][:], in_=w1[kc * 128:(kc + 1) * 128, :]),
                 writes=[r_wst[b]], dma=r_wst[b])
            for (c0, c1, bk) in groups:
                P.op("pe", lambda kc=kc, b=b, c0=c0, c1=c1, bk=bk: T.matmul(
                    bank[bk][0:1, 0:c1 - c0], lhsT=v16[:, 32 + kc:33 + kc], rhs=wst[b][:, c0:c1],
                    start=(kc == 0), stop=(kc == KC - 1)), reads=[r_v16, r_wst[b]], writes=[rb[bk]])
            P.op("dve", lambda kc=kc, b=b: V.tensor_scalar(out=Wb[:, kc, 0:NC1], in0=wst[b][:], scalar1=gs[:, kc:kc + 1],
                                                          scalar2=None, op0=ALU.mult),
                 reads=[r_wst[b], r_gs], writes=[r_Wb])
        for (c0, c1, bk) in groups:
            P.op("dve", lambda c0=c0, c1=c1, bk=bk: V.tensor_copy(out=shWrow[:, c0:c1], in_=bank[bk][0:1, 0:c1 - c0]),
                 reads=[], writes=[rb[bk], r_shWrow])
        P.op("dve", lambda: V.tensor_tensor(out=shWrow[:, 1024:1027], in0=shWrow[:, 1024:1027], in1=scal[:, 0:3], op=ALU.add),
             reads=[r_scal, r_shWrow], writes=[r_shWrow])
        for (c0, c1, bk) in groups:
            P.op("pe", lambda c0=c0, c1=c1, bk=bk: T.matmul(bank[bk][:, 0:c1 - c0], lhsT=c32[0:1, 128:256],
                                                          rhs=shWrow[0:1, c0:c1], start=True, stop=True),
                 reads=[r_c32, r_shWrow], writes=[rb[bk]])
            P.op("dve", lambda c0=c0, c1=c1, bk=bk: V.tensor_copy(out=shWb[:, c0:c1], in_=bank[bk][:, 0:c1 - c0]),
                 writes=[rb[bk], r_shWb])

        xv = xT.rearrange("(kc p) t -> p kc t", p=128)
        trp = bank[2][:, 256:512].bitcast(BF16)
        psS = bank[2]
        final = []
        ptk = 0
        pend = []

        def stageA1(i):
            p = i % 2
            gb = (i // 2) % 2
            if i % 2 == 0:
                gi = i // 2
                P.op("sp", lambda gi=gi, gb=gb: nc.sync.dma_start(out=xst[gb][:], in_=xv[:, :, gi * XG:(gi + 1) * XG]),
                     writes=[r_xst[gb]], dma=r_xst[gb])
                P.op("act", lambda gb=gb: S.copy(out=xb[gb][:], in_=xst[gb][:]), reads=[r_xst[gb]], writes=[r_xb[gb]])
                P.op("pool", lambda gb=gb: G.tensor_tensor(out=sq[gb][:], in0=xst[gb][:], in1=xst[gb][:], op=ALU.mult),
                     reads=[r_xst[gb]], writes=[r_sq[gb]])
            ts = slice((i % 2) * 128, (i % 2) * 128 + 128)
            smt, r_smt = sm[p], r_sm[p]
            for kc in range(KC):
                P.op("pe", lambda gb=gb, kc=kc, ts=ts: T.matmul(psS[:, 8:9], lhsT=sq[gb][:, kc, ts], rhs=cb[:, 256:257],
                                                              start=(kc == 0), stop=(kc == KC - 1)),
                     reads=[r_sq[gb], r_cb], writes=[rb[2]])
            P.op("act", lambda smt=smt: S.activation(out=smt[:, 0:1], in_=psS[:, 8:9], func=AF.Ln, bias=EPS, scale=1.0 / D),
                 writes=[rb[2], r_smt])
            P.op("act", lambda smt=smt: S.activation(out=smt[:, 1:2], in_=smt[:, 0:1], func=AF.Exp, scale=-0.5),
                 reads=[r_smt], writes=[r_smt])
            for (c0, c1, bk) in groups:
                for kc in range(KC):
                    P.op("pe", lambda gb=gb, kc=kc, ts=ts, c0=c0, c1=c1, bk=bk: T.matmul(
                        bank[bk][:, 0:c1 - c0], lhsT=xb[gb][:, kc, ts], rhs=Wb[:, kc, c0:c1],
                        start=(kc == 0), stop=(kc == KC - 1)), reads=[r_xb[gb], r_Wb], writes=[rb[bk]])
            for (c0, c1, bk) in groups:
                P.op("dve", lambda p=p, smt=smt, c0=c0, c1=c1, bk=bk: V.scalar_tensor_tensor(
                    out=pj[p][:, c0:c1], in0=bank[bk][:, 0:c1 - c0], scalar=smt[:, 1:2], in1=shWb[:, c0:c1],
                    op0=ALU.mult, op1=ALU.add), reads=[r_smt, r_shWb], writes=[rb[bk], r_pj[p]])
            pjt, r_pjt = pj[p], r_pj[p]
            P.deferred = pend
            P.op("act", lambda pjt=pjt, smt=smt: S.activation(out=smt[:, 2:4], in_=pjt[:, 1024:1026], func=AF.Exp, scale=2.0 / 15.0),
                 reads=[r_pjt], writes=[r_smt])
            P.op("dve", lambda smt=smt: V.tensor_scalar(out=smt[:, 2:4], in0=smt[:, 2:4], scalar1=1.0, scalar2=None, op0=ALU.add),
                 reads=[r_smt], writes=[r_smt])
            P.op("dve", lambda smt=smt: V.reciprocal(out=smt[:, 2:4], in_=smt[:, 2:4]), reads=[r_smt], writes=[r_smt])
            P.op("dve", lambda smt=smt: V.tensor_scalar(out=smt[:, 4:6], in0=smt[:, 2:4], scalar1=-30.0, scalar2=15.0,
                                                       op0=ALU.mult, op1=ALU.add), reads=[r_smt], writes=[r_smt])
            P.op("dve", lambda smt=smt, pjt=pjt: V.tensor_copy(out=smt[:, 6:7], in_=pjt[:, 1026:1027]), reads=[r_pjt, r_smt], writes=[r_smt])
            P.op("act", lambda smt=smt: S.activation(out=smt[:, 8:10], in_=smt[:, 5:7], func=AF.Exp, scale=-1.0),
                 reads=[r_smt], writes=[r_smt])
            P.op("act", lambda smt=smt: S.activation(out=smt[:, 8:10], in_=smt[:, 8:10], func=AF.Ln, bias=1.0, scale=1.0),
                 reads=[r_smt], writes=[r_smt])
            P.op("pool", lambda pjt=pjt: G.tensor_tensor(out=junk[:], in0=pjt[:, 0:256], in1=pjt[:, 0:256], op=ALU.mult),
                 reads=[r_pjt], writes=[r_junk])
            P.op("dve", lambda smt=smt: V.tensor_reduce(out=smt[:, 18:20], in_=junk[:].rearrange("p (a b) -> p a b", a=2), axis=AX.X, op=ALU.add),
                 reads=[r_junk, r_smt], writes=[r_smt])
            P.op("act", lambda smt=smt: S.activation(out=smt[:, 18:20], in_=smt[:, 18:20], func=AF.Ln, bias=EPS, scale=1.0 / 128), reads=[r_smt], writes=[r_smt])
            P.op("act", lambda smt=smt: S.activation(out=smt[:, 20:22], in_=smt[:, 18:20], func=AF.Exp, scale=-0.5), reads=[r_smt], writes=[r_smt])
            P.op("dve", lambda p=p, pjt=pjt, smt=smt: V.scalar_tensor_tensor(out=qn[p][:], in0=pjt[:, 0:128], scalar=smt[:, 20:21], in1=gv[:, 0:128],
                                                                          op0=ALU.mult, op1=ALU.mult), reads=[r_pjt, r_smt, r_gv], writes=[r_qn[p]])
            P.op("dve", lambda p=p, pjt=pjt, smt=smt: V.scalar_tensor_tensor(out=kn[p][:], in0=pjt[:, 128:256], scalar=smt[:, 21:22], in1=gv[:, 128:256],
                                                                          op0=ALU.mult, op1=ALU.mult), reads=[r_pjt, r_smt, r_gv], writes=[r_kn[p]])
            P.op("pool", lambda p=p, pjt=pjt: G.tensor_scalar(out=mqb[p][:], in0=pjt[:, 384:512], scalar1=128.0 ** -0.5, scalar2=None, op0=ALU.mult),
                 reads=[r_pjt], writes=[r_mqb[p]])
            P.op("pool", lambda p=p, pjt=pjt: G.tensor_copy(out=vm[p][:, 0:256], in_=pjt[:, 640:896]), reads=[r_pjt], writes=[r_vm[p]])
            P.op("pool", lambda i=i, pjt=pjt: G.tensor_copy(out=vaug[:, i, 0:128], in_=pjt[:, 256:384]), reads=[r_pjt], writes=[r_v[i]])
            P.op("act", lambda p=p, pjt=pjt: S.activation(out=sg[p][:], in_=pjt[:, 896:1024], func=AF.Exp, scale=-1.0), reads=[r_pjt], writes=[r_sg[p]])
            P.op("pool", lambda p=p: G.tensor_scalar(out=sg[p][:], in0=sg[p][:], scalar1=1.0, scalar2=None, op0=ALU.add), reads=[r_sg[p]], writes=[r_sg[p]])
            P.op("dve", lambda p=p: V.reciprocal(out=sg[p][:], in_=sg[p][:]), reads=[r_sg[p]], writes=[r_sg[p]])
            P.deferred = None

        def flush(n=None):
            k = len(pend) if n is None else min(n, len(pend))
            for _ in range(k):
                a = pend.pop(0)
                P.op(a[0], a[1], reads=a[2], writes=a[3], dma=a[4])

        def stageA2(i):
            p = i % 2
            gb = (i // 2) % 2
            ts = slice((i % 2) * 128, (i % 2) * 128 + 128)
            smt, r_smt = sm[p], r_sm[p]
            pjt, r_pjt = pj[p], r_pj[p]
            A_, B_, DEC_ = smt[:, 15:16], smt[:, 16:17], smt[:, 17:18]
            P.op("pe", lambda smt=smt: T.matmul(psS[:, 10:12], lhsT=UT32, rhs=smt[:, 8:10], start=True, stop=True),
                 reads=[r_c32, r_smt], writes=[rb[2]])
            P.op("pe", lambda smt=smt: T.matmul(psS[:, 12:14], lhsT=ONE32, rhs=smt[:, 8:10], start=True, stop=True),
                 reads=[r_c32, r_smt], writes=[rb[2]])
            P.op("dve", lambda smt=smt: V.tensor_copy(out=smt[:, 10:14], in_=psS[:, 10:14]), reads=[r_smt], writes=[rb[2], r_smt])
            P.op("dve", lambda smt=smt: V.tensor_tensor(out=smt[:, 14:15], in0=smt[:, 4:5], in1=smt[:, 10:11], op=ALU.add),
                 reads=[r_smt], writes=[r_smt])
            P.op("act", lambda smt=smt: S.activation(out=smt[:, 15:16], in_=smt[:, 14:15], func=AF.Exp), reads=[r_smt], writes=[r_smt])
            P.op("act", lambda smt=smt: S.activation(out=smt[:, 16:17], in_=smt[:, 10:11], func=AF.Exp, scale=-1.0), reads=[r_smt], writes=[r_smt])
            P.op("act", lambda smt=smt: S.activation(out=smt[:, 17:18], in_=smt[:, 12:13], func=AF.Exp, scale=-1.0), reads=[r_smt], writes=[r_smt])
            A_, B_, DEC_ = smt[:, 15:16], smt[:, 16:17], smt[:, 17:18]
            P.op("dve", lambda smt=smt, i=i: V.tensor_copy(out=cmat[:, i:i + 1], in_=smt[:, 11:12]), reads=[r_smt], writes=[r_cmat])
            P.op("dve", lambda smt=smt, i=i: V.tensor_scalar(out=nU[:, 0:i + 1], in0=nU[:, 0:i + 1], scalar1=smt[:, 13:14], scalar2=None,
                                                            op0=ALU.add), reads=[r_smt, r_nU], writes=[r_nU])
            P.op("dve", lambda p=p, i=i: V.tensor_tensor(out=biasm[p][:, 0:i + 1], in0=cmat[:, 0:i + 1], in1=nU[:, 0:i + 1], op=ALU.subtract),
                 reads=[r_cmat, r_nU], writes=[r_bias[p]])
            for k, (src, r_src) in enumerate([(qn[p], r_qn[p]), (kn[p], r_kn[p])]):
                P.op("pe", lambda k=k, src=src: T.transpose(out=trp[:, k * 128:(k + 1) * 128], in_=src[:], identity=IDB),
                     reads=[r_src, r_cb], writes=[rb[2]])
            P.op("act", lambda p=p: S.copy(out=qT[p][:], in_=trp[:, 0:128]), writes=[rb[2], r_qT[p]])
            P.op("act", lambda i=i: S.copy(out=kT[:, i * 128:(i + 1) * 128], in_=trp[:, 128:256]), writes=[rb[2], r_kT[i]])
            P.op("dve", lambda p=p, pjt=pjt, A_=A_: V.tensor_scalar(out=kab[p][:], in0=pjt[:, 512:640], scalar1=A_, scalar2=None, op0=ALU.mult),
                 reads=[r_pjt, r_smt], writes=[r_kab[p]])
            for k, (src, r_src) in [(2, (mqb[p], r_mqb[p])), (3, (kab[p], r_kab[p]))]:
                P.op("pe", lambda k=k, src=src: T.transpose(out=trp[:, k * 128:(k + 1) * 128], in_=src[:], identity=IDB),
                     reads=[r_src, r_cb], writes=[rb[2]])
            P.op("dve", lambda p=p: V.tensor_copy(out=mqT[p][:], in_=trp[:, 256:384]), writes=[rb[2], r_mqT[p]])
            P.op("dve", lambda p=p: V.tensor_copy(out=kaT[p][:], in_=trp[:, 384:512]), writes=[rb[2], r_kaT[p]])
            P.op("pe", lambda p=p: T.matmul(bank[3][:, 0:257], lhsT=kab[p][:], rhs=vm[p][:, 0:257], start=True, stop=True),
                 reads=[r_kab[p], r_vm[p]], writes=[rb[3]])
            P.op("pe", lambda p=p: T.matmul(bank[4][:, 0:128], lhsT=kaT[p][:], rhs=mqT[p][:], start=True, stop=True),
                 reads=[r_kaT[p], r_mqT[p]], writes=[rb[4]])
            P.op("dve", lambda p=p: V.tensor_tensor(out=SmT[p][:], in0=bank[4][:, 0:128], in1=UT32, op=ALU.mult),
                 reads=[r_c32], writes=[rb[4], r_SmT[p]])
            P.op("pe", lambda p=p: T.matmul(bank[4][:, 128:385], lhsT=mqT[p][:], rhs=Cb[:, 0:257], start=True, stop=False),
                 reads=[r_mqT[p], r_Cb], writes=[rb[4]])
            P.op("pe", lambda p=p: T.matmul(bank[4][:, 128:385], lhsT=SmT[p][:], rhs=vm[p][:, 0:257], start=False, stop=True),
                 reads=[r_SmT[p], r_vm[p]], writes=[rb[4]])
            P.op("dve", lambda: V.tensor_tensor(out=Ctmp[:], in0=bank[3][:, 0:257], in1=Cst[:], op=ALU.add),
                 reads=[r_C], writes=[rb[3], r_Ctmp])
            P.op("dve", lambda DEC_=DEC_: V.tensor_scalar(out=Cst[:], in0=Ctmp[:], scalar1=DEC_, scalar2=None, op0=ALU.mult),
                 reads=[r_Ctmp, r_smt], writes=[r_C])
            P.op("pool", lambda: G.tensor_copy(out=Cb[:, 0:257], in_=Cst[:]), reads=[r_C], writes=[r_Cb])
            P.op("dve", lambda smt=smt, B_=B_: V.tensor_scalar(out=smt[:, 22:23], in0=bank[4][:, 384:385], scalar1=B_, scalar2=None,
                                                             op0=ALU.mult), reads=[r_smt], writes=[rb[4], r_smt])
            P.op("dve", lambda smt=smt: V.tensor_scalar(out=smt[:, 30:31], in0=smt[:, 22:23], scalar1=-1.0, scalar2=1.0,
                                                       op0=ALU.mult, op1=ALU.max), reads=[r_smt], writes=[r_smt])
            P.op("dve", lambda smt=smt: V.tensor_tensor(out=smt[:, 22:23], in0=smt[:, 22:23], in1=smt[:, 30:31], op=ALU.max),
                 reads=[r_smt], writes=[r_smt])
            P.op("dve", lambda smt=smt: V.reciprocal(out=smt[:, 23:24], in_=smt[:, 22:23]), reads=[r_smt], writes=[r_smt])
            P.op("dve", lambda smt=smt, B_=B_: V.tensor_tensor(out=smt[:, 24:25], in0=smt[:, 23:24], in1=B_, op=ALU.mult), reads=[r_smt], writes=[r_smt])
            P.op("dve", lambda smt=smt: V.tensor_scalar(out=h_m[:], in0=bank[4][:, 128:384], scalar1=smt[:, 24:25], scalar2=None, op0=ALU.mult),
                 reads=[r_smt], writes=[rb[4], r_hm])
            P.op("pool", lambda: G.tensor_tensor(out=junk2[:], in0=h_m[:], in1=h_m[:], op=ALU.mult), reads=[r_hm], writes=[r_junk2])
            P.op("dve", lambda smt=smt: V.tensor_reduce(out=smt[:, 25:26], in_=junk2[:], axis=AX.X, op=ALU.add), reads=[r_junk2, r_smt], writes=[r_smt])
            P.op("act", lambda smt=smt: S.activation(out=smt[:, 25:26], in_=smt[:, 25:26], func=AF.Ln, bias=EPS, scale=1.0 / 256), reads=[r_smt], writes=[r_smt])
            P.op("act", lambda smt=smt: S.activation(out=smt[:, 26:27], in_=smt[:, 25:26], func=AF.Exp, scale=-0.5), reads=[r_smt], writes=[r_smt])
            P.op("dve", lambda smt=smt: V.scalar_tensor_tensor(out=otmp[:], in0=h_m[:, 0:128], scalar=smt[:, 26:27], in1=gv[:, 384:512],
                                                             op0=ALU.mult, op1=ALU.mult), reads=[r_hm, r_smt, r_gv], writes=[r_otmp])
            P.op("dve", lambda p=p: V.tensor_tensor(out=outt[p][:, 0:128], in0=otmp[:], in1=sg[p][:], op=ALU.mult),
                 reads=[r_otmp, r_sg[p]], writes=[r_outt[p]])
            P.deferred = None

        def stageB(i):
            nonlocal ptk
            p = i % 2
            gb = (i // 2) % 2
            ts = slice((i % 2) * 128, (i % 2) * 128 + 128)
            smt, r_smt = sm[p], r_sm[p]
            pjt, r_pjt = pj[p], r_pj[p]
            A_, B_, DEC_ = smt[:, 15:16], smt[:, 16:17], smt[:, 17:18]
            def qk(sb, p=p):
                pb = 5 + (sb % 2)
                P.op("pe", lambda: T.matmul(bank[pb][:, 0:128], lhsT=kT[:, sb * 128:(sb + 1) * 128], rhs=qT[p][:], start=True, stop=True),
                     reads=[r_kT[sb], r_qT[p]], writes=[rb[pb]])
            def pv(sb, k, p=p, i=i):
                pb = 5 + (sb % 2)
                P.op("act", lambda: S.activation(out=PT[k][:], in_=bank[pb][:, 0:128], func=AF.Exp, bias=biasm[p][:, sb:sb + 1], scale=1.0),
                     reads=[r_bias[p]], writes=[rb[pb], r_PT[k]])
                if sb == i:
                    P.op("pool", lambda: G.tensor_tensor(out=PT[k][:], in0=PT[k][:], in1=UTB, op=ALU.mult), reads=[r_cb, r_PT[k]], writes=[r_PT[k]])
                P.op("pe", lambda: T.matmul(bank[7][:, 0:129], lhsT=PT[k][:], rhs=vaug[:, sb, 0:129], start=(sb == 0), stop=(sb == i)),
                     reads=[r_PT[k], r_v[sb]], writes=[rb[7]])
            share = (len(pend) + i) // (i + 1)
            qk(0)
            for sb in range(i + 1):
                if sb + 1 <= i:
                    qk(sb + 1)
                pv(sb, ptk % NPT)
                ptk += 1
                flush(share)
            flush()
            P.op("dve", lambda smt=smt: V.reciprocal(out=smt[:, 27:28], in_=bank[7][:, 128:129]), reads=[r_smt], writes=[rb[7], r_smt])
            P.op("dve", lambda smt=smt: V.tensor_scalar(out=hfu[:], in0=bank[7][:, 0:128], scalar1=smt[:, 27:28], scalar2=None, op0=ALU.mult),
                 reads=[r_smt], writes=[rb[7], r_hfu])
            P.op("pool", lambda: G.tensor_tensor(out=junk2[:, 0:128], in0=hfu[:], in1=hfu[:], op=ALU.mult), reads=[r_hfu], writes=[r_junk2])
            P.op("dve", lambda smt=smt: V.tensor_reduce(out=smt[:, 28:29], in_=junk2[:, 0:128], axis=AX.X, op=ALU.add), reads=[r_junk2, r_smt], writes=[r_smt])
            P.op("act", lambda smt=smt: S.activation(out=smt[:, 28:29], in_=smt[:, 28:29], func=AF.Ln, bias=EPS, scale=1.0 / 128), reads=[r_smt], writes=[r_smt])
            P.op("act", lambda smt=smt: S.activation(out=smt[:, 29:30], in_=smt[:, 28:29], func=AF.Exp, scale=-0.5), reads=[r_smt], writes=[r_smt])
            P.op("dve", lambda p=p, smt=smt: V.scalar_tensor_tensor(out=outt[p][:, 128:256], in0=hfu[:], scalar=smt[:, 29:30], in1=gv[:, 256:384],
                                                                  op0=ALU.mult, op1=ALU.mult), reads=[r_hfu, r_smt, r_gv], writes=[r_outt[p]])
            f = P.op("sp", lambda p=p, i=i: nc.sync.dma_start(out=hout[i * 128:(i + 1) * 128, :], in_=outt[p][:]), reads=[r_outt[p]], dma=r_outt[p])
            final.append(f)
        stageA1(0)
        flush()
        stageA2(0)
        for i in range(NT):
            if i + 1 < NT:
                stageA1(i + 1)
            stageB(i)
            if i + 1 < NT:
                stageA2(i + 1)
        P.emit(final_ops=final[-2:])
    return nc


W_OFF = dict(mq=0, mk=512, mv=1024, mo=2048, mi=3072, mf=3076, fq=3080, fk=4104, fv=5128, ff=6152)


def l1_inputs(j, xT, w_in, norm_mix, sc1, sh1, b_i, b_f, fox_b_f, gq, gk, gfo, gmo):
    h, half = j // 2, j % 2
    o = W_OFF
    cols = np.concatenate([
        np.arange(o["fq"] + 128 * j, o["fq"] + 128 * j + 128), np.arange(o["fk"] + 128 * j, o["fk"] + 128 * j + 128),
        np.arange(o["fv"] + 128 * j, o["fv"] + 128 * j + 128), np.arange(o["mq"] + 128 * h, o["mq"] + 128 * h + 128),
        np.arange(o["mk"] + 128 * h, o["mk"] + 128 * h + 128),
        np.arange(o["mv"] + 256 * h + 128 * half, o["mv"] + 256 * h + 128 * half + 128),
        np.arange(o["mv"] + 256 * h + 128 * (1 - half), o["mv"] + 256 * h + 128 * (1 - half) + 128),
        np.arange(o["mo"] + 256 * h + 128 * half, o["mo"] + 256 * h + 128 * half + 128),
        [o["mi"] + h, o["mf"] + h, o["ff"] + j]]).astype(np.int64)
    lay = lambda v: v.reshape(KC, 128).T
    v16 = np.ascontiguousarray(np.concatenate([lay(norm_mix), lay(sc1), lay(sh1)], axis=1), dtype=np.float32)
    scal = np.array([[b_i[h], b_f[h], fox_b_f[j]]], np.float32)
    bc = lambda v: np.broadcast_to(v[None, :], (128, v.shape[0]))
    gvec = np.ascontiguousarray(np.concatenate([bc(gq), bc(gk), bc(gfo[j]), bc(gmo[h, 128 * half:128 * half + 128])], axis=1), dtype=np.float32)
    ut = np.triu(np.ones((128, 128), np.float32))
    cf32 = np.ascontiguousarray(np.concatenate([ut, np.ones((128, 128), np.float32)], axis=1))
    cbf = np.ascontiguousarray(np.concatenate([np.eye(128, dtype=np.float32), ut, np.ones((128, 128), np.float32)], axis=1)).astype(ml_dtypes.bfloat16)
    return {"xT": xT, "w1": np.ascontiguousarray(w_in[:, cols]), "v16": v16, "scal": scal, "gvec": gvec, "cf32": cf32, "cbf": cbf}


NE = 32


def rank_ops(P, nc, bankr, rbr, maskb, r_maskb, i, cb, r_cb, mask32, r_mask, rk_out, r_rk, lo=32):
    V, T = nc.vector, nc.tensor
    for ii in range(i + 1):
        lhs = cb[:, 256:384] if ii < i else cb[:, 128:256]
        P.op("pe", lambda ii=ii, lhs=lhs: T.matmul(bankr[:, lo:lo + NE], lhsT=lhs, rhs=maskb[:, ii, :], start=(ii == 0), stop=(ii == i)),
             reads=[r_cb, r_maskb], writes=[rbr])
    P.op("dve", lambda: V.scalar_tensor_tensor(out=rk_out, in0=bankr[:, lo:lo + NE], scalar=1.0, in1=mask32, op0=ALU.add, op1=ALU.mult),
         reads=[r_mask], writes=[rbr, r_rk])
    P.op("dve", lambda: V.tensor_scalar(out=rk_out, in0=rk_out, scalar1=-1.0, scalar2=None, op0=ALU.add), reads=[r_rk], writes=[r_rk])


def build_l2(TL, CAP):
    NTL = TL // 128
    nc = new_nc()
    mixT = nc.dram_tensor("mixT", [D, TL], BF16, kind="ExternalInput").ap()
    xd = nc.dram_tensor("x", [TL, D], F32, kind="ExternalInput").ap()
    wod = nc.dram_tensor("w_out", [D, D], F32, kind="ExternalInput").ap()
    bcd = nc.dram_tensor("bc", [128, 3 * D + NE], F32, kind="ExternalInput").ap()
    sh2d = nc.dram_tensor("sh2", [128, D], F32, kind="ExternalInput").ap()
    wrd = nc.dram_tensor("w_r", [D, NE], F32, kind="ExternalInput").ap()
    cf32d = nc.dram_tensor("cf32", [128, 128 + 640], F32, kind="ExternalInput").ap()
    cbfd = nc.dram_tensor("cbf", [128, 384], BF16, kind="ExternalInput").ap()
    hres_o = nc.dram_tensor("hres", [TL, D], F32, kind="ExternalOutput").ap()
    G_o = nc.dram_tensor("G", [TL, NE], F32, kind="ExternalOutput").ap()
    XT_o = nc.dram_tensor("XT", [NE, D, CAP], BF16, kind="ExternalOutput").ap()
    V, S, G_, T = nc.vector, nc.scalar, nc.gpsimd, nc.tensor
    with contextlib.ExitStack() as st:
        P = Prog(nc, st)
        R = P.res
        bank = [P.ps(f"bank{i}", [128, 512]) for i in range(8)]
        rb = [R(f"bank{i}") for i in range(8)]
        wobf = P.sb("wob", [128, KC * D], BF16); r_wob = R()
        wob = wobf[:, :].rearrange("p (kc n) -> p kc n", kc=KC)
        bc = P.sb("bct", [128, 3 * D + NE], F32); r_bc = R()
        g1b, gs2b, brb = bc[:, 0:D], bc[:, D:2 * D], bc[:, 3 * D:3 * D + NE]
        sh2b = P.sb("sh2b", [128, D], F32); r_sh2 = R()
        wr = P.sb("wr", [128, KC, NE], F32); r_wr = R()
        c32 = P.sb("c32", [128, 768], F32); r_c32 = R()
        cb = P.sb("cb", [128, 384], BF16); r_cb = R()
        ID32, IOTA = c32[:, 0:128], c32[:, 128:128 + CAP]
        mx = [P.sb(f"mx{i}", [128, KC, 128], BF16) for i in range(2)]; r_mx = [R(), R()]
        _xt = P.sb("xt0", [128, D], F32); _rxt = R(); xt = [_xt, _xt]; r_xt = [_rxt, _rxt]
        hres = [P.sb(f"hres{i}", [128, D], F32) for i in range(2)]; r_hres = [R(), R()]
        h2f = P.sb("h2f", [128, D], F32); r_h2f = R()
        h2b = P.sb("h2b", [128, NTL, D], BF16); r_h2b = [R() for _ in range(NTL)]
        h2T = P.sb("h2T", [128, KC, 128], F32); r_h2T = R()
        maskb = P.sb("maskb", [128, NTL, NE], BF16); r_maskb = R()
        rk = P.sb("rk", [128, NTL, NE], F32); r_rk = R()
        sm = [P.sb(f"sm{i}", [128, 16], F32) for i in range(2)]; r_sm = [R(), R()]
        lg = [P.sb(f"lg{i}", [128, 4 * NE], F32) for i in range(2)]; r_lg = [R(), R()]
        assert 2 * NTL * CAP + 2 * KC * CAP <= KC * D
        sel = [wobf[:, q * NTL * CAP:(q + 1) * NTL * CAP].rearrange("p (i s) -> p i s", i=NTL) for q in range(2)]; r_sel = [R(), R()]
        xb0 = 2 * NTL * CAP
        xte = [wobf[:, xb0 + q * KC * CAP:xb0 + (q + 1) * KC * CAP].rearrange("p (fc s) -> p fc s", fc=KC) for q in range(2)]; r_xte = [R(), R()]

        r_wobk = [R() for _ in range(KC)]
        for kc in range(KC):
            P.op("pool", lambda kc=kc: G_.dma_start(out=wob[:, kc, :], in_=wod[kc * 128:(kc + 1) * 128, :]), writes=[r_wobk[kc]], dma=r_wobk[kc])
        P.op("sp", lambda: nc.sync.dma_start(out=bc[:], in_=bcd), writes=[r_bc], dma=r_bc)
        P.op("sp", lambda: nc.sync.dma_start(out=sh2b[:], in_=sh2d), writes=[r_sh2], dma=r_sh2)
        P.op("sp", lambda: nc.sync.dma_start(out=wr[:], in_=wrd.rearrange("(kc p) n -> p kc n", p=128)), writes=[r_wr], dma=r_wr)
        P.op("sp", lambda: nc.sync.dma_start(out=c32[:], in_=cf32d), writes=[r_c32], dma=r_c32)
        P.op("sp", lambda: nc.sync.dma_start(out=cb[:], in_=cbfd), writes=[r_cb], dma=r_cb)
        P.op("dve", lambda: V.scalar_tensor_tensor(out=bc[:, D:2 * D], in0=bc[:, 2 * D:3 * D], scalar=1.0, in1=bc[:, D:2 * D], op0=ALU.add, op1=ALU.mult),
             reads=[r_bc], writes=[r_bc])
        mv = mixT.rearrange("(kc p) t -> p kc t", p=128)
        finals = []
        for i in range(NTL):
            p = i % 2
            tsl = slice(i * 128, (i + 1) * 128)
            smt, r_smt = sm[p], r_sm[p]
            lgt, r_lgt = lg[p], r_lg[p]
            P.op("sp", lambda p=p, tsl=tsl: nc.sync.dma_start(out=mx[p][:], in_=mv[:, :, tsl]), writes=[r_mx[p]], dma=r_mx[p])
            P.op("sp", lambda p=p, tsl=tsl: nc.sync.dma_start(out=xt[p][:], in_=xd[tsl, :]), writes=[r_xt[p]], dma=r_xt[p])
            for dg in range(4):
                for kc in range(KC):
                    P.op("pe", lambda p=p, dg=dg, kc=kc: T.matmul(bank[dg][:, :], lhsT=mx[p][:, kc, :], rhs=wob[:, kc, dg * 512:(dg + 1) * 512],
                                                                 start=(kc == 0), stop=(kc == KC - 1)), reads=[r_mx[p], r_wobk[kc]], writes=[rb[dg]])
            for dg in range(4):
                ds = slice(dg * 512, (dg + 1) * 512)
                P.op("dve", lambda p=p, dg=dg, ds=ds: V.tensor_tensor(out=hres[p][:, ds], in0=bank[dg][:, :], in1=g1b[:, ds], op=ALU.mult),
                     reads=[r_bc], writes=[rb[dg], r_hres[p]])
                P.op("pool", lambda p=p, ds=ds: G_.tensor_tensor(out=hres[p][:, ds], in0=hres[p][:, ds], in1=xt[p][:, ds], op=ALU.add),
                     reads=[r_xt[p], r_hres[p]], writes=[r_hres[p]])
            f = P.op("sp", lambda p=p, tsl=tsl: nc.sync.dma_start(out=hres_o[tsl, :], in_=hres[p][:]), reads=[r_hres[p]], dma=r_hres[p])
            finals.append(f)
            P.op("pool", lambda p=p: G_.tensor_tensor(out=h2f[:], in0=hres[p][:], in1=hres[p][:], op=ALU.mult), reads=[r_hres[p]], writes=[r_h2f])
            P.op("dve", lambda smt=smt: V.tensor_reduce(out=smt[:, 0:1], in_=h2f[:], axis=AX.X, op=ALU.add), reads=[r_h2f], writes=[r_smt])
            P.op("act", lambda smt=smt: S.activation(out=smt[:, 1:2], in_=smt[:, 0:1], func=AF.Ln, bias=EPS, scale=1.0 / D), reads=[r_smt], writes=[r_smt])
            P.op("act", lambda smt=smt: S.activation(out=smt[:, 2:3], in_=smt[:, 1:2], func=AF.Exp, scale=-0.5), reads=[r_smt], writes=[r_smt])
            P.op("dve", lambda p=p, smt=smt: V.scalar_tensor_tensor(out=h2f[:], in0=hres[p][:], scalar=smt[:, 2:3], in1=gs2b, op0=ALU.mult, op1=ALU.mult),
                 reads=[r_hres[p], r_smt, r_bc, r_h2f], writes=[r_h2f])
            P.op("pool", lambda: G_.tensor_tensor(out=h2f[:], in0=h2f[:], in1=sh2b[:], op=ALU.add), reads=[r_sh2, r_h2f], writes=[r_h2f])
            P.op("act", lambda i=i: S.copy(out=h2b[:, i, :], in_=h2f[:]), reads=[r_h2f], writes=[r_h2b[i]])
            for q4 in range(4):
                tb = 4 + (q4 % 2)
                for k4 in range(4):
                    kc = q4 * 4 + k4
                    P.op("pe", lambda tb=tb, k4=k4, kc=kc: T.transpose(out=bank[tb][:, k4 * 128:(k4 + 1) * 128], in_=h2f[:, kc * 128:(kc + 1) * 128], identity=ID32),
                         reads=[r_h2f, r_c32], writes=[rb[tb]])
                eng = "act" if q4 % 2 == 0 else "dve"
                if eng == "act":
                    P.op("act", lambda tb=tb, q4=q4: S.copy(out=h2T[:, q4 * 4:(q4 + 1) * 4, :], in_=bank[tb][:, :].rearrange("p (a b) -> p a b", a=4)),
                         writes=[rb[tb], r_h2T])
                else:
                    P.op("dve", lambda tb=tb, q4=q4: V.tensor_copy(out=h2T[:, q4 * 4:(q4 + 1) * 4, :], in_=bank[tb][:, :].rearrange("p (a b) -> p a b", a=4)),
                         writes=[rb[tb], r_h2T])
            for kc in range(KC):
                P.op("pe", lambda kc=kc: T.matmul(bank[6][:, 0:NE], lhsT=h2T[:, kc, :], rhs=wr[:, kc, :], start=(kc == 0), stop=(kc == KC - 1)),
                     reads=[r_h2T, r_wr], writes=[rb[6]])
            P.op("dve", lambda lgt=lgt: V.tensor_tensor(out=lgt[:, 0:NE], in0=bank[6][:, 0:NE], in1=brb, op=ALU.add), reads=[r_bc], writes=[rb[6], r_lgt])
            P.op("dve", lambda lgt=lgt, smt=smt: V.max(out=smt[:, 8:16], in_=lgt[:, 0:NE]), reads=[r_lgt], writes=[r_smt])
            P.op("dve", lambda lgt=lgt, smt=smt: V.tensor_scalar(out=lgt[:, NE:2 * NE], in0=lgt[:, 0:NE], scalar1=smt[:, 11:12], scalar2=None, op0=ALU.is_ge),
                 reads=[r_lgt, r_smt], writes=[r_lgt])
            P.op("dve", lambda smt=smt: V.tensor_scalar(out=smt[:, 3:4], in0=smt[:, 8:9], scalar1=-1.0, scalar2=None, op0=ALU.mult), reads=[r_smt], writes=[r_smt])
            P.op("act", lambda lgt=lgt, smt=smt: S.activation(out=lgt[:, 2 * NE:3 * NE], in_=lgt[:, 0:NE], func=AF.Exp, bias=smt[:, 3:4], scale=1.0),
                 reads=[r_lgt, r_smt], writes=[r_lgt])
            P.op("dve", lambda lgt=lgt: V.tensor_tensor(out=lgt[:, 2 * NE:3 * NE], in0=lgt[:, 2 * NE:3 * NE], in1=lgt[:, NE:2 * NE], op=ALU.mult), reads=[r_lgt], writes=[r_lgt])
            P.op("dve", lambda lgt=lgt, smt=smt: V.tensor_reduce(out=smt[:, 4:5], in_=lgt[:, 2 * NE:3 * NE], axis=AX.X, op=ALU.add), reads=[r_lgt], writes=[r_smt])
            P.op("dve", lambda smt=smt: V.reciprocal(out=smt[:, 5:6], in_=smt[:, 4:5]), reads=[r_smt], writes=[r_smt])
            P.op("dve", lambda lgt=lgt, smt=smt: V.tensor_scalar(out=lgt[:, 3 * NE:4 * NE], in0=lgt[:, 2 * NE:3 * NE], scalar1=smt[:, 5:6], scalar2=None, op0=ALU.mult),
                 reads=[r_lgt, r_smt], writes=[r_lgt])
            f = P.op("sp", lambda lgt=lgt, tsl=tsl: nc.sync.dma_start(out=G_o[tsl, :], in_=lgt[:, 3 * NE:4 * NE]), reads=[r_lgt], dma=r_lgt)
            finals.append(f)
            P.op("pool", lambda lgt=lgt, i=i: G_.tensor_copy(out=maskb[:, i, :], in_=lgt[:, NE:2 * NE]), reads=[r_lgt], writes=[r_maskb])
            rank_ops(P, nc, bank[6], rb[6], maskb, r_maskb, i, cb, r_cb, lgt[:, NE:2 * NE], r_lgt, rk[:, i, :], r_rk)
        gbanks = [0, 1, 2, 3, 7, 4, 5]
        gi = 0
        ncc = (CAP + 511) // 512
        ccw = CAP // ncc
        P.op("dve", lambda: V.memset(sel[0][:, 0, 0:2], 0.0), writes=r_wobk + r_sel + r_xte)
        def mksel(e):
            q = e % 2
            for i in range(NTL):
                P.op("dve", lambda i=i: V.tensor_scalar(out=sel[q][:, i, :], in0=IOTA, scalar1=rk[:, i, e:e + 1], scalar2=None, op0=ALU.is_equal),
                     reads=[r_c32, r_rk], writes=[r_sel[q]])
        mksel(0)
        for e in range(NE):
            q = e % 2
            if e + 1 < NE:
                mksel(e + 1)
            for fc in range(KC):
                for cc in range(ncc):
                    cs = slice(cc * ccw, (cc + 1) * ccw)
                    gbk = gbanks[gi % len(gbanks)]; gi += 1
                    for i in range(NTL):
                        P.op("pe", lambda gbk=gbk, q=q, i=i, fc=fc, cs=cs: T.matmul(bank[gbk][:, 0:ccw], lhsT=h2b[:, i, fc * 128:(fc + 1) * 128], rhs=sel[q][:, i, cs],
                                                                                start=(i == 0), stop=(i == NTL - 1)), reads=[r_h2b[i], r_sel[q]], writes=[rb[gbk]])
                    if gi % 2 == 0:
                        P.op("act", lambda gbk=gbk, q=q, fc=fc, cs=cs: S.copy(out=xte[q][:, fc, cs], in_=bank[gbk][:, 0:ccw]), writes=[rb[gbk], r_xte[q]])
                    else:
                        P.op("dve", lambda gbk=gbk, q=q, fc=fc, cs=cs: V.tensor_copy(out=xte[q][:, fc, cs], in_=bank[gbk][:, 0:ccw]), writes=[rb[gbk], r_xte[q]])
            f = P.op("sp", lambda q=q, e=e: nc.sync.dma_start(out=XT_o[e].rearrange("(fc p) s -> p fc s", p=128), in_=xte[q][:]), reads=[r_xte[q]], dma=r_xte[q])
            finals.append(f)
        P.emit(final_ops=finals)
    return nc


def l2_inputs(mixT_g, x_g, w_out, g1, norm_ffn, sc2, sh2, w_router, b_router):
    bcst = lambda v: np.broadcast_to(v[None, :], (128, v.shape[0]))
    bc = np.ascontiguousarray(np.concatenate([bcst(g1), bcst(norm_ffn), bcst(sc2), bcst(b_router)], axis=1), dtype=np.float32)
    sl = np.tril(np.ones((128, 128), np.float32), -1).T
    cf32 = np.ascontiguousarray(np.concatenate([np.eye(128, dtype=np.float32), bcst(np.arange(640, dtype=np.float32))], axis=1))
    cbf = np.ascontiguousarray(np.concatenate([np.eye(128, dtype=np.float32), sl, np.ones((128, 128), np.float32)], axis=1)).astype(ml_dtypes.bfloat16)
    return {"mixT": mixT_g, "x": x_g, "w_out": w_out, "bc": bc, "sh2": np.ascontiguousarray(bcst(sh2), dtype=np.float32),
            "w_r": w_router, "cf32": cf32, "cbf": cbf}


EPC = 4


def build_l3(NS, NCH=1):
    nc = new_nc()
    NSC = NS // NCH
    NSG = (NSC + 511) // 512
    SGW = NSC // NSG
    NSB = NSC // 128
    xtd = nc.dram_tensor("XT", [EPC, D, NS], BF16, kind="ExternalInput").ap()
    wugd = nc.dram_tensor("w_ug", [EPC, D, 2 * D], F32, kind="ExternalInput").ap()
    bugd = nc.dram_tensor("b_ug", [EPC, 128, 32], F32, kind="ExternalInput").ap()
    wdd = nc.dram_tensor("w_d", [EPC, D, D], F32, kind="ExternalInput").ap()
    bdd = nc.dram_tensor("b_d", [EPC, 128, D], F32, kind="ExternalInput").ap()
    yd = nc.dram_tensor("y", [EPC, NS, D], BF16, kind="ExternalOutput").ap()
    cug = nc.dram_tensor("cache_ug", [32, 128, KC * 128], BF16).ap() if NCH > 1 else None
    cdn = nc.dram_tensor("cache_dn", [KC, 128, D], BF16).ap() if NCH > 1 else None
    V, S, G_, T = nc.vector, nc.scalar, nc.gpsimd, nc.tensor
    with contextlib.ExitStack() as st:
        P = Prog(nc, st)
        R = P.res
        bank = [P.ps(f"bank{i}", [128, 512]) for i in range(8)]
        rb = [R(f"bank{i}") for i in range(8)]
        xs = P.sb("xs", [128, KC, NSC], BF16); r_xs = R()
        actT = P.sb("actT", [128, KC, NSC], BF16); r_actT = [R() for _ in range(KC)]
        wd = P.sb("wd", [128, KC, D], BF16); r_wd = [R() for _ in range(KC)]
        NW = 3
        wg = [P.sb(f"wg{i}", [128, KC, 128], BF16) for i in range(NW)]; r_wg = [R() for _ in range(NW)]
        wu = [P.sb(f"wu{i}", [128, KC, 128], BF16) for i in range(NW)]; r_wu = [R() for _ in range(NW)]
        bug = P.sb("bug", [128, 32], F32); r_bug = R()
        bdb = P.sb("bdb", [128, D], F32); r_bdb = R()
        gcl = [P.sb(f"gcl{i}", [128, SGW], F32) for i in range(2)]; r_gcl = [R(), R()]
        sig = [P.sb(f"sig{i}", [128, SGW], F32) for i in range(2)]; r_sig = [R(), R()]
        ucl = [P.sb(f"ucl{i}", [128, SGW], F32) for i in range(2)]; r_ucl = [R(), R()]
        yst = [P.sb(f"yst{i}", [128, D], BF16) for i in range(2)]; r_yst = [R(), R()]
        r_cug = [R() for _ in range(32)]
        r_cdn = [R() for _ in range(KC)]
        r_wgst = [R() for _ in range(NW)]; r_wust = [R() for _ in range(NW)]; r_wdst = [R() for _ in range(KC)]
        finals = []
        wi = 0
        ei = 0
        for v in range(EPC * NCH):
            e, ch = v // NCH, v % NCH
            so = ch * NSC
            P.op("sp", lambda e=e, so=so: nc.sync.dma_start(out=xs[:], in_=xtd[e][:, so:so + NSC].rearrange("(kc p) s -> p kc s", p=128)), writes=[r_xs], dma=r_xs)
            if ch == 0:
                P.op("sp", lambda e=e: nc.sync.dma_start(out=bug[:], in_=bugd[e]), writes=[r_bug], dma=r_bug)
                P.op("sp", lambda e=e: nc.sync.dma_start(out=bdb[:], in_=bdd[e]), writes=[r_bdb], dma=r_bdb)
            wv = wugd[e].rearrange("(kc p) n -> p kc n", p=128)
            for c in range(KC):
                w = wi % NW; wi += 1
                if ch == 0:
                    P.op("pool", lambda w=w, c=c, wv=wv: G_.dma_start(out=wg[w][:], in_=wv[:, :, c * 128:(c + 1) * 128]), writes=[r_wg[w]], dma=r_wg[w])
                    P.op("pool", lambda w=w, c=c, wv=wv: G_.dma_start(out=wu[w][:], in_=wv[:, :, D + c * 128:D + (c + 1) * 128]), writes=[r_wu[w]], dma=r_wu[w])
                    P.op("pool", lambda e=e, c=c: G_.dma_start(out=wd[:, c, :], in_=wdd[e, c * 128:(c + 1) * 128, :]), writes=[r_wd[c]], dma=r_wd[c])
                    if NCH > 1:
                        P.op("act", lambda w=w, c=c: S.dma_start(out=cug[c], in_=wg[w][:].rearrange("p a b -> p (a b)")), reads=[r_wg[w]], writes=[r_cug[c]], dma=r_wgst[w])
                        P.op("act", lambda w=w, c=c: S.dma_start(out=cug[16 + c], in_=wu[w][:].rearrange("p a b -> p (a b)")), reads=[r_wu[w]], writes=[r_cug[16 + c]], dma=r_wust[w])
                        P.op("act", lambda c=c: S.dma_start(out=cdn[c], in_=wd[:, c, :]), reads=[r_wd[c]], writes=[r_cdn[c]], dma=r_wdst[c])
                else:
                    P.op("sp", lambda w=w, c=c: nc.sync.dma_start(out=wg[w][:].rearrange("p a b -> p (a b)"), in_=cug[c]), reads=[r_cug[c]], writes=[r_wg[w]], dma=r_wg[w])
                    P.op("sp", lambda w=w, c=c: nc.sync.dma_start(out=wu[w][:].rearrange("p a b -> p (a b)"), in_=cug[16 + c]), reads=[r_cug[16 + c]], writes=[r_wu[w]], dma=r_wu[w])
                    P.op("sp", lambda c=c: nc.sync.dma_start(out=wd[:, c, :], in_=cdn[c]), reads=[r_cdn[c]], writes=[r_wd[c]], dma=r_wd[c])
                for sg in range(NSG):
                    ss = slice(sg * SGW, (sg + 1) * SGW)
                    bg_, bu_ = sg * 2, sg * 2 + 1
                    for kc in range(KC):
                        P.op("pe", lambda w=w, kc=kc, ss=ss, bg_=bg_: T.matmul(bank[bg_][:, 0:SGW], lhsT=wg[w][:, kc, :], rhs=xs[:, kc, ss], start=(kc == 0), stop=(kc == KC - 1)),
                             reads=[r_wg[w], r_xs], writes=[rb[bg_]])
                    for kc in range(KC):
                        P.op("pe", lambda w=w, kc=kc, ss=ss, bu_=bu_: T.matmul(bank[bu_][:, 0:SGW], lhsT=wu[w][:, kc, :], rhs=xs[:, kc, ss], start=(kc == 0), stop=(kc == KC - 1)),
                             reads=[r_wu[w], r_xs], writes=[rb[bu_]])
                    q = ei % 2; ei += 1
                    P.op("dve", lambda q=q, c=c, bg_=bg_: V.tensor_scalar(out=gcl[q][:], in0=bank[bg_][:, 0:SGW], scalar1=bug[:, c:c + 1], scalar2=7.0, op0=ALU.add, op1=ALU.min),
                         reads=[r_bug], writes=[rb[bg_], r_gcl[q]])
                    P.op("dve", lambda q=q, c=c, bu_=bu_: V.tensor_scalar(out=ucl[q][:], in0=bank[bu_][:, 0:SGW], scalar1=bug[:, 16 + c:17 + c], scalar2=7.0, op0=ALU.add, op1=ALU.min),
                         reads=[r_bug], writes=[rb[bu_], r_ucl[q]])
                    P.op("act", lambda q=q: S.activation(out=sig[q][:], in_=gcl[q][:], func=AF.Sigmoid, scale=1.702), reads=[r_gcl[q]], writes=[r_sig[q]])
                    P.op("dve", lambda q=q: V.tensor_scalar(out=ucl[q][:], in0=ucl[q][:], scalar1=-7.0, scalar2=1.0, op0=ALU.max, op1=ALU.add), reads=[r_ucl[q]], writes=[r_ucl[q]])
                    P.op("dve", lambda q=q: V.tensor_tensor(out=gcl[q][:], in0=gcl[q][:], in1=sig[q][:], op=ALU.mult), reads=[r_gcl[q], r_sig[q]], writes=[r_gcl[q]])
                    P.op("dve", lambda q=q, c=c, ss=ss: V.tensor_tensor(out=actT[:, c, ss], in0=gcl[q][:], in1=ucl[q][:], op=ALU.mult), reads=[r_gcl[q], r_ucl[q]], writes=[r_actT[c]])
            for sb in range(NSB):
                yq = sb % 2
                for dg in range(4):
                    bk = 6 + (dg % 2)
                    for kc in range(KC):
                        P.op("pe", lambda sb=sb, dg=dg, kc=kc, bk=bk: T.matmul(bank[bk][:, :], lhsT=actT[:, kc, sb * 128:(sb + 1) * 128], rhs=wd[:, kc, dg * 512:(dg + 1) * 512],
                                                                             start=(kc == 0), stop=(kc == KC - 1)), reads=[r_actT[kc], r_wd[kc]], writes=[rb[bk]])
                    P.op("dve", lambda yq=yq, dg=dg, bk=bk: V.tensor_tensor(out=yst[yq][:, dg * 512:(dg + 1) * 512], in0=bank[bk][:, :], in1=bdb[:, dg * 512:(dg + 1) * 512], op=ALU.add),
                         reads=[r_bdb], writes=[rb[bk], r_yst[yq]])
                f = P.op("sp", lambda e=e, sb=sb, yq=yq, so=so: nc.sync.dma_start(out=yd[e, so + sb * 128:so + (sb + 1) * 128, :], in_=yst[yq][:]), reads=[r_yst[yq]], dma=r_yst[yq])
                finals.append(f)
        P.emit(final_ops=finals[-2:])
    return nc


def build_l4(TL, CAP):
    NTL = TL // 128
    nc = new_nc()
    NCK = (CAP + 127) // 128
    CW = CAP // NCK
    yd = nc.dram_tensor("yb", [NE, CAP, D], BF16, kind="ExternalInput").ap()
    Gd = nc.dram_tensor("G", [TL, NE], F32, kind="ExternalInput").ap()
    hrd = nc.dram_tensor("hres", [TL, D], F32, kind="ExternalInput").ap()
    bcd = nc.dram_tensor("bc", [128, 4 * D], F32, kind="ExternalInput").ap()
    cf32d = nc.dram_tensor("cf32", [128, 768], F32, kind="ExternalInput").ap()
    cbfd = nc.dram_tensor("cbf", [128, 384], BF16, kind="ExternalInput").ap()
    outd = nc.dram_tensor("out", [TL, D], F32, kind="ExternalOutput").ap()
    V, S, G_, T = nc.vector, nc.scalar, nc.gpsimd, nc.tensor
    with contextlib.ExitStack() as st:
        P = Prog(nc, st)
        R = P.res
        bank = [P.ps(f"bank{i}", [128, 512]) for i in range(8)]
        rb = [R(f"bank{i}") for i in range(8)]
        bc = P.sb("bct", [128, 4 * D], F32); r_bc = R()
        c32 = P.sb("c32", [128, 768], F32); r_c32 = R()
        cb = P.sb("cb", [128, 384], BF16); r_cb = R()
        IOTA = c32[:, 128:128 + CAP]
        Gt = P.sb("Gt", [128, NTL, NE], F32); r_G = R()
        mask32 = P.sb("mask32", [128, NTL, NE], F32); r_mask = R()
        maskb = P.sb("maskb", [128, NTL, NE], BF16); r_maskb = R()
        rk = P.sb("rk", [128, NTL, NE], F32); r_rk = R()
        acc = P.sb("acc", [128, NTL, D], F32); r_acc = [R() for _ in range(NTL)]
        yb = [P.sb(f"yb{i}", [128, NCK, D], BF16) for i in range(2)]; r_yb = [R(), R()]
        selw = [P.sb(f"selw{i}", [128, CAP], BF16) for i in range(2)]; r_selw = [R(), R()]
        swT = [P.sb(f"swT{i}", [128, NCK, 128], BF16) for i in range(2)]; r_swT = [R(), R()]
        hr = P.sb("hr", [128, D], F32); r_hr = R()
        tmp = P.sb("tmp", [128, D], F32); r_tmp = R()
        ot = [P.sb(f"ot{i}", [128, D], F32) for i in range(2)]; r_ot = [R(), R()]
        sm = [P.sb(f"sm{i}", [128, 8], F32) for i in range(2)]; r_sm = [R(), R()]
        P.op("sp", lambda: nc.sync.dma_start(out=bc[:], in_=bcd), writes=[r_bc], dma=r_bc)
        P.op("sp", lambda: nc.sync.dma_start(out=c32[:], in_=cf32d), writes=[r_c32], dma=r_c32)
        P.op("sp", lambda: nc.sync.dma_start(out=cb[:], in_=cbfd), writes=[r_cb], dma=r_cb)
        P.op("sp", lambda: nc.sync.dma_start(out=Gt[:], in_=Gd.rearrange("(i p) e -> p i e", p=128)), writes=[r_G], dma=r_G)
        P.op("pool", lambda: G_.memset(acc[:], 0.0), writes=r_acc)
        P.op("dve", lambda: V.scalar_tensor_tensor(out=bc[:, D:2 * D], in0=bc[:, 2 * D:3 * D], scalar=1.0, in1=bc[:, D:2 * D], op0=ALU.add, op1=ALU.mult),
             reads=[r_bc], writes=[r_bc])
        g2b, gsfb, shfb = bc[:, 0:D], bc[:, D:2 * D], bc[:, 3 * D:4 * D]
        P.op("dve", lambda: V.tensor_scalar(out=mask32[:], in0=Gt[:], scalar1=0.0, scalar2=None, op0=ALU.is_gt), reads=[r_G], writes=[r_mask])
        P.op("pool", lambda: G_.tensor_copy(out=maskb[:], in_=mask32[:]), reads=[r_mask], writes=[r_maskb])
        for i in range(NTL):
            rank_ops(P, nc, bank[6], rb[6], maskb, r_maskb, i, cb, r_cb, mask32[:, i, :], r_mask, rk[:, i, :], r_rk, lo=0)
        def prep(n):
            e, i = n // NTL, n % NTL
            k, q = e % 2, n % 2
            if i == 0:
                if CAP % 128 == 0:
                    P.op("sp", lambda: nc.sync.dma_start(out=yb[k][:], in_=yd[e].rearrange("(ck p) d -> p ck d", p=128)), writes=[r_yb[k]], dma=r_yb[k])
                else:
                    P.op("sp", lambda: nc.sync.dma_start(out=yb[k][0:CW, 0, :], in_=yd[e]), writes=[r_yb[k]], dma=r_yb[k])
            P.op("dve", lambda: V.tensor_scalar(out=selw[q][:], in0=IOTA, scalar1=rk[:, i, e:e + 1], scalar2=Gt[:, i, e:e + 1], op0=ALU.is_equal, op1=ALU.mult),
                 reads=[r_c32, r_rk, r_G], writes=[r_selw[q]])
            tbk = 4 + q
            tview = bank[tbk][0:CW, :].bitcast(BF16)
            for ck in range(NCK):
                P.op("pe", lambda ck=ck: T.transpose(out=tview[:, ck * 128:(ck + 1) * 128], in_=selw[q][:, ck * CW:(ck + 1) * CW], identity=cb[:, 0:128]),
                     reads=[r_selw[q], r_cb], writes=[rb[tbk]])
            P.op("act", lambda: S.copy(out=swT[q][0:CW, :, :], in_=tview[:, 0:NCK * 128].rearrange("p (a b) -> p a b", a=NCK)), writes=[rb[tbk], r_swT[q]])

        def compute(n):
            e, i = n // NTL, n % NTL
            k, q = e % 2, n % 2
            for dg in range(4):
                for ck in range(NCK):
                    P.op("pe", lambda ck=ck, dg=dg: T.matmul(bank[dg][:, :], lhsT=swT[q][0:CW, ck, :], rhs=yb[k][0:CW, ck, dg * 512:(dg + 1) * 512],
                                                            start=(ck == 0), stop=(ck == NCK - 1)), reads=[r_swT[q], r_yb[k]], writes=[rb[dg]])
                ds = slice(dg * 512, (dg + 1) * 512)
                P.op("dve", lambda dg=dg, ds=ds: V.tensor_tensor(out=acc[:, i, ds], in0=bank[dg][:, :], in1=acc[:, i, ds], op=ALU.add),
                     reads=[r_acc[i]], writes=[rb[dg], r_acc[i]])

        NIT = NE * NTL
        prep(0)
        for n in range(NIT):
            if n + 1 < NIT:
                prep(n + 1)
            compute(n)
        finals = []
        for i in range(NTL):
            p = i % 2
            tsl = slice(i * 128, (i + 1) * 128)
            smt, r_smt = sm[p], r_sm[p]
            P.op("sp", lambda tsl=tsl: nc.sync.dma_start(out=hr[:], in_=hrd[tsl, :]), writes=[r_hr], dma=r_hr)
            P.op("dve", lambda i=i: V.tensor_tensor(out=acc[:, i, :], in0=acc[:, i, :], in1=g2b, op=ALU.mult), reads=[r_bc, r_acc[i]], writes=[r_acc[i]])
            P.op("pool", lambda i=i: G_.tensor_tensor(out=acc[:, i, :], in0=acc[:, i, :], in1=hr[:], op=ALU.add), reads=[r_hr, r_acc[i]], writes=[r_acc[i]])
            P.op("pool", lambda i=i: G_.tensor_tensor(out=tmp[:], in0=acc[:, i, :], in1=acc[:, i, :], op=ALU.mult), reads=[r_acc[i]], writes=[r_tmp])
            P.op("dve", lambda smt=smt: V.tensor_reduce(out=smt[:, 0:1], in_=tmp[:], axis=AX.X, op=ALU.add), reads=[r_tmp], writes=[r_smt])
            P.op("act", lambda smt=smt: S.activation(out=smt[:, 1:2], in_=smt[:, 0:1], func=AF.Ln, bias=EPS, scale=1.0 / D), reads=[r_smt], writes=[r_smt])
            P.op("act", lambda smt=smt: S.activation(out=smt[:, 2:3], in_=smt[:, 1:2], func=AF.Exp, scale=-0.5), reads=[r_smt], writes=[r_smt])
            P.op("dve", lambda smt=smt, i=i: V.scalar_tensor_tensor(out=tmp[:], in0=acc[:, i, :], scalar=smt[:, 2:3], in1=gsfb, op0=ALU.mult, op1=ALU.mult),
                 reads=[r_acc[i], r_smt, r_bc, r_tmp], writes=[r_tmp])
            P.op("pool", lambda p=p: G_.tensor_tensor(out=ot[p][:], in0=tmp[:], in1=shfb, op=ALU.add), reads=[r_tmp, r_bc], writes=[r_ot[p]])
            f = P.op("sp", lambda p=p, tsl=tsl: nc.sync.dma_start(out=outd[tsl, :], in_=ot[p][:]), reads=[r_ot[p]], dma=r_ot[p])
            finals.append(f)
        P.emit(final_ops=finals[-2:])
    return nc


TL_FULL = SEQ // NCORES
CAP_FULL = 512
NCH_FULL = 4


_DBG = {}


def _bcst(v):
    return np.broadcast_to(np.asarray(v)[None, :], (128, v.shape[0]))


def _run(nc, in_maps):
    return run_bass_kernel_spmd(nc, in_maps, core_ids=list(range(NCORES))).results


def kernel(x, c, w_ada, b_ada, norm_mix, w_in, b_i, b_f, fox_b_f, fox_q_norm, fox_k_norm,
           mlstm_out_norm, fox_out_norm, w_out, norm_ffn, w_router, b_router, w_up_gate,
           b_up_gate, w_down, b_down, w_ada_final, b_ada_final, norm_final):
    f32 = lambda a: np.asarray(a, dtype=np.float32)
    x = f32(x); c = f32(c)
    seq = x.shape[1]
    TL = seq // NCORES
    CAP = CAP_FULL
    mod = run_l0(c, f32(w_ada), f32(b_ada), f32(w_ada_final), f32(b_ada_final))
    sh1, sc1, g1, sh2, sc2, g2 = [mod[i * D:(i + 1) * D] for i in range(6)]
    shf, scf = mod[6 * D:7 * D], mod[7 * D:8 * D]
    xT = np.ascontiguousarray(x[0].T)
    w_in0 = f32(w_in)[0]
    in1 = [l1_inputs(j, xT, w_in0, f32(norm_mix)[0], sc1, sh1, f32(b_i)[0], f32(b_f)[0], f32(fox_b_f)[0],
                     f32(fox_q_norm)[0], f32(fox_k_norm)[0], f32(fox_out_norm)[0], f32(mlstm_out_norm)[0]) for j in range(NCORES)]
    r1 = _run(build_l1(seq), in1)
    del in1, xT
    mix = np.empty((seq, D), dtype=ml_dtypes.bfloat16)
    for j in range(NCORES):
        h, half = j // 2, j % 2
        o = np.asarray(r1[j]["hout"])
        mix[:, 256 * h + 128 * half:256 * h + 128 * half + 128] = o[:, 0:128]
        mix[:, 1024 + 128 * j:1024 + 128 * j + 128] = o[:, 128:256]
    mixT = np.ascontiguousarray(mix.T)
    w_out0 = f32(w_out)[0]
    in2 = [l2_inputs(np.ascontiguousarray(mixT[:, g * TL:(g + 1) * TL]), np.ascontiguousarray(x[0, g * TL:(g + 1) * TL]), w_out0, g1,
                     f32(norm_ffn)[0], sc2, sh2, f32(w_router)[0], f32(b_router)[0]) for g in range(NCORES)]
    r2 = _run(build_l2(TL, CAP), in2)
    cf32, cbf = in2[0]["cf32"], in2[0]["cbf"]
    del in2
    in3 = []
    for cidx in range(NCORES):
        es = list(range(EPC * cidx, EPC * cidx + EPC))
        XT = np.ascontiguousarray(np.stack([np.concatenate([np.asarray(r2[g]["XT"])[e] for g in range(NCORES)], axis=1) for e in es]))
        in3.append({"XT": XT, "w_ug": np.ascontiguousarray(f32(w_up_gate)[0, es[0]:es[-1] + 1]),
                    "b_ug": np.ascontiguousarray(np.stack([f32(b_up_gate)[0, e].reshape(32, 128).T for e in es])),
                    "w_d": np.ascontiguousarray(f32(w_down)[0, es[0]:es[-1] + 1]),
                    "b_d": np.ascontiguousarray(np.stack([_bcst(f32(b_down)[0, e]) for e in es]))})
    r3 = _run(build_l3(NCORES * CAP, NCH_FULL), in3)
    del in3
    bc4 = np.ascontiguousarray(np.concatenate([_bcst(g2), _bcst(f32(norm_final)), _bcst(scf), _bcst(shf)], axis=1), dtype=np.float32)
    in4 = []
    for g in range(NCORES):
        yb = np.ascontiguousarray(np.stack([np.asarray(r3[e // EPC]["y"])[e % EPC][g * CAP:(g + 1) * CAP] for e in range(NE)]))
        in4.append({"yb": yb, "G": np.asarray(r2[g]["G"]), "hres": np.asarray(r2[g]["hres"]), "bc": bc4, "cf32": cf32, "cbf": cbf})
    r4 = _run(build_l4(TL, CAP), in4)
    out = np.concatenate([np.asarray(r["out"]) for r in r4], axis=0)[None]
    _DBG.update(mod=mod, mix=mix, G=np.concatenate([np.asarray(r2[g]["G"]) for g in range(NCORES)]), hres=np.concatenate([np.asarray(r2[g]["hres"]) for g in range(NCORES)]))
    return out.astype(np.float32)
```
